# Optimizing a Trainium2 kernel written in Bass

```python
import math
import jax
import jax.numpy as jnp
from jax import lax
import numpy as np

D_MODEL = 1024
BATCH = 8
SEQ = 4096
DEPTH = 1

A_DK = 128
A_DV = 128
A_HEADS = D_MODEL // (2 * A_DV)
CONV_K = 4
B_DK = 128
B_DV = 128
B_HEADS = D_MODEL // (2 * B_DV)
ROPE_BASE = 10000.0
CHUNK = 64
N_GROUPS = 8
EXPERTS_PER_GROUP = 8
N_EXPERTS = N_GROUPS * EXPERTS_PER_GROUP
TOP_K = 2
D_EXPERT = 256
EPS = 1e-6

A_QK = A_HEADS * A_DK
A_V = A_HEADS * A_DV
A_CONV = 2 * A_QK + A_V
B_QK = B_HEADS * B_DK
B_V = B_HEADS * B_DV
D_MIX = A_V + B_V
IN_SIZES = (A_QK, A_QK, A_V, A_V, A_HEADS, A_HEADS, B_QK, B_QK, B_V, B_V)
D_IN = sum(IN_SIZES)

kernel_name = "hybrid_deltanet_retention_hmoe"


def rms_norm(x, w):
    xf = x.astype(jnp.float32)
    y = xf * lax.rsqrt(jnp.mean(xf * xf, axis=-1, keepdims=True) + EPS)
    return (y * w.astype(jnp.float32)).astype(x.dtype)


def l2norm(t):
    return t * lax.rsqrt(jnp.sum(t * t, axis=-1, keepdims=True) + EPS)


def to_heads(t, n_heads):
    b, s, _ = t.shape
    return t.reshape(b, s, n_heads, -1).transpose(0, 2, 1, 3)


def to_chunks(t):
    b, h, s = t.shape[:3]
    return t.reshape(b, h, s // CHUNK, CHUNK, *t.shape[3:])


def from_chunks(o):
    n, b, h, c, d = o.shape
    return jnp.moveaxis(o, 0, 2).reshape(b, h, n * c, d).transpose(0, 2, 1, 3)


def causal_conv(x, w):
    return lax.conv_general_dilated(
        x, w[:, None, :], window_strides=(1,), padding=[(CONV_K - 1, 0)],
        dimension_numbers=("NWC", "WIO", "NWC"), feature_group_count=x.shape[-1])


def rope(t, cos, sin):
    half = t.shape[-1] // 2
    t1, t2 = t[..., :half], t[..., half:]
    return jnp.concatenate([t1 * cos - t2 * sin, t1 * sin + t2 * cos], axis=-1)


def gated_delta_net(qkv, gate, beta_in, alpha_in, conv_w, a_log, dt_bias, norm_w):
    b, s, _ = qkv.shape
    qkv = jax.nn.silu(causal_conv(qkv, conv_w.astype(jnp.float32)))
    q, k, v = jnp.split(qkv, [A_QK, 2 * A_QK], axis=-1)
    q = l2norm(to_heads(q, A_HEADS)) * (A_DK ** -0.5)
    k = l2norm(to_heads(k, A_HEADS))
    v = to_heads(v, A_HEADS)
    beta = jax.nn.sigmoid(beta_in).transpose(0, 2, 1)
    g = (-jnp.exp(a_log.astype(jnp.float32))
         * jax.nn.softplus(alpha_in + dt_bias.astype(jnp.float32))).transpose(0, 2, 1)
    q, k, v, beta, g = (to_chunks(t) for t in (q, k, v, beta, g))
    gc = jnp.cumsum(g, axis=-1)
    causal = jnp.tril(jnp.ones((CHUNK, CHUNK), dtype=bool))
    strict = jnp.tril(jnp.ones((CHUNK, CHUNK), dtype=bool), -1)
    diff = gc[..., :, None] - gc[..., None, :]
    decay = jnp.where(causal, jnp.exp(jnp.where(causal, diff, 0.0)), 0.0)
    kb = k * beta[..., None]
    a_mat = jnp.where(strict, jnp.einsum("bhnid,bhnjd->bhnij", kb, k) * decay, 0.0)
    rhs = jnp.concatenate([v * beta[..., None], kb * jnp.exp(gc)[..., None]], axis=-1)
    uw = lax.linalg.triangular_solve(a_mat, rhs, left_side=True, lower=True, unit_diagonal=True)
    u, w = uw[..., :A_DV], uw[..., A_DV:]
    attn = jnp.einsum("bhnid,bhnjd->bhnij", q, k) * decay
    q_dec = q * jnp.exp(gc)[..., None]
    k_dec = k * jnp.exp(gc[..., -1:] - gc)[..., None]
    last = jnp.exp(gc[..., -1])

    def step(state, inp):
        q_c, k_c, u_c, w_c, attn_c, last_c = inp
        v_new = u_c - jnp.einsum("bhck,bhkv->bhcv", w_c, state)
        o_c = (jnp.einsum("bhck,bhkv->bhcv", q_c, state)
               + jnp.einsum("bhcj,bhjv->bhcv", attn_c, v_new))
        state = state * last_c[..., None, None] + jnp.einsum("bhck,bhcv->bhkv", k_c, v_new)
        return state, o_c

    xs = tuple(jnp.moveaxis(t, 2, 0) for t in (q_dec, k_dec, u, w, attn, last))
    s0 = jnp.zeros((b, A_HEADS, A_DK, A_DV), jnp.float32)
    _, o = lax.scan(step, s0, xs)
    o = from_chunks(o)
    o = rms_norm(o, norm_w) * jax.nn.silu(gate.reshape(b, s, A_HEADS, A_DV))
    return o.reshape(b, s, A_V)


def retention(q, k, v, gate, cos, sin, norm_w):
    b, s, _ = q.shape
    q = rope(to_heads(q, B_HEADS), cos, sin)
    k = rope(to_heads(k, B_HEADS), cos, sin) * (B_DK ** -0.5)
    v = to_heads(v, B_HEADS)
    log_gamma = jnp.log(1.0 - 2.0 ** (-5.0 - jnp.arange(B_HEADS, dtype=jnp.float32)))
    pos = jnp.arange(CHUNK, dtype=jnp.float32)
    rel = pos[:, None] - pos[None, :]
    decay = jnp.where(rel >= 0, jnp.exp(jnp.maximum(rel, 0.0) * log_gamma[:, None, None]), 0.0)
    q, k, v = (to_chunks(t) for t in (q, k, v))
    attn = jnp.einsum("bhnid,bhnjd->bhnij", q, k) * decay[None, :, None]
    inner = jnp.einsum("bhnij,bhnjv->bhniv", attn, v)
    q_dec = q * jnp.exp((pos + 1.0) * log_gamma[:, None])[None, :, None, :, None]
    k_dec = k * jnp.exp((CHUNK - 1.0 - pos) * log_gamma[:, None])[None, :, None, :, None]
    chunk_decay = jnp.exp(CHUNK * log_gamma)[None, :, None, None]

    def step(state, inp):
        q_c, k_c, v_c = inp
        o_c = jnp.einsum("bhck,bhkv->bhcv", q_c, state)
        state = state * chunk_decay + jnp.einsum("bhck,bhcv->bhkv", k_c, v_c)
        return state, o_c

    xs = tuple(jnp.moveaxis(t, 2, 0) for t in (q_dec, k_dec, v))
    s0 = jnp.zeros((b, B_HEADS, B_DK, B_DV), jnp.float32)
    _, cross = lax.scan(step, s0, xs)
    o = from_chunks(cross) + from_chunks(jnp.moveaxis(inner, 2, 0))
    o = rms_norm(o, norm_w.reshape(B_HEADS, B_DV)) * jax.nn.silu(gate.reshape(b, s, B_HEADS, B_DV))
    return o.reshape(b, s, B_V)


def hierarchical_moe(h, w_router_group, w_router_expert, w_gate, w_up, w_down):
    b, s, d = h.shape
    ht = h.reshape(b * s, d)
    hf = ht.astype(jnp.float32)
    g_logits = hf @ w_router_group.astype(jnp.float32)
    g_prob = jax.nn.softmax(g_logits, axis=-1)
    g_idx = jnp.argmax(g_logits, axis=-1)
    g_w = jnp.take_along_axis(g_prob, g_idx[:, None], axis=-1)[:, 0]
    e_logits = (hf @ w_router_expert.astype(jnp.float32)).reshape(-1, N_GROUPS, EXPERTS_PER_GROUP)
    e_logits = jnp.take_along_axis(e_logits, g_idx[:, None, None], axis=1)[:, 0]
    e_prob = jax.nn.softmax(e_logits, axis=-1)
    top_p, top_i = lax.top_k(e_prob, TOP_K)
    top_p = top_p / jnp.sum(top_p, axis=-1, keepdims=True)
    weights = (g_w[:, None] * top_p).reshape(-1)
    expert_id = (g_idx[:, None] * EXPERTS_PER_GROUP + top_i).reshape(-1).astype(jnp.int32)
    order = jnp.argsort(expert_id)
    tok = order // TOP_K
    xs = ht[tok]
    group_sizes = jnp.zeros((N_EXPERTS,), jnp.int32).at[expert_id].add(1)
    gate = lax.ragged_dot(xs, w_gate, group_sizes)
    up = lax.ragged_dot(xs, w_up, group_sizes)
    y = lax.ragged_dot((jax.nn.silu(gate) * up).astype(xs.dtype), w_down, group_sizes)
    y = y * weights[order].astype(y.dtype)[:, None]
    out = jnp.zeros_like(ht).at[tok].add(y)
    return out.reshape(b, s, d)


def setup_inputs(seed: int = 0) -> dict:
    key = jax.random.key(seed)
    ks = jax.random.split(key, 20)
    f32 = jnp.float32

    def nrm(k, shape, scale):
        return jax.random.normal(k, shape, f32) * scale

    def gain(k, shape):
        return 1.0 + 0.02 * jax.random.normal(k, shape, f32)

    x = nrm(ks[0], (BATCH, SEQ, D_MODEL), 1.0)
    attn_norm = gain(ks[1], (DEPTH, D_MODEL))
    w_in = nrm(ks[2], (DEPTH, D_MODEL, D_IN), D_MODEL ** -0.5)
    conv_a = nrm(ks[3], (DEPTH, CONV_K, A_CONV), CONV_K ** -0.5)
    a_log = jnp.log(jax.random.uniform(ks[4], (DEPTH, A_HEADS), f32, 1.0, 16.0))
    dt = jnp.exp(jax.random.uniform(ks[5], (DEPTH, A_HEADS), f32, math.log(1e-3), math.log(1e-1)))
    dt_bias = dt + jnp.log(-jnp.expm1(-dt))
    norm_a = gain(ks[6], (DEPTH, A_DV))
    norm_b = gain(ks[7], (DEPTH, B_V))
    w_out = nrm(ks[8], (DEPTH, D_MIX, D_MODEL), D_MIX ** -0.5)
    ffn_norm = gain(ks[9], (DEPTH, D_MODEL))
    w_router_group = nrm(ks[10], (DEPTH, D_MODEL, N_GROUPS), D_MODEL ** -0.5)
    w_router_expert = nrm(ks[11], (DEPTH, D_MODEL, N_EXPERTS), D_MODEL ** -0.5)
    w_gate = nrm(ks[12], (DEPTH, N_EXPERTS, D_MODEL, D_EXPERT), D_MODEL ** -0.5)
    w_up = nrm(ks[13], (DEPTH, N_EXPERTS, D_MODEL, D_EXPERT), D_MODEL ** -0.5)
    w_down = nrm(ks[14], (DEPTH, N_EXPERTS, D_EXPERT, D_MODEL), D_EXPERT ** -0.5)
    final_norm = gain(ks[15], (D_MODEL,))
    return {"x": x, "attn_norm": attn_norm, "w_in": w_in, "conv_a": conv_a,
            "a_log": a_log, "dt_bias": dt_bias, "norm_a": norm_a, "norm_b": norm_b,
            "w_out": w_out, "ffn_norm": ffn_norm, "w_router_group": w_router_group,
            "w_router_expert": w_router_expert, "w_gate": w_gate, "w_up": w_up,
            "w_down": w_down, "final_norm": final_norm}


def reference(x, attn_norm, w_in, conv_a, a_log, dt_bias, norm_a, norm_b, w_out, ffn_norm,
              w_router_group, w_router_expert, w_gate, w_up, w_down, final_norm):
    s = x.shape[1]
    pos = jnp.arange(s, dtype=jnp.float32)
    inv_freq = ROPE_BASE ** (-jnp.arange(0, B_DK, 2, dtype=jnp.float32) / B_DK)
    ang = pos[:, None] * inv_freq[None, :]
    cos, sin = jnp.cos(ang), jnp.sin(ang)
    offsets = [int(o) for o in np.cumsum(IN_SIZES)[:-1]]
    for l in range(DEPTH):
        h = rms_norm(x, attn_norm[l])
        proj = jnp.einsum("bsd,de->bse", h, w_in[l]).astype(jnp.float32)
        q_a, k_a, v_a, g_a, beta_a, alpha_a, q_b, k_b, v_b, g_b = jnp.split(proj, offsets, axis=-1)
        o_a = gated_delta_net(jnp.concatenate([q_a, k_a, v_a], axis=-1), g_a, beta_a, alpha_a,
                              conv_a[l], a_log[l], dt_bias[l], norm_a[l])
        o_b = retention(q_b, k_b, v_b, g_b, cos, sin, norm_b[l])
        mixed = jnp.concatenate([o_a, o_b], axis=-1).astype(x.dtype)
        x = x + jnp.einsum("bse,ed->bsd", mixed, w_out[l])
        h2 = rms_norm(x, ffn_norm[l])
        x = x + hierarchical_moe(h2, w_router_group[l], w_router_expert[l],
                                 w_gate[l], w_up[l], w_down[l])
    return rms_norm(x, final_norm)
```

```python
import os
import numpy as np
from contextlib import ExitStack
import concourse.bass as bass
import concourse.mybir as mybir
from concourse.bass_utils import run_bass_kernel_spmd

F32 = mybir.dt.float32
BF16 = mybir.dt.bfloat16
I32 = mybir.dt.int32
AF = mybir.ActivationFunctionType
OP = mybir.AluOpType
AX = mybir.AxisListType

S = 4096
D = 1024
NT = 32
NB = 8
TPB = 4
BLK = 512
NE = 64
CAP = 256
DIN = 4104
EPS = 1e-6
NCORES = 8
GAM = [1.0 - 2.0 ** (-5.0 - h) for h in range(4)]

_cfo = {}
_o = 0
for _n, _w in (("ident", 128), ("ub", 128), ("slb", 128), ("bb", 128), ("ones", 128), ("negm4", 512),
               ("strn4", 128), ("e4", 512), ("su", 128), ("pm", 128), ("cm", 2),
               ("kdsc", 4), ("osc", 4), ("iotacap", 64)):
    _cfo[_n] = (_o, _o + _w)
    _o += _w
NCF = _o
NCOL = 72
NROW = 8


import types


def _freeze(fn):
    if fn.__closure__ is None:
        return fn
    cells = []
    for c in fn.__closure__:
        try:
            cells.append(types.CellType(c.cell_contents))
        except ValueError:
            cells.append(c)
    return types.FunctionType(fn.__code__, fn.__globals__, fn.__name__, fn.__defaults__, tuple(cells))


COST = {"pe": 0.12, "act": 0.7, "dve": 0.6, "pool": 1.6, "sp": 0.05}
LAT = 0.25
DMA_LAT = 2.5


class Eng:
    def __init__(s, name, sem):
        s.name = name; s.sem = sem; s.cnt = 0; s.waited = {}; s.prog = []; s.tfree = 0.0


class DSem:
    def __init__(s, sem):
        s.sem = sem; s.val = 0


class Buf:
    def __init__(s, name="", excl=False):
        s.name = name; s.w = None; s.r = {}; s.excl = excl
        s.tw = 0.0; s.tr = 0.0


class TB:
    def __init__(s, t, name=""):
        s.t = t; s.b = Buf(name)


class FW:
    def __init__(s, nc, stack):
        s.nc = nc
        s.stack = stack
        s.E = {}
        for n in ("pe", "act", "dve", "pool", "sp"):
            s.E[n] = Eng(n, stack.enter_context(nc.semaphore("sem_" + n)))
        s.dsems = []

    def dsem(s):
        d = DSem(s.stack.enter_context(s.nc.semaphore("dsem%d" % len(s.dsems))))
        s.dsems.append(d)
        return d

    def est_start(s, en, reads, writes):
        reads = [b.b if isinstance(b, TB) else b for b in reads]
        writes = [b.b if isinstance(b, TB) else b for b in writes]
        t = s.E[en].tfree
        for b in reads:
            t = max(t, (b.tw + LAT) if not b.excl else (max(b.tw, b.tr) + LAT))
        for b in writes:
            t = max(t, max(b.tw, b.tr) + LAT)
        return t

    def emit(s, en, fn, reads=(), writes=(), dma=None, cost=None, frozen=False):
        eng = s.E[en]
        deps = []
        reads = [b.b if isinstance(b, TB) else b for b in reads]
        writes = [b.b if isinstance(b, TB) else b for b in writes]
        t0 = s.est_start(en, reads, writes)
        c = COST[en] if cost is None else cost
        eng.tfree = t0 + c
        tfin = t0 + c + (DMA_LAT if dma is not None else 0.0)
        for b in reads:
            if b.excl:
                b.tw = max(b.tw, tfin)
            else:
                b.tr = max(b.tr, tfin)
        for b in writes:
            b.tw = max(b.tw, tfin); b.tr = 0.0
        writes = writes + [b for b in reads if b.excl]
        reads = [b for b in reads if not b.excl]
        for b in reads:
            if b.w is not None:
                deps.append(b.w)
        for b in writes:
            if b.w is not None:
                deps.append(b.w)
            deps.extend(b.r.values())
        waits = []
        for (sem, val) in deps:
            if sem is eng.sem and en in ("pe", "sp"):
                continue
            k = id(sem)
            if eng.waited.get(k, 0) >= val:
                continue
            eng.waited[k] = val
            waits.append((sem, val))
        if dma is None:
            eng.cnt += 1
            tok = (eng.sem, eng.cnt)
            inc = 1
        else:
            dma.val += 16
            tok = (dma.sem, dma.val)
            inc = 16
        eng.prog.append((waits, fn if frozen else _freeze(fn), tok[0], inc))
        for b in reads:
            if isinstance(b, TB):
                b = b.b
            b.r[id(tok[0])] = tok
        for b in writes:
            if isinstance(b, TB):
                b = b.b
            b.w = tok
            b.r = {}
        return tok

    def barrier(s):
        toks = [(e.sem, e.cnt) for e in s.E.values() if e.cnt > 0]
        toks += [(d.sem, d.val) for d in s.dsems if d.val > 0]
        for en, eng in s.E.items():
            waits = []
            for (sem, val) in toks:
                if sem is eng.sem:
                    continue
                if eng.waited.get(id(sem), 0) >= val:
                    continue
                eng.waited[id(sem)] = val
                waits.append((sem, val))
            if waits:
                eng.cnt += 1
                eng.prog.append((waits, (lambda e: e.nop()), eng.sem, 1))

    def finish(s):
        nc = s.nc
        finals = [(d.sem, d.val) for d in s.dsems if d.val > 0]
        with nc.Block() as block:
            def run(eng, e):
                for (waits, fn, sem, inc) in eng.prog:
                    for (ws, wv) in waits:
                        e.wait_ge(ws, wv)
                    fn(e).then_inc(sem, inc)

            @block.tensor
            def _(e):
                run(s.E["pe"], e)

            @block.scalar
            def _(e):
                run(s.E["act"], e)

            @block.vector
            def _(e):
                run(s.E["dve"], e)

            @block.gpsimd
            def _(e):
                run(s.E["pool"], e)

            @block.sync
            def _(e):
                run(s.E["sp"], e)
                for (ws, wv) in finals:
                    e.wait_ge(ws, wv)


def build(debug=False):
    nc = bass.Bass("TRN2", target_bir_lowering=False)

    def dram(name, shape, ty, kind="ExternalInput"):
        return nc.dram_tensor(name, shape, ty, kind=kind).ap()

    x = dram("x", [S, D], F32)
    w_in = dram("w_in", [128, 8, DIN], F32)
    w_out = dram("w_out", [128, 8, D], F32)
    w_r = dram("w_r", [128, 8 * 72], F32)
    wgu = dram("wgu", [NE, 128, 4096], F32)
    wd = dram("wd", [NE, 128, 2048], F32)
    cols_d = dram("cols", [128, NCOL], F32)
    rows_d = dram("rows", [1, NROW], F32)
    fin_d = dram("fin", [1, D], F32)
    cf_d = dram("cf", [128, NCF], F32)
    rope_d = dram("rope", [128, 2, S], F32)
    out = dram("out", [S, D], F32, "ExternalOutput")
    xs_all = dram("xs_all", [NE * CAP, D], BF16, "Internal")
    y_all = dram("y_all", [NE * CAP, D], BF16, "Internal")
    x1_scr = dram("x1_scr", [S, D], F32, "ExternalOutput" if debug else "Internal")
    wgu_bf = dram("wgu_bf", [NE, 128, 4096], BF16, "Internal")
    wd_bf = dram("wd_bf", [NE, 128, 2048], BF16, "Internal")
    dbg_o = dram("dbg_o", [S, D], F32, "ExternalOutput") if debug else None

    with ExitStack() as st:
        fw = FW(nc, st)
        sink = [None]

        def E(en, fn, reads=(), writes=(), dma=None, cost=None):
            if sink[0] is None:
                fw.emit(en, fn, reads, writes, dma, cost)
            else:
                sink[0].append((en, _freeze(fn), list(reads), list(writes), dma, cost))
        dumps = {}

        def dump(name, ap, shape, ty, rbufs):
            if not debug or name in dumps:
                return
            dumps[name] = dram("dd_" + name, list(shape), ty, "ExternalOutput")
            ds_ = fw.dsem()
            E("sp", lambda e: e.dma_start(out=dumps[name], in_=ap), reads=rbufs, dma=ds_)

        def sb(name, shape, ty, stack=st):
            return TB(stack.enter_context(nc.sbuf_tensor(name, shape, ty)), name)

        PB = [TB(st.enter_context(nc.psum_tensor("pb%d" % i, [128, 512], F32)), "pb%d" % i) for i in range(8)]
        for _pb in PB:
            _pb.b.excl = True
        prot = {"mm": [0, 1], "tr": [2], "cv": [3], "dn": [4], "nm": [5], "st": [6], "o": [7], "tr2": [2, 3], "dw": [6, 7], "mm4": [0, 1, 4, 5], "n0": [0], "g": [1, 3], "g2": [6, 7], "dnA": [2], "nmA": [4], "dnB": [0], "nmB": [5], "rmm": [1], "rcv": [3]}
        pidx = {k: 0 for k in prot}

        def bank(role):
            l = prot[role]
            i = l[pidx[role] % len(l)]
            pidx[role] += 1
            return PB[i]

        def bfv(pb):
            return pb.t[:, :].bitcast(BF16)

        def v3(ap, a):
            return ap.rearrange("p (a b) -> p a b", a=a)

        def bc3(ap2, a, b):
            return ap2.unsqueeze(2).to_broadcast([ap2.shape[0], a, b])

        def bcm(ap2, a, b):
            return ap2.unsqueeze(1).to_broadcast([ap2.shape[0], a, b])

        CF = sb("CF", [128, NCF], F32)
        COLS = sb("COLS", [128, NCOL], F32)
        ROWS = sb("ROWS", [128, NROW], F32)
        CBF = sb("CBF", [128, 512], BF16)
        W_R = sb("W_R", [128, 8 * 72], F32)
        NEGA = sb("NEGA", [128, 4], F32)
        CW = sb("CW", [128, NT * 2], F32)
        OFFS = sb("OFFS", [128, NT * 2], I32)
        BASECAP = sb("BASECAP", [128, NE], F32)

        def cf(name, lo=None, hi=None):
            a, b = _cfo[name]
            return CF.t[:, a:b]

        IDF = cf("ident")
        IDB = CBF.t[:, 0:128]
        SUB = CBF.t[:, 128:256]
        PMB = CBF.t[:, 256:384]
        ONB = CBF.t[:, 384:512]

        dq = [fw.dsem() for _ in range(4)]
        E("sp", lambda e: e.dma_start(out=CF.t[:, :], in_=cf_d), writes=[CF], dma=dq[0])
        E("sp", lambda e: e.dma_start(out=COLS.t[:, :], in_=cols_d), writes=[COLS], dma=dq[1])
        E("sp", lambda e: e.dma_start(out=ROWS.t[:, :], in_=bass.AP(tensor=rows_d.tensor, offset=0, ap=[[0, 128], [1, NROW]])),
          writes=[ROWS], dma=dq[2])
        E("sp", lambda e: e.dma_start(out=W_R.t[:, :], in_=w_r), writes=[W_R], dma=dq[3])
        for i, nm in enumerate(("ident", "su", "pm", "ones")):
            E("dve", lambda e, i=i, nm=nm: e.tensor_copy(out=CBF.t[:, i * 128:(i + 1) * 128], in_=cf(nm)),
              reads=[CF], writes=[CBF])
        for kc in range(8):
            E("dve", lambda e, kc=kc: e.tensor_scalar(out=W_R.t[:, kc * 72:(kc + 1) * 72], in0=W_R.t[:, kc * 72:(kc + 1) * 72],
                                                     scalar1=COLS.t[:, 56 + kc:57 + kc], scalar2=None, op0=OP.mult),
              reads=[W_R, COLS], writes=[W_R])
        E("act", lambda e: e.activation(out=NEGA.t[:, :], in_=ROWS.t[:, 0:4], func=AF.Exp), reads=[ROWS], writes=[NEGA])
        E("dve", lambda e: e.tensor_scalar(out=NEGA.t[:, :], in0=NEGA.t[:, :], scalar1=-1.0, scalar2=None, op0=OP.mult),
          reads=[NEGA], writes=[NEGA])
        E("dve", lambda e: e.tensor_copy(out=BASECAP.t[:, :], in_=cf("iotacap")), reads=[CF], writes=[BASECAP])

        ZT = sb("ZT", [128, 1024], BF16)
        E("pool", lambda e: e.memset(ZT.t[:, :], 0.0), writes=[ZT])
        XSB = Buf("xs_all")
        zsem = fw.dsem()
        zsem0 = fw.dsem()
        xs_c = xs_all.rearrange("(c q r) d -> c q (r d)", c=16, q=16)
        xs_v = xs_all.rearrange("(e p r) d -> e p (r d)", e=2 * NE, p=128)

        with ExitStack() as p1:
            def sb1(name, shape, ty):
                return sb(name, shape, ty, p1)

            WIN = sb1("WIN", [128, 8, DIN], BF16)
            WOUT = sb1("WOUT", [128, 8, D], BF16)
            CDT = [sb1("CDT%d" % i, [128, 4, 128], BF16) for i in range(2)]
            wsem = [fw.dsem()]
            for kc in range(8):
                E("pool", lambda e, kc=kc: e.dma_start(out=WIN.t[:, kc, :], in_=w_in[:, kc, :]), writes=[WIN], dma=wsem[0])

            def wg(c0):
                return WIN

            pcs = fw.dsem()
            def precast(ex):
                E("pool", lambda e, ex=ex: e.dma_start(out=wgu_bf[ex], in_=wgu[ex]), dma=pcs)
                E("pool", lambda e, ex=ex: e.dma_start(out=wd_bf[ex], in_=wd[ex]), dma=pcs)
            XT = [sb1("XT%d" % i, [128, D], F32) for i in range(2)]
            xsem = [fw.dsem() for _ in range(2)]
            xrsem = fw.dsem()
            SS = sb1("SS", [128, 8], F32)
            SSG = sb1("SSG", [128, 8], F32)
            SS4 = [sb1("SSx%d" % i, [128, 2], F32) for i in range(TPB)]
            HT = sb1("HT", [128, 8, BLK], BF16)
            PRE = [sb1("PRE%d" % i, [128, 3 + BLK], BF16) for i in range(2)]
            HIST = sb1("HIST", [128, 12, 3], BF16)
            VT = sb1("VT", [128, 4, BLK], BF16)
            SQ = sb1("SQ", [128, BLK], BF16)
            QTA = sb1("QTA", [128, 4, BLK], BF16)
            KTA = sb1("KTA", [128, 4, BLK], BF16)
            QTB = sb1("QTB", [128, 4, BLK], BF16)
            KTB = sb1("KTB", [128, 4, BLK], BF16)
            rsem = fw.dsem()
            SG = [sb1("SG%d" % i, [128, D], BF16) for i in range(TPB)]
            VB = [sb1("VB%d" % i, [128, 512], BF16) for i in range(2)] * 2
            VA2 = [sb1("VA%d" % i, [128, 512], BF16) for i in range(2)]
            KDEC2 = [sb1("KDEC%d" % i, [128, 512], BF16) for i in range(2)]
            KDECB = [sb1("KDECB%d" % i, [128, 512], BF16) for i in range(2)] * 2
            YF3 = [sb1("YF%d" % i, [128, 512], BF16) for i in range(3)]
            BA = sb1("BA", [128, TPB, 8], F32)
            BETA = sb1("BETA", [128, 16], F32)
            GG = sb1("GG", [128, 16], F32)
            GM = sb1("GM", [128, TPB, 8], F32)
            EGC = sb1("EGC", [128, 16], F32)
            NEGC = sb1("NEGC", [128, 16], F32)
            EDEC = sb1("EDEC", [128, 16], F32)
            LASTB = sb1("LASTB", [128, TPB * 8], F32)
            TMPS = sb1("TMPS", [128, 16], F32)
            RR = sb1("RR", [128, 512], F32)
            DECT = sb1("DECT", [128, 512], F32)
            WW = sb1("WW", [128, 512], F32)
            TT = [sb1("TT%d" % i, [128, 512], BF16) for i in range(2)]
            NN = [sb1("NN%d" % i, [128, 512], BF16) for i in range(2)]
            YY = [sb1("YY%d" % i, [128, 512], BF16) for i in range(2)]
            ATT2 = [sb1("ATT%d" % i, [128, 512], BF16) for i in range(2)]
            TMPZ = sb1("TMPZ", [128, 512], F32)
            ZZ = sb1("ZZ", [128, 512], BF16)
            VN = sb1("VN", [128, 512], BF16)
            SA = sb1("SA", [128, 512], F32)
            SAB = sb1("SAB", [128, 512], BF16)
            SBS = sb1("SBS", [128, 512], F32)
            SBB = sb1("SBB", [128, 512], BF16)
            ATB = sb1("ATB", [128, 512], BF16)
            OALLL = [sb1("OALL%d" % i, [128, D], F32) for i in range(2)] * 2
            RSTD8 = sb1("RSTD8", [128, 8], F32)
            MIX = sb1("MIX", [128, D], BF16)
            MIXT = sb1("MIXT", [128, 8, 128], BF16)
            X1 = sb1("X1", [128, D], F32)
            x1sem = fw.dsem()
            H2B = sb1("H2B", [128, D], BF16)
            H2T = sb1("H2T", [128, 8, 128], F32)
            OSQ = TB.__new__(TB); OSQ.b = H2T.b; OSQ.t = H2T.t[:, :, :].rearrange("p a b -> p (a b)")
            HBF = sb1("HBF1", [128, D], BF16)
            RINV = DECT
            RA = TB.__new__(TB); RA.b = X1.b; RA.t = X1.t[:, 0:512]
            RB = TMPZ
            SQ_B = TB.__new__(TB); SQ_B.b = PRE[0].b; SQ_B.t = PRE[0].t[:, 0:512]
            RINV_B = WW
            JUNKS = [TB.__new__(TB), TB.__new__(TB)]
            JUNKS[0].b = QTB.b; JUNKS[0].t = QTB.t[:, 0:2, :].rearrange("p a b -> p (a b)")
            JUNKS[1].b = KTB.b; JUNKS[1].t = KTB.t[:, 0:2, :].rearrange("p a b -> p (a b)")

            def alias(base, ap):
                a_ = TB.__new__(TB); a_.b = base.b; a_.t = ap
                return a_
            xt1bf = XT[1].t[:, 512:1024].bitcast(BF16)
            hbfv = HBF.t[:, :]
            NSETS = [
                (RR, DECT, WW, TT, NN, YY),
                (alias(XT[0], XT[0].t[:, 0:512]), alias(XT[0], XT[0].t[:, 512:1024]), alias(XT[1], XT[1].t[:, 0:512]),
                 [alias(XT[1], xt1bf[:, 0:512]), alias(XT[1], xt1bf[:, 512:1024])],
                 [alias(HBF, hbfv[:, 0:512]), alias(HBF, hbfv[:, 512:1024])],
                 [alias(PRE[0], PRE[0].t[:, 0:512]), alias(PRE[1], PRE[1].t[:, 0:512])]),
            ]
            NBANKS = [("dnA", "nmA"), ("dnB", "nmB")]
            s3 = lambda l2, third: [l2[0], l2[1], third, l2[0]]
            KDEC = s3(KDEC2, alias(CDT[0], CDT[0].t[:, :, :].rearrange("p a b -> p (a b)")))
            VA = s3(VA2, alias(CDT[1], CDT[1].t[:, :, :].rearrange("p a b -> p (a b)")))
            ATTL = s3(ATT2, alias(SQ, SQ.t[:, :]))
            YFL = [YF3[0], YF3[1], YF3[2], YF3[0]]
            TBF = SQ

            ROPE = TB.__new__(TB); ROPE.b = H2T.b; ROPE.t = H2T.t[:, :, :].rearrange("p (a c) b -> p a (c b)", a=2)
            wss = [fw.dsem() for _ in range(2)]
            for kc in range(8):
                stg = (X1, OSQ)[kc % 2]
                E("sp", lambda e, kc=kc, stg=stg: e.dma_start(out=stg.t[:, :], in_=w_out[:, kc, :]), writes=[stg], dma=wss[kc % 2])
                E("dve", lambda e, kc=kc, stg=stg: e.tensor_scalar(out=WOUT.t[:, kc, :], in0=stg.t[:, :], scalar1=COLS.t[:, 48 + kc:49 + kc],
                                                                   scalar2=None, op0=OP.mult),
                  reads=[stg, COLS], writes=[WOUT])
            LG = sb1("LG", [128, 72], F32)
            RT = sb1("RT", [128, 16], F32)
            GMASK = sb1("GMASK", [128, 8], F32)
            PEN = sb1("PEN", [128, 8], F32)
            EL = sb1("EL", [128, 64], F32)
            EL2 = sb1("EL2", [128, 64], F32)
            OH1 = sb1("OH1", [128, 64], F32)
            OH2 = sb1("OH2", [128, 64], F32)
            OH12 = sb1("OH12", [128, 64], BF16)
            POSM = sb1("POSM", [128, 64], F32)
            PRD = sb1("PRD", [128, 64], F32)
            OFF_F = sb1("OFF_F", [128, 2], F32)
            scsem = fw.dsem()
            dbgsem = fw.dsem() if debug else None

            for z in (SA, SBS):
                E("pool", lambda e, z=z: e.memset(z.t[:, :], 0.0), writes=[z])
            for z in (SAB, SBB):
                E("pool", lambda e, z=z: e.memset(z.t[:, :], 0.0), writes=[z])
            E("pool", lambda e: e.memset(HIST.t[:, :, :], 0.0), writes=[HIST])

            def rstd_from_ss(ss_ap, out_ap, n, rbufs, wbufs):
                E("dve", lambda e: e.tensor_scalar(out=out_ap, in0=ss_ap, scalar1=1.0 / n, scalar2=EPS, op0=OP.mult, op1=OP.add),
                  reads=rbufs, writes=wbufs)
                E("act", lambda e: e.activation(out=out_ap, in_=out_ap, func=AF.Ln), reads=wbufs, writes=wbufs)
                E("act", lambda e: e.activation(out=out_ap, in_=out_ap, func=AF.Exp, scale=-0.5), reads=wbufs, writes=wbufs)

            pendG = [None]

            from collections import deque

            def run_rr(gens, stop_on_first=False, stop_on=(), must_finish=(), after=None):
                gens = [g for g in gens if g is not None]
                for g in gens:
                    if g not in pend:
                        pend[g] = deque()
                alive = {g: True for g in gens}

                def fill(g):
                    q = pend[g]
                    while not q and alive[g]:
                        sink[0] = q
                        try:
                            next(g)
                        except StopIteration:
                            alive[g] = False
                        sink[0] = None
                    return bool(q)

                watch = [gens[0]] if stop_on_first else [gens[i] for i in stop_on]
                need = [gens[i] for i in must_finish]
                active = list(gens)
                while True:
                    best = None
                    for pi, g in enumerate(active):
                        if after and g in after and any(alive[x] or pend[x] for x in after[g]):
                            continue
                        if fill(g):
                            op = pend[g][0]
                            t = fw.est_start(op[0], op[2], op[3])
                            if best is None or t < best[0] - 1e-9:
                                best = (t, pi, g)
                    if best is None:
                        break
                    g = best[2]
                    en, fn, rd, wr, dma, cost = pend[g].popleft()
                    fw.emit(en, fn, rd, wr, dma, cost, frozen=True)
                    if watch and not any(alive[w] or pend[w] for w in watch):
                        active = [g2 for g2 in need if alive[g2] or pend[g2]]
                        watch = []
                        if not active:
                            break
                        stopping = True
                for g in gens:
                    if not alive[g] and not pend[g]:
                        pend.pop(g, None)

            pend = {}

            def ldx_blk(bb, tl):
                t = bb * TPB + tl
                xt = XT[tl % 2]
                E("sp", lambda e, t=t, xt=xt: e.dma_start(out=xt.t[:, :], in_=x[t * 128:(t + 1) * 128, :]), writes=[xt], dma=xsem[tl % 2])

            for b in range(NB):
                def thrP1():
                    def ldx(tl):
                        ldx_blk(b, tl)
                    if b == 0:
                        ldx(0)
                    for tl in range(TPB):
                        if tl + 1 < TPB and not (b > 0 and tl == 0):
                            ldx(tl + 1)
                        xt = XT[tl % 2]
                        ss = SS4[tl]
                        jk = JUNKS[tl % 2]
                        E("pool", lambda e, ss=ss: e.memset(ss.t[:, 0:1], 0.0), writes=[ss], cost=0.2)
                        yield
                        E("act", lambda e, xt=xt, ss=ss, jk=jk: e.activation(out=jk.t, in_=xt.t[:, :], func=AF.Square, accum_out=ss.t[:, 0:1]),
                          reads=[xt, ss], writes=[jk, ss], cost=1.1)
                        yield
                        rstd_from_ss(ss.t[:, 0:1], ss.t[:, 1:2], D, [ss], [ss])
                        E("dve", lambda e, xt=xt, ss=ss: e.tensor_scalar(out=HBF.t[:, :], in0=xt.t[:, :], scalar1=ss.t[:, 1:2], scalar2=None, op0=OP.mult),
                          reads=[xt, ss], writes=[HBF], cost=1.1)
                        yield
                        pt = bank("tr")
                        for kc in range(8):
                            E("pe", lambda e, kc=kc, pt=pt: e.transpose(out=bfv(pt)[:, kc * 128:(kc + 1) * 128], in_=HBF.t[:, kc * 128:(kc + 1) * 128],
                                                                        identity=IDB),
                              reads=[HBF, CBF], writes=[pt])
                            yield
                        E("dve", lambda e, pt=pt, tl=tl: e.tensor_tensor(out=HT.t[:, :, tl * 128:(tl + 1) * 128], in0=v3(bfv(pt)[:, :], 8),
                                                                         in1=bc3(COLS.t[:, 64:72], 8, 128), op=OP.mult),
                          reads=[pt, COLS], writes=[HT])
                        yield

                    if b == 0:
                        XS0 = Buf("xs_chunk0")
                        for ex in range(8):
                            E("sp", lambda e, ex=ex: e.dma_start(out=xs_v[ex], in_=ZT.t[:, :]), reads=[ZT], writes=[XS0], dma=zsem0)
                            yield
                        for k in range(1, 16):
                            E("sp", lambda e, k=k: e.dma_start(out=xs_c[k], in_=xs_c[0]), reads=[XS0], writes=[XSB], dma=zsem)
                            yield
                        dump("ht", HT.t[:, :, :], [128, 8, BLK], BF16, [HT])
                    def proj_fm(c0):
                        pm = bank("mm")
                        for kc in range(8):
                            E("pe", lambda e, kc=kc, pm=pm: e.matmul(pm.t[:, :], lhsT=WIN.t[:, kc, c0:c0 + 128], rhs=HT.t[:, kc, :],
                                                                     start=(kc == 0), stop=(kc == 7)),
                              reads=[wg(c0), HT], writes=[pm])
                        return pm

                    for c in range(12):
                        pm = proj_fm(c * 128)
                        pre = PRE[c % 2]
                        E("pool", lambda e, c=c, pre=pre: e.tensor_copy(out=pre.t[:, 0:3], in_=HIST.t[:, c, :]), reads=[HIST], writes=[pre])
                        yield
                        E("act", lambda e, pm=pm, pre=pre: e.activation(out=pre.t[:, 3:3 + BLK], in_=pm.t[:, :], func=AF.Copy), reads=[pm], writes=[pre])
                        yield
                        E("pool", lambda e, c=c, pre=pre: e.tensor_copy(out=HIST.t[:, c, :], in_=pre.t[:, BLK:BLK + 3]), reads=[pre], writes=[HIST])
                        yield
                        pc = bank("cv")
                        cd = CDT[c % 2]
                        for j in range(4):
                            E("dve", lambda e, j=j, c=c, cd=cd: e.tensor_scalar(out=cd.t[:, j, :], in0=IDF, scalar1=COLS.t[:, c * 4 + j:c * 4 + j + 1], scalar2=None, op0=OP.mult),
                              reads=[CF, COLS], writes=[cd])
                            yield
                        for j in range(4):
                            E("pe", lambda e, j=j, c=c, pc=pc, pre=pre, cd=cd: e.matmul(pc.t[:, :], lhsT=cd.t[:, j, :], rhs=pre.t[:, j:j + BLK],
                                                                                       start=(j == 0), stop=(j == 3)),
                              reads=[cd, pre], writes=[pc])
                            yield
                        if c < 8:
                            sd = QTA if c < 4 else KTA
                            E("act", lambda e, c=c, pc=pc, sd=sd: e.activation(out=sd.t[:, c % 4, :], in_=pc.t[:, :], func=AF.Silu), reads=[pc], writes=[sd])
                            yield
                        else:
                            E("act", lambda e, c=c, pc=pc: e.activation(out=VT.t[:, c - 8, :], in_=pc.t[:, :], func=AF.Silu), reads=[pc], writes=[VT])
                            yield
                    for tl in range(TPB):
                        for (c0, n, kind) in ((1536, 512, "ga"), (3592, 512, "gb"), (2048, 8, "ba")):
                            pm = bank("mm")
                            for kc in range(8):
                                E("pe", lambda e, kc=kc, pm=pm, c0=c0, n=n, tl=tl: e.matmul(pm.t[:, 0:n], lhsT=HT.t[:, kc, tl * 128:(tl + 1) * 128],
                                                                                           rhs=WIN.t[:, kc, c0:c0 + n], start=(kc == 0), stop=(kc == 7)),
                                  reads=[wg(c0), HT], writes=[pm])
                                yield
                            if kind == "ga":
                                E("act", lambda e, pm=pm, tl=tl: e.activation(out=SG[tl].t[:, 0:512], in_=pm.t[:, :], func=AF.Silu), reads=[pm], writes=[SG[tl]])
                                yield
                            elif kind == "gb":
                                E("act", lambda e, pm=pm, tl=tl: e.activation(out=SG[tl].t[:, 512:1024], in_=pm.t[:, :], func=AF.Silu), reads=[pm], writes=[SG[tl]])
                                yield
                            else:
                                E("dve", lambda e, pm=pm, tl=tl: e.tensor_copy(out=BA.t[:, tl, :], in_=pm.t[:, 0:8]), reads=[pm], writes=[BA])
                                yield

                    yield
                def thrL(par, SQ, RINV, lbank):
                    for c in range(par, 8, 2):
                        dst = QTA if c < 4 else KTA
                        E("pool", lambda e, c=c, dst=dst: e.tensor_tensor(out=SQ.t[:, :], in0=dst.t[:, c % 4, :], in1=dst.t[:, c % 4, :], op=OP.mult),
                          reads=[dst], writes=[SQ])
                        yield
                        pc = bank(lbank)
                        E("pe", lambda e, pc=pc: e.matmul(pc.t[:, :], lhsT=ONB, rhs=SQ.t[:, :], start=True, stop=True), reads=[CBF, SQ], writes=[pc])
                        yield
                        E("act", lambda e, pc=pc: e.activation(out=RINV.t[:, :], in_=pc.t[:, :], func=AF.Ln, bias=EPS), reads=[pc], writes=[RINV])
                        yield
                        lb = float(np.log(128.0 ** -0.5)) if c < 4 else 0.0
                        E("act", lambda e, lb=lb: e.activation(out=RINV.t[:, :], in_=RINV.t[:, :], func=AF.Exp, scale=-0.5, bias=lb),
                          reads=[RINV], writes=[RINV])
                        yield
                        E("dve", lambda e, c=c, dst=dst: e.tensor_tensor(out=dst.t[:, c % 4, :], in0=dst.t[:, c % 4, :], in1=RINV.t[:, :], op=OP.mult),
                          reads=[dst, RINV], writes=[dst])
                        yield
                    yield
                def thrRope():
                    def proj_fm(c0):
                        pm = bank("rmm")
                        for kc in range(8):
                            E("pe", lambda e, kc=kc, pm=pm: e.matmul(pm.t[:, :], lhsT=WIN.t[:, kc, c0:c0 + 128], rhs=HT.t[:, kc, :],
                                                                     start=(kc == 0), stop=(kc == 7)),
                              reads=[wg(c0), HT], writes=[pm])
                        return pm
                    for c in range(8):
                        c0 = 2056 + c * 128
                        pm = proj_fm(c0)
                        E("act", lambda e, pm=pm: e.activation(out=TBF.t[:, :], in_=pm.t[:, :], func=AF.Copy), reads=[pm], writes=[TBF])
                        yield
                        pc = bank("rcv")
                        E("pe", lambda e, pc=pc: e.matmul(pc.t[:, :], lhsT=PMB, rhs=TBF.t[:, :], start=True, stop=True), reads=[CBF, TBF], writes=[pc])
                        yield
                        E("dve", lambda e, pm=pm: e.tensor_tensor(out=RA.t[:, :], in0=pm.t[:, :], in1=ROPE.t[:, 0, :], op=OP.mult),
                          reads=[pm, ROPE], writes=[RA])
                        yield
                        E("dve", lambda e, pc=pc: e.tensor_tensor(out=RB.t[:, :], in0=pc.t[:, :], in1=ROPE.t[:, 1, :], op=OP.mult),
                          reads=[pc, ROPE], writes=[RB])
                        yield
                        dst = QTB if c < 4 else KTB
                        E("pool", lambda e, c=c, dst=dst: e.tensor_tensor(out=dst.t[:, c % 4, :], in0=RA.t[:, :], in1=RB.t[:, :], op=OP.add),
                          reads=[RA, RB], writes=[dst])
                        yield

                    yield
                def thrScal():
                    ba3 = BA.t[:, :, :]
                    b16 = v3(BETA.t[:, :], TPB)
                    g16 = v3(GG.t[:, :], TPB)
                    t16 = v3(TMPS.t[:, :], TPB)
                    E("act", lambda e: e.activation(out=b16, in_=ba3[:, :, 0:4], func=AF.Exp, scale=-1.0), reads=[BA], writes=[BETA])
                    yield
                    E("dve", lambda e: e.tensor_scalar(out=BETA.t[:, :], in0=BETA.t[:, :], scalar1=1.0, scalar2=None, op0=OP.add), reads=[BETA], writes=[BETA])
                    yield
                    E("dve", lambda e: e.reciprocal(out=BETA.t[:, :], in_=BETA.t[:, :]), reads=[BETA], writes=[BETA])
                    yield
                    E("dve", lambda e: e.tensor_tensor(out=g16, in0=ba3[:, :, 4:8], in1=bcm(ROWS.t[:, 4:8], TPB, 4), op=OP.add),
                      reads=[BA, ROWS], writes=[GG])
                    yield
                    E("act", lambda e: e.activation(out=GG.t[:, :], in_=GG.t[:, :], func=AF.Exp), reads=[GG], writes=[GG])
                    yield
                    E("act", lambda e: e.activation(out=GG.t[:, :], in_=GG.t[:, :], func=AF.Ln, bias=1.0), reads=[GG], writes=[GG])
                    yield
                    E("dve", lambda e: e.tensor_tensor(out=g16, in0=g16, in1=bcm(NEGA.t[:, :], TPB, 4), op=OP.mult), reads=[GG, NEGA], writes=[GG])
                    yield
                    ps = bank("st")
                    E("pe", lambda e, ps=ps: e.matmul(ps.t[:, 0:16], lhsT=cf("ub"), rhs=GG.t[:, :], start=True, stop=True), reads=[CF, GG], writes=[ps])
                    yield
                    E("pe", lambda e, ps=ps: e.matmul(ps.t[:, 16:32], lhsT=cf("bb"), rhs=GG.t[:, :], start=True, stop=True), reads=[CF, GG], writes=[ps])
                    yield
                    for cc in range(2):
                        E("dve", lambda e, cc=cc: e.tensor_scalar(out=GM.t[:, :, cc * 4:(cc + 1) * 4], in0=g16, scalar1=cf("cm")[:, cc:cc + 1],
                                                                  scalar2=None, op0=OP.mult),
                          reads=[GG, CF], writes=[GM])
                        yield
                    E("pe", lambda e, ps=ps: e.matmul(ps.t[:, 32:64], lhsT=cf("ones"), rhs=GM.t[:, :, :].rearrange("p a b -> p (a b)"),
                                                      start=True, stop=True), reads=[CF, GM], writes=[ps])
                    yield
                    E("act", lambda e, ps=ps: e.activation(out=EGC.t[:, :], in_=ps.t[:, 0:16], func=AF.Exp), reads=[ps], writes=[EGC])
                    yield
                    E("dve", lambda e: e.tensor_scalar(out=NEGC.t[:, :], in0=EGC.t[:, :], scalar1=-1.0, scalar2=None, op0=OP.mult), reads=[EGC], writes=[NEGC])
                    yield
                    E("act", lambda e, ps=ps: e.activation(out=TMPS.t[:, :], in_=ps.t[:, 16:32], func=AF.Copy), reads=[ps], writes=[TMPS])
                    yield
                    E("dve", lambda e, ps=ps: e.tensor_tensor(out=TMPS.t[:, :], in0=TMPS.t[:, :], in1=ps.t[:, 0:16], op=OP.subtract),
                      reads=[ps, TMPS], writes=[TMPS])
                    yield
                    E("act", lambda e: e.activation(out=EDEC.t[:, :], in_=TMPS.t[:, :], func=AF.Exp), reads=[TMPS], writes=[EDEC])
                    yield
                    E("act", lambda e, ps=ps: e.activation(out=LASTB.t[:, :], in_=ps.t[:, 32:64], func=AF.Exp), reads=[ps], writes=[LASTB])
                    yield

                    yield
                def dumps23():
                    if b == 0:
                        dump("qta", QTA.t[:, :, :], [128, 4, BLK], BF16, [QTA])
                        dump("kta", KTA.t[:, :, :], [128, 4, BLK], BF16, [KTA])
                        dump("vt", VT.t[:, :, :], [128, 4, BLK], BF16, [VT])
                        dump("qtb", QTB.t[:, :, :], [128, 4, BLK], BF16, [QTB])
                        dump("ktb", KTB.t[:, :, :], [128, 4, BLK], BF16, [KTB])
                        dump("sg0", SG[0].t[:, :], [128, D], BF16, [SG[0]])
                        dump("ba", BA.t[:, :, :], [128, TPB, 8], F32, [BA])
                    if b == 0:
                        dump("beta", BETA.t[:, :], [128, 16], F32, [BETA])
                        dump("gg", GG.t[:, :], [128, 16], F32, [GG])
                        dump("egc", EGC.t[:, :], [128, 16], F32, [EGC])
                        dump("edec", EDEC.t[:, :], [128, 16], F32, [EDEC])
                        dump("lastb", LASTB.t[:, :], [128, 32], F32, [LASTB])
                    pass
                def tok_major(tl, trb):
                    cs = slice(tl * 128, (tl + 1) * 128)
                    pt = bank(trb)
                    for h in range(4):
                        E("pe", lambda e, h=h, pt=pt, cs=cs: e.transpose(out=bfv(pt)[:, h * 128:(h + 1) * 128], in_=KTA.t[:, h, cs], identity=IDB),
                          reads=[KTA, CBF], writes=[pt])
                    E("dve", lambda e, pt=pt, tl=tl: e.tensor_tensor(out=v3(KDEC[tl].t[:, :], 4), in0=v3(bfv(pt)[:, 0:512], 4),
                                                                     in1=bc3(EDEC.t[:, tl * 4:(tl + 1) * 4], 4, 128), op=OP.mult),
                      reads=[pt, EDEC], writes=[KDEC[tl]])
                    pt = bank(trb)
                    for h in range(4):
                        E("pe", lambda e, h=h, pt=pt, cs=cs: e.transpose(out=bfv(pt)[:, h * 128:(h + 1) * 128], in_=VT.t[:, h, cs], identity=IDB),
                          reads=[VT, CBF], writes=[pt])
                    E("act", lambda e, pt=pt, tl=tl: e.activation(out=VA[tl].t[:, :], in_=bfv(pt)[:, 0:512], func=AF.Copy), reads=[pt], writes=[VA[tl]])

                def tok_major_s(tl):
                    cs = slice(tl * 128, (tl + 1) * 128)
                    pm = bank("st")
                    for kc in range(8):
                        E("pe", lambda e, kc=kc, pm=pm: e.matmul(pm.t[:, :], lhsT=HT.t[:, kc, cs], rhs=WIN.t[:, kc, 3080:3592], start=(kc == 0), stop=(kc == 7)),
                          reads=[wg(3080), HT], writes=[pm])
                    E("act", lambda e, pm=pm: e.activation(out=VB[tl].t[:, :], in_=pm.t[:, :], func=AF.Copy), reads=[pm], writes=[VB[tl]])
                    pt = bank("o")
                    for h in range(4):
                        E("pe", lambda e, h=h, pt=pt, cs=cs: e.transpose(out=bfv(pt)[:, h * 128:(h + 1) * 128], in_=KTB.t[:, h, cs], identity=IDB),
                          reads=[KTB, CBF], writes=[pt])
                    E("dve", lambda e, pt=pt, tl=tl: e.tensor_tensor(out=v3(KDECB[tl].t[:, :], 4), in0=v3(bfv(pt)[:, 0:512], 4),
                                                                     in1=bc3(cf("kdsc"), 4, 128), op=OP.mult),
                      reads=[pt, CF], writes=[KDECB[tl]])


                def thrN(tl, ns=0):
                    t = b * TPB + tl
                    cs = slice(tl * 128, (tl + 1) * 128)
                    sc = slice(tl * 4, (tl + 1) * 4)
                    hc = [slice(h * 128, (h + 1) * 128) for h in range(4)]
                    RR, DECT, WW, TT, NN, YY = NSETS[ns]
                    dnb, nmb = NBANKS[ns]
                    tok_major(tl, dnb)
                    yield
                    E("pool", lambda e, sc=sc: e.tensor_tensor(out=v3(RR.t[:, :], 4), in0=bcm(cf("ub"), 4, 128), in1=bc3(GG.t[:, sc], 4, 128), op=OP.mult),
                      reads=[CF, GG], writes=[RR])
                    yield
                    pd = bank(dnb)
                    E("pe", lambda e, pd=pd: e.matmul(pd.t[:, :], lhsT=cf("slb"), rhs=RR.t[:, :], start=True, stop=False), reads=[CF, RR], writes=[pd])
                    yield
                    E("pe", lambda e, pd=pd: e.matmul(pd.t[:, :], lhsT=IDF, rhs=cf("negm4"), start=False, stop=True), reads=[CF], writes=[pd])
                    yield
                    E("act", lambda e, pd=pd: e.activation(out=DECT.t[:, :], in_=pd.t[:, :], func=AF.Exp), reads=[pd], writes=[DECT])
                    yield
                    E("pool", lambda e: e.tensor_tensor(out=v3(WW.t[:, :], 4), in0=v3(DECT.t[:, :], 4), in1=bcm(cf("strn4"), 4, 128), op=OP.mult), reads=[DECT, CF], writes=[WW])
                    yield
                    E("pool", lambda e, sc=sc: e.tensor_tensor(out=v3(WW.t[:, :], 4), in0=v3(WW.t[:, :], 4), in1=bc3(BETA.t[:, sc], 4, 128), op=OP.mult),
                      reads=[WW, BETA], writes=[WW])
                    yield
                    pk = bank(dnb)
                    for h in range(4):
                        E("pe", lambda e, h=h, pk=pk: e.matmul(pk.t[:, hc[h]], lhsT=KTA.t[:, h, cs], rhs=KTA.t[:, h, cs], start=True, stop=True),
                          reads=[KTA], writes=[pk])
                        yield
                    E("dve", lambda e, pk=pk: e.tensor_tensor(out=TT[0].t[:, :], in0=pk.t[:, :], in1=WW.t[:, :], op=OP.mult), reads=[pk, WW], writes=[TT[0]])
                    yield
                    pq = bank(dnb)
                    for h in range(4):
                        E("pe", lambda e, h=h, pq=pq: e.matmul(pq.t[:, hc[h]], lhsT=KTA.t[:, h, cs], rhs=QTA.t[:, h, cs], start=True, stop=True),
                          reads=[KTA, QTA], writes=[pq])
                        yield
                    E("dve", lambda e, pq=pq: e.tensor_tensor(out=ATTL[tl].t[:, :], in0=pq.t[:, :], in1=DECT.t[:, :], op=OP.mult), reads=[pq, DECT], writes=[ATTL[tl]])
                    yield
                    pt = bank(dnb)
                    for h in range(4):
                        E("pe", lambda e, h=h, pt=pt: e.transpose(out=bfv(pt)[:, hc[h]], in_=TT[0].t[:, hc[h]], identity=IDB), reads=[TT[0], CBF], writes=[pt])
                        yield
                    E("act", lambda e, pt=pt: e.activation(out=NN[0].t[:, :], in_=bfv(pt)[:, 0:512], func=AF.Copy), reads=[pt], writes=[NN[0]])
                    yield
                    E("pool", lambda e: e.tensor_tensor(out=v3(YY[0].t[:, :], 4), in0=v3(TT[0].t[:, :], 4), in1=bcm(IDF, 4, 128), op=OP.add), reads=[TT[0], CF], writes=[YY[0]])
                    yield
                    for p in range(5):
                        Tp, Np, Tn, Nn = TT[p % 2], NN[p % 2], TT[(p + 1) % 2], NN[(p + 1) % 2]
                        Yp, Yn = YY[p % 2], (YY[(p + 1) % 2] if p < 4 else YFL[tl])
                        pn = bank(nmb)
                        for h in range(4):
                            E("pe", lambda e, h=h, pn=pn, Tp=Tp, Np=Np: e.matmul(pn.t[:, hc[h]], lhsT=Tp.t[:, hc[h]], rhs=Np.t[:, hc[h]], start=True, stop=True),
                              reads=[Tp, Np], writes=[pn])
                            yield
                        if p < 4:
                            pt2 = bank(dnb)
                            for h in range(4):
                                E("pe", lambda e, h=h, pt2=pt2, Tp=Tp, Np=Np: e.matmul(pt2.t[:, hc[h]], lhsT=Np.t[:, hc[h]], rhs=Tp.t[:, hc[h]], start=True, stop=True),
                                  reads=[Tp, Np], writes=[pt2])
                                yield
                        E("act", lambda e, pn=pn, Nn=Nn: e.activation(out=Nn.t[:, :], in_=pn.t[:, :], func=AF.Copy), reads=[pn], writes=[Nn])
                        yield
                        if p < 4:
                            E("dve", lambda e, pt2=pt2, Tn=Tn: e.tensor_copy(out=Tn.t[:, :], in_=pt2.t[:, :]), reads=[pt2], writes=[Tn])
                            yield
                        py = bank(nmb)
                        for h in range(4):
                            E("pe", lambda e, h=h, py=py, Nn=Nn, Yp=Yp: e.matmul(py.t[:, hc[h]], lhsT=Nn.t[:, hc[h]], rhs=Yp.t[:, hc[h]], start=True, stop=True),
                              reads=[Nn, Yp], writes=[py])
                            yield
                        E("dve", lambda e, py=py, Yp=Yp, Yn=Yn: e.tensor_tensor(out=Yn.t[:, :], in0=py.t[:, :], in1=Yp.t[:, :], op=OP.add),
                          reads=[py, Yp], writes=[Yn])
                        yield
                    YF = YFL[tl]

                    if t == 0:
                        dump("dect", DECT.t[:, :], [128, 512], F32, [DECT])
                        dump("att", ATTL[tl].t[:, :], [128, 512], BF16, [ATTL[tl]])
                        dump("yf", YF.t[:, :], [128, 512], BF16, [YF])
                        dump("vb", VB[0].t[:, :], [128, 512], BF16, [VB[0]])
                        dump("va", VA[0].t[:, :], [128, 512], BF16, [VA[0]])
                        dump("kdec", KDEC[0].t[:, :], [128, 512], BF16, [KDEC[0]])
                        dump("kdecb", KDECB[0].t[:, :], [128, 512], BF16, [KDECB[0]])
                    yield
                def thrS(tl):
                    t = b * TPB + tl
                    cs = slice(tl * 128, (tl + 1) * 128)
                    sc = slice(tl * 4, (tl + 1) * 4)
                    hc = [slice(h * 128, (h + 1) * 128) for h in range(4)]
                    precast(2 * t)
                    precast(2 * t + 1)
                    yield
                    tok_major_s(tl)
                    yield
                    for c in range(2):
                        rw = slice(c * 64, (c + 1) * 64)
                        pks = bank("st")
                        for h in range(4):
                            E("pe", lambda e, h=h, pks=pks: e.matmul(pks.t[:, hc[h]], lhsT=KTA.t[:, h, cs], rhs=SAB.t[:, hc[h]], start=True, stop=True),
                              reads=[KTA, SAB], writes=[pks])
                            yield
                        pqs = bank("o")
                        for h in range(4):
                            E("pe", lambda e, h=h, pqs=pqs: e.matmul(pqs.t[:, hc[h]], lhsT=QTA.t[:, h, cs], rhs=SAB.t[:, hc[h]], start=True, stop=True),
                              reads=[QTA, SAB], writes=[pqs])
                            yield
                        E("dve", lambda e, pks=pks, rw=rw, sc=sc: e.tensor_tensor(out=v3(TMPZ.t[rw, :], 4), in0=v3(pks.t[rw, :], 4),
                                                                                  in1=bc3(NEGC.t[rw, sc], 4, 128), op=OP.mult),
                          reads=[pks, NEGC], writes=[TMPZ])
                        yield
                        E("dve", lambda e, rw=rw, tl=tl: e.tensor_tensor(out=ZZ.t[rw, :], in0=TMPZ.t[rw, :], in1=VA[tl].t[rw, :], op=OP.add),
                          reads=[TMPZ, VA[tl]], writes=[ZZ])
                        yield
                        E("dve", lambda e, pqs=pqs, rw=rw, sc=sc: e.tensor_tensor(out=v3(OALLL[tl].t[rw, 0:512], 4), in0=v3(pqs.t[rw, :], 4),
                                                                                  in1=bc3(EGC.t[rw, sc], 4, 128), op=OP.mult),
                          reads=[pqs, EGC], writes=[OALLL[tl]])
                        yield
                        pxz = bank("st")
                        for h in range(4):
                            E("pe", lambda e, h=h, pxz=pxz, rw=rw: e.matmul(pxz.t[:, hc[h]], lhsT=YFL[tl].t[rw, hc[h]], rhs=ZZ.t[rw, hc[h]], start=True, stop=True),
                              reads=[YFL[tl], ZZ], writes=[pxz])
                            yield
                        E("dve", lambda e, pxz=pxz, rw=rw, sc=sc: e.tensor_tensor(out=v3(VN.t[rw, :], 4), in0=v3(pxz.t[rw, :], 4),
                                                                                  in1=bc3(BETA.t[rw, sc], 4, 128), op=OP.mult),
                          reads=[pxz, BETA], writes=[VN])
                        yield
                        pkv = bank("st")
                        for h in range(4):
                            E("pe", lambda e, h=h, pkv=pkv, rw=rw, tl=tl: e.matmul(pkv.t[:, hc[h]], lhsT=KDEC[tl].t[rw, hc[h]], rhs=VN.t[rw, hc[h]],
                                                                                  start=True, stop=True),
                              reads=[KDEC[tl], VN], writes=[pkv])
                            yield
                        for h in range(4):
                            li = tl * 8 + c * 4 + h
                            E("dve", lambda e, h=h, pkv=pkv, li=li: e.scalar_tensor_tensor(out=SA.t[:, hc[h]], in0=SA.t[:, hc[h]], scalar=LASTB.t[:, li:li + 1],
                                                                                           in1=pkv.t[:, hc[h]], op0=OP.mult, op1=OP.add),
                              reads=[SA, LASTB, pkv], writes=[SA])
                            yield
                        E("act", lambda e: e.activation(out=SAB.t[:, :], in_=SA.t[:, :], func=AF.Copy), reads=[SA], writes=[SAB])
                        yield
                    pav = bank("o")
                    for h in range(4):
                        E("pe", lambda e, h=h, pav=pav: e.matmul(pav.t[:, hc[h]], lhsT=ATTL[tl].t[:, hc[h]], rhs=VN.t[:, hc[h]], start=True, stop=True),
                          reads=[ATTL[tl], VN], writes=[pav])
                        yield
                    E("dve", lambda e, pav=pav: e.tensor_tensor(out=OALLL[tl].t[:, 0:512], in0=pav.t[:, :], in1=OALLL[tl].t[:, 0:512], op=OP.add),
                      reads=[pav, OALLL[tl]], writes=[OALLL[tl]])
                    yield

                    pq = bank("st")
                    for h in range(4):
                        E("pe", lambda e, h=h, pq=pq: e.matmul(pq.t[:, hc[h]], lhsT=KTB.t[:, h, cs], rhs=QTB.t[:, h, cs], start=True, stop=True),
                          reads=[KTB, QTB], writes=[pq])
                        yield
                    E("dve", lambda e, pq=pq: e.tensor_tensor(out=ATB.t[:, :], in0=pq.t[:, :], in1=cf("e4"), op=OP.mult), reads=[pq, CF], writes=[ATB])
                    yield
                    pob = bank("o")
                    for h in range(4):
                        E("pe", lambda e, h=h, pob=pob: e.matmul(pob.t[:, hc[h]], lhsT=QTB.t[:, h, cs], rhs=SBB.t[:, hc[h]], start=True, stop=False),
                          reads=[QTB, SBB], writes=[pob])
                        yield
                        E("pe", lambda e, h=h, pob=pob, tl=tl: e.matmul(pob.t[:, hc[h]], lhsT=ATB.t[:, hc[h]], rhs=VB[tl].t[:, hc[h]], start=False, stop=True),
                          reads=[ATB, VB[tl]], writes=[pob])
                        yield
                    E("dve", lambda e, pob=pob: e.tensor_tensor(out=v3(OALLL[tl].t[:, 512:1024], 4), in0=v3(pob.t[:, :], 4), in1=bc3(cf("osc"), 4, 128), op=OP.mult),
                      reads=[pob, CF], writes=[OALLL[tl]])
                    yield
                    psb = bank("st")
                    for h in range(4):
                        E("pe", lambda e, h=h, psb=psb, tl=tl: e.matmul(psb.t[:, hc[h]], lhsT=KDECB[tl].t[:, hc[h]], rhs=VB[tl].t[:, hc[h]], start=True, stop=True),
                          reads=[KDECB[tl], VB[tl]], writes=[psb])
                        yield
                    for h in range(4):
                        E("dve", lambda e, h=h, psb=psb: e.scalar_tensor_tensor(out=SBS.t[:, hc[h]], in0=SBS.t[:, hc[h]], scalar=float(GAM[h] ** 128),
                                                                                in1=psb.t[:, hc[h]], op0=OP.mult, op1=OP.add),
                          reads=[SBS, psb], writes=[SBS])
                        yield
                    E("act", lambda e: e.activation(out=SBB.t[:, :], in_=SBS.t[:, :], func=AF.Copy), reads=[SBS], writes=[SBB])
                    yield

                    if debug:
                        E("sp", lambda e, t=t: e.dma_start(out=dbg_o[t * 128:(t + 1) * 128, :], in_=OALLL[tl].t[:, :]), reads=[OALLL[tl]], dma=dbgsem)
                        yield

                    yield
                def thrG(tl, b=b, gb="g"):
                    t = b * TPB + tl
                    cs = slice(tl * 128, (tl + 1) * 128)
                    sc = slice(tl * 4, (tl + 1) * 4)
                    hc = [slice(h * 128, (h + 1) * 128) for h in range(4)]
                    E("pool", lambda e: e.tensor_tensor(out=OSQ.t[:, :], in0=OALLL[tl].t[:, :], in1=OALLL[tl].t[:, :], op=OP.mult), reads=[OALLL[tl]], writes=[OSQ])
                    yield
                    E("dve", lambda e: e.tensor_reduce(out=RSTD8.t[:, :], in_=v3(OSQ.t[:, :], 8), axis=AX.X, op=OP.add), reads=[OSQ], writes=[RSTD8])
                    yield
                    rstd_from_ss(RSTD8.t[:, :], RSTD8.t[:, :], 128, [RSTD8], [RSTD8])
                    E("dve", lambda e: e.tensor_tensor(out=v3(OSQ.t[:, :], 8), in0=v3(OALLL[tl].t[:, :], 8), in1=bc3(RSTD8.t[:, :], 8, 128), op=OP.mult),
                      reads=[OALLL[tl], RSTD8], writes=[OSQ])
                    yield
                    E("dve", lambda e, tl=tl: e.tensor_tensor(out=MIX.t[:, :], in0=OSQ.t[:, :], in1=SG[tl].t[:, :], op=OP.mult),
                      reads=[OSQ, SG[tl]], writes=[MIX])
                    yield
                    pt = bank(gb)
                    for kc in range(8):
                        E("pe", lambda e, kc=kc, pt=pt: e.transpose(out=bfv(pt)[:, kc * 128:(kc + 1) * 128], in_=MIX.t[:, kc * 128:(kc + 1) * 128], identity=IDB),
                          reads=[MIX, CBF], writes=[pt])
                        yield
                    E("act", lambda e, pt=pt: e.activation(out=MIXT.t[:, :, :], in_=v3(bfv(pt)[:, :], 8), func=AF.Copy), reads=[pt], writes=[MIXT])
                    yield
                    E("sp", lambda e, t=t: e.dma_start(out=X1.t[:, :], in_=x[t * 128:(t + 1) * 128, :]), writes=[X1], dma=xrsem)
                    yield
                    for half in range(2):
                        pm = bank(gb)
                        for kc in range(8):
                            E("pe", lambda e, kc=kc, pm=pm, half=half: e.matmul(pm.t[:, :], lhsT=MIXT.t[:, kc, :], rhs=WOUT.t[:, kc, half * 512:(half + 1) * 512],
                                                                               start=(kc == 0), stop=(kc == 7)),
                              reads=[MIXT, WOUT], writes=[pm])
                            yield
                        E("dve", lambda e, pm=pm, half=half, tl=tl: e.tensor_tensor(out=X1.t[:, half * 512:(half + 1) * 512], in0=pm.t[:, :],
                                                                                   in1=X1.t[:, half * 512:(half + 1) * 512], op=OP.add),
                          reads=[pm, X1], writes=[X1])
                        yield
                    E("sp", lambda e, t=t: e.dma_start(out=x1_scr[t * 128:(t + 1) * 128, :], in_=X1.t[:, :]), reads=[X1], dma=x1sem)
                    yield
                    if t == 0:
                        dump("mix", MIX.t[:, :], [128, D], BF16, [MIX])
                    E("pool", lambda e: e.memset(SSG.t[:, 2:3], 0.0), writes=[SSG])
                    yield
                    E("act", lambda e: e.activation(out=H2B.t[:, :], in_=X1.t[:, :], func=AF.Square, accum_out=SSG.t[:, 2:3]),
                      reads=[X1, SSG], writes=[H2B, SSG])
                    yield
                    rstd_from_ss(SSG.t[:, 2:3], SSG.t[:, 3:4], D, [SSG], [SSG])
                    E("act", lambda e: e.activation(out=H2B.t[:, :], in_=X1.t[:, :], func=AF.Copy, scale=SSG.t[:, 3:4]), reads=[X1, SSG], writes=[H2B])
                    yield
                    for half in range(2):
                        pc = bank(gb)
                        for q in range(4):
                            kc = half * 4 + q
                            E("pe", lambda e, kc=kc, q=q, pc=pc: e.transpose(out=pc.t[:, q * 128:(q + 1) * 128], in_=X1.t[:, kc * 128:(kc + 1) * 128], identity=IDF),
                              reads=[X1, CF], writes=[pc])
                            yield
                        E("act", lambda e, pc=pc, half=half: e.activation(out=H2T.t[:, half * 4:(half + 1) * 4, :], in_=v3(pc.t[:, :], 4), func=AF.Copy),
                          reads=[pc], writes=[H2T])
                        yield
                    pl = bank(gb)
                    for kc in range(8):
                        E("pe", lambda e, kc=kc, pl=pl: e.matmul(pl.t[:, 0:72], lhsT=H2T.t[:, kc, :], rhs=W_R.t[:, kc * 72:(kc + 1) * 72],
                                                                 start=(kc == 0), stop=(kc == 7)),
                          reads=[H2T, W_R], writes=[pl])
                        yield
                    E("dve", lambda e, pl=pl: e.tensor_scalar(out=LG.t[:, :], in0=pl.t[:, 0:72], scalar1=SSG.t[:, 3:4], scalar2=None, op0=OP.mult), reads=[pl, SSG], writes=[LG])
                    yield
                    R = RT.t
                    E("dve", lambda e: e.tensor_reduce(out=R[:, 0:1], in_=LG.t[:, 0:8], axis=AX.X, op=OP.max), reads=[LG], writes=[RT])
                    yield
                    E("dve", lambda e: e.tensor_scalar(out=GMASK.t[:, :], in0=LG.t[:, 0:8], scalar1=R[:, 0:1], scalar2=None, op0=OP.is_equal),
                      reads=[LG, RT], writes=[GMASK])
                    yield
                    E("dve", lambda e: e.tensor_scalar(out=R[:, 1:2], in0=R[:, 0:1], scalar1=-1.0, scalar2=None, op0=OP.mult), reads=[RT], writes=[RT])
                    yield
                    E("pool", lambda e: e.memset(R[:, 2:3], 0.0), writes=[RT])
                    yield
                    E("act", lambda e: e.activation(out=PEN.t[:, :], in_=LG.t[:, 0:8], func=AF.Exp, bias=R[:, 1:2], accum_out=R[:, 2:3]),
                      reads=[LG, RT], writes=[PEN, RT])
                    yield
                    E("dve", lambda e: e.reciprocal(out=R[:, 3:4], in_=R[:, 2:3]), reads=[RT], writes=[RT])
                    yield
                    E("dve", lambda e: e.tensor_scalar(out=PEN.t[:, :], in0=GMASK.t[:, :], scalar1=1e30, scalar2=-1e30, op0=OP.mult, op1=OP.add),
                      reads=[GMASK], writes=[PEN])
                    yield
                    E("dve", lambda e: e.tensor_tensor(out=v3(EL.t[:, :], 8), in0=v3(LG.t[:, 8:72], 8), in1=bc3(PEN.t[:, :], 8, 8), op=OP.add),
                      reads=[LG, PEN], writes=[EL])
                    yield
                    E("dve", lambda e: e.tensor_reduce(out=R[:, 4:5], in_=EL.t[:, :], axis=AX.X, op=OP.max), reads=[EL], writes=[RT])
                    yield
                    E("dve", lambda e: e.tensor_scalar(out=OH1.t[:, :], in0=EL.t[:, :], scalar1=R[:, 4:5], scalar2=None, op0=OP.is_equal),
                      reads=[EL, RT], writes=[OH1])
                    yield
                    E("dve", lambda e: e.scalar_tensor_tensor(out=EL2.t[:, :], in0=OH1.t[:, :], scalar=-1e30, in1=EL.t[:, :], op0=OP.mult, op1=OP.add),
                      reads=[OH1, EL], writes=[EL2])
                    yield
                    E("dve", lambda e: e.tensor_reduce(out=R[:, 5:6], in_=EL2.t[:, :], axis=AX.X, op=OP.max), reads=[EL2], writes=[RT])
                    yield
                    E("dve", lambda e: e.tensor_scalar(out=OH2.t[:, :], in0=EL2.t[:, :], scalar1=R[:, 5:6], scalar2=None, op0=OP.is_equal),
                      reads=[EL2, RT], writes=[OH2])
                    yield
                    E("dve", lambda e: e.tensor_tensor(out=R[:, 6:7], in0=R[:, 5:6], in1=R[:, 4:5], op=OP.subtract), reads=[RT], writes=[RT])
                    yield
                    E("act", lambda e: e.activation(out=R[:, 7:8], in_=R[:, 6:7], func=AF.Exp), reads=[RT], writes=[RT])
                    yield
                    E("dve", lambda e: e.tensor_scalar(out=R[:, 8:9], in0=R[:, 7:8], scalar1=1.0, scalar2=None, op0=OP.add), reads=[RT], writes=[RT])
                    yield
                    E("dve", lambda e: e.reciprocal(out=R[:, 9:10], in_=R[:, 8:9]), reads=[RT], writes=[RT])
                    yield
                    E("dve", lambda e: e.tensor_tensor(out=R[:, 10:11], in0=R[:, 7:8], in1=R[:, 9:10], op=OP.mult), reads=[RT], writes=[RT])
                    yield
                    E("dve", lambda e, t=t: e.tensor_scalar(out=CW.t[:, 2 * t:2 * t + 2], in0=R[:, 9:11], scalar1=R[:, 3:4], scalar2=None, op0=OP.mult),
                      reads=[RT], writes=[CW])
                    yield
                    if t == 0:
                        dump("lg", LG.t[:, :], [128, 72], F32, [LG])
                        dump("rt", RT.t[:, :], [128, 16], F32, [RT])
                    E("dve", lambda e: e.tensor_tensor(out=OH12.t[:, :], in0=OH1.t[:, :], in1=OH2.t[:, :], op=OP.add), reads=[OH1, OH2], writes=[OH12])
                    yield
                    pcn = bank(gb)
                    E("pe", lambda e, pcn=pcn: e.matmul(pcn.t[:, 0:64], lhsT=SUB, rhs=OH12.t[:, :], start=True, stop=True), reads=[CBF, OH12], writes=[pcn])
                    yield
                    E("pe", lambda e, pcn=pcn: e.matmul(pcn.t[:, 64:128], lhsT=ONB, rhs=OH12.t[:, :], start=True, stop=True), reads=[CBF, OH12], writes=[pcn])
                    yield
                    E("dve", lambda e, pcn=pcn: e.tensor_tensor(out=POSM.t[:, :], in0=pcn.t[:, 0:64], in1=BASECAP.t[:, :], op=OP.add),
                      reads=[pcn, BASECAP], writes=[POSM])
                    yield
                    E("dve", lambda e, pcn=pcn: e.tensor_tensor(out=BASECAP.t[:, :], in0=pcn.t[:, 64:128], in1=BASECAP.t[:, :], op=OP.add),
                      reads=[pcn, BASECAP], writes=[BASECAP])
                    yield
                    for k, oh in enumerate((OH1, OH2)):
                        E("dve", lambda e, oh=oh: e.tensor_tensor(out=PRD.t[:, :], in0=oh.t[:, :], in1=POSM.t[:, :], op=OP.mult), reads=[oh, POSM], writes=[PRD])
                        yield
                        E("dve", lambda e, k=k: e.tensor_reduce(out=OFF_F.t[:, k:k + 1], in_=PRD.t[:, :], axis=AX.X, op=OP.add), reads=[PRD], writes=[OFF_F])
                        yield
                    E("dve", lambda e, t=t: e.tensor_copy(out=OFFS.t[:, 2 * t:2 * t + 2], in_=OFF_F.t[:, :]), reads=[OFF_F], writes=[OFFS])
                    yield
                    for k in range(2):
                        E("pool", lambda e, t=t, k=k: e.indirect_dma_start(out=xs_all[:, :], out_offset=bass.IndirectOffsetOnAxis(ap=OFFS.t[:, 2 * t + k:2 * t + k + 1], axis=0),
                                                                          in_=H2B.t[:, :], in_offset=None),
                          reads=[H2B, OFFS, XSB], dma=scsem)
                        yield
                    yield
                if b == 0:
                    E("sp", lambda e: e.dma_start(out=ROPE.t[:, :, :], in_=rope_d[:, :, 0:BLK]), writes=[ROPE], dma=rsem)
                run_rr([thrP1(), pendG[0]])
                if b > 0:
                    E("sp", lambda e, b=b: e.dma_start(out=ROPE.t[:, :, :], in_=rope_d[:, :, b * BLK:(b + 1) * BLK]), writes=[ROPE], dma=rsem)
                run_rr([thrL(0, SQ, RINV, "cv"), thrL(1, SQ_B, RINV_B, "tr"), thrScal()])
                dumps23()
                n0 = thrN(0, 0); n1 = thrN(1, 1); rp = thrRope(); s0 = thrS(0)
                nA = thrN(2, 0)
                nB = thrN(3, 1)
                run_rr([s0, n0, n1, rp, nA], after={s0: [n0, rp], nA: [n0]}, stop_on=(0,), must_finish=(1, 2, 3))
                run_rr([thrS(1), thrG(0), nA, nB], stop_on=(0,), must_finish=(1, 2))
                run_rr([thrS(2), thrG(1), nB])
                if b + 1 < NB:
                    ldx_blk(b + 1, 0)
                    ldx_blk(b + 1, 1)
                run_rr([thrS(3), thrG(2)])
                pendG[0] = thrG(TPB - 1, gb="g2")
            run_rr([pendG[0]])
            if debug:
                offs_d = dram("offs_d", [128, NT * 2], I32, "ExternalOutput")
                cw_d = dram("cw_d", [128, NT * 2], F32, "ExternalOutput")
                E("sp", lambda e: e.dma_start(out=offs_d, in_=OFFS.t[:, :]), reads=[OFFS], dma=fw.dsem())
                E("sp", lambda e: e.dma_start(out=cw_d, in_=CW.t[:, :]), reads=[CW], dma=fw.dsem())
            fw.barrier()

        with ExitStack() as p2:
            def sb2(name, shape, ty):
                return sb(name, shape, ty, p2)
            NWB = 6
            WGU = [sb2("WGU%d" % i, [128, 4096], BF16) for i in range(NWB)]
            WD = [sb2("WD%d" % i, [128, 2048], BF16) for i in range(NWB)]
            wsm = [fw.dsem() for _ in range(NWB)]
            wsm2 = [fw.dsem() for _ in range(NWB)]
            XS = [sb2("XS%d" % i, [128, 2, D], BF16) for i in range(4)]
            xsm = [fw.dsem() for _ in range(4)]
            XST = [sb2("XST%d" % i, [128, 8, CAP], BF16) for i in range(2)]
            GS = [sb2("GS%d" % i, [128, CAP], F32) for i in range(2)]
            ACTT = [sb2("ACTT%d" % i, [128, 2, CAP], BF16) for i in range(2)]
            YS = [sb2("YS%d" % i, [128, 2, D], BF16) for i in range(2)]
            ysm = [fw.dsem() for _ in range(2)]
            YAB = Buf("y_all")
            xs_e = xs_all.rearrange("(e r p) d -> e p r d", e=NE, p=128)
            y_e = y_all.rearrange("(e r p) d -> e p r d", e=NE, p=128)

            def load_w(ex):
                i = ex % NWB
                E("pool", lambda e: e.dma_start(out=WGU[i].t[:, :], in_=wgu_bf[ex]), writes=[WGU[i]], dma=wsm[i])
                E("pool", lambda e: e.dma_start(out=WD[i].t[:, :], in_=wd_bf[ex]), writes=[WD[i]], dma=wsm2[i])

            def ldxs(ex):
                j4 = ex % 4
                E("sp", lambda e: e.dma_start(out=XS[j4].t[:, :, :], in_=xs_e[ex]), reads=[XSB], writes=[XS[j4]], dma=xsm[j4])

            def stA(ex):
                j = ex % 2
                j4 = ex % 4
                for r in range(2):
                    pt = bank("tr2")
                    for kc in range(8):
                        E("pe", lambda e, kc=kc, pt=pt, r=r: e.transpose(out=bfv(pt)[:, kc * 128:(kc + 1) * 128], in_=XS[j4].t[:, r, kc * 128:(kc + 1) * 128], identity=IDB),
                          reads=[XS[j4], CBF], writes=[pt])
                    E("dve", lambda e, pt=pt, r=r: e.tensor_tensor(out=XST[j].t[:, :, r * 128:(r + 1) * 128], in0=v3(bfv(pt)[:, :], 8),
                                                                   in1=bc3(COLS.t[:, 56:64], 8, 128), op=OP.mult),
                      reads=[pt, COLS], writes=[XST[j]])

            def stB(ex):
                i = ex % NWB
                j = ex % 2
                for fc in range(2):
                    pg = bank("mm4")
                    for kc in range(8):
                        E("pe", lambda e, kc=kc, pg=pg, fc=fc: e.matmul(pg.t[:, 0:CAP], lhsT=WGU[i].t[:, kc * 256 + fc * 128:kc * 256 + fc * 128 + 128],
                                                                       rhs=XST[j].t[:, kc, :], start=(kc == 0), stop=(kc == 7)),
                          reads=[WGU[i], XST[j]], writes=[pg])
                    pu = bank("mm4")
                    for kc in range(8):
                        E("pe", lambda e, kc=kc, pu=pu, fc=fc: e.matmul(pu.t[:, 0:CAP], lhsT=WGU[i].t[:, 2048 + kc * 256 + fc * 128:2048 + kc * 256 + fc * 128 + 128],
                                                                       rhs=XST[j].t[:, kc, :], start=(kc == 0), stop=(kc == 7)),
                          reads=[WGU[i], XST[j]], writes=[pu])
                    gs = GS[fc]
                    E("act", lambda e, pg=pg, gs=gs: e.activation(out=gs.t[:, :], in_=pg.t[:, 0:CAP], func=AF.Silu), reads=[pg], writes=[gs])
                    E("dve", lambda e, pu=pu, fc=fc, gs=gs: e.tensor_tensor(out=ACTT[j].t[:, fc, :], in0=pu.t[:, 0:CAP], in1=gs.t[:, :], op=OP.mult),
                      reads=[pu, gs], writes=[ACTT[j]])

            def stC(ex):
                i = ex % NWB
                j = ex % 2
                for r in range(2):
                    for half in range(2):
                        py = bank("dw")
                        for fc in range(2):
                            E("pe", lambda e, fc=fc, py=py, r=r, half=half: e.matmul(py.t[:, :], lhsT=ACTT[j].t[:, fc, r * 128:(r + 1) * 128],
                                                                                    rhs=WD[i].t[:, fc * 1024 + half * 512:fc * 1024 + half * 512 + 512],
                                                                                    start=(fc == 0), stop=(fc == 1)),
                              reads=[ACTT[j], WD[i]], writes=[py])
                        if half == 0:
                            E("act", lambda e, py=py, r=r: e.activation(out=YS[j].t[:, r, 0:512], in_=py.t[:, :], func=AF.Copy), reads=[py], writes=[YS[j]])
                        else:
                            E("dve", lambda e, py=py, r=r: e.tensor_copy(out=YS[j].t[:, r, 512:1024], in_=py.t[:, :]), reads=[py], writes=[YS[j]])
                E("sp", lambda e: e.dma_start(out=y_e[ex], in_=YS[j].t[:, :, :]), reads=[YS[j]], dma=ysm[j])

            for ex in range(4):
                load_w(ex)
            for ex in range(3):
                ldxs(ex)
            for it in range(NE + 2):
                if it < NE:
                    stA(it)
                if it + 3 < NE:
                    ldxs(it + 3)
                if 0 <= it - 1 < NE:
                    stB(it - 1)
                if 0 <= it - 2 < NE:
                    stC(it - 2)
                if it + 4 < NE:
                    load_w(it + 4)
            fw.barrier()

        with ExitStack() as p3:
            def sb3(name, shape, ty):
                return sb(name, shape, ty, p3)
            FIN = sb3("FIN", [128, D], F32)
            fsem = fw.dsem()
            E("sp", lambda e: e.dma_start(out=FIN.t[:, :], in_=bass.AP(tensor=fin_d.tensor, offset=0, ap=[[0, 128], [1, D]])), writes=[FIN], dma=fsem)
            NB3 = 4
            X1L = [sb3("X1L%d" % i, [128, D], F32) for i in range(NB3)]
            Y1 = [sb3("Y1_%d" % i, [128, D], BF16) for i in range(NB3)]
            Y2 = [sb3("Y2_%d" % i, [128, D], BF16) for i in range(NB3)]
            l1 = [fw.dsem() for _ in range(NB3)]
            l2 = [fw.dsem() for _ in range(NB3)]
            l3 = [fw.dsem() for _ in range(NB3)]
            ACC = [sb3("ACC%d" % i, [128, D], F32) for i in range(2)]
            OUTT = [sb3("OUTT%d" % i, [128, D], F32) for i in range(2)]
            osm = [fw.dsem() for _ in range(2)]
            JK = sb3("JK", [128, D], BF16)
            S3 = sb3("S3", [128, 4 * NT], F32)
            E("pool", lambda e: e.memset(S3.t[:, :], 0.0), writes=[S3])

            def loads3(t):
                j = t % NB3
                E("sp", lambda e, t=t, j=j: e.dma_start(out=X1L[j].t[:, :], in_=x1_scr[t * 128:(t + 1) * 128, :]), writes=[X1L[j]], dma=l1[j])
                E("pool", lambda e, t=t, j=j: e.indirect_dma_start(out=Y1[j].t[:, :], out_offset=None, in_=y_all[:, :],
                                                                  in_offset=bass.IndirectOffsetOnAxis(ap=OFFS.t[:, 2 * t:2 * t + 1], axis=0)),
                  reads=[OFFS], writes=[Y1[j]], dma=l2[j])
                E("pool", lambda e, t=t, j=j: e.indirect_dma_start(out=Y2[j].t[:, :], out_offset=None, in_=y_all[:, :],
                                                                  in_offset=bass.IndirectOffsetOnAxis(ap=OFFS.t[:, 2 * t + 1:2 * t + 2], axis=0)),
                  reads=[OFFS], writes=[Y2[j]], dma=l3[j])

            for t in range(min(3, NT)):
                loads3(t)
            for t in range(NT):
                j = t % NB3
                k = t % 2
                E("dve", lambda e, t=t, j=j, k=k: e.scalar_tensor_tensor(out=ACC[k].t[:, :], in0=Y1[j].t[:, :], scalar=CW.t[:, 2 * t:2 * t + 1], in1=X1L[j].t[:, :],
                                                                        op0=OP.mult, op1=OP.add),
                  reads=[Y1[j], CW, X1L[j]], writes=[ACC[k]])
                E("dve", lambda e, t=t, j=j, k=k: e.scalar_tensor_tensor(out=ACC[k].t[:, :], in0=Y2[j].t[:, :], scalar=CW.t[:, 2 * t + 1:2 * t + 2], in1=ACC[k].t[:, :],
                                                                        op0=OP.mult, op1=OP.add),
                  reads=[Y2[j], CW, ACC[k]], writes=[ACC[k]])
                if t + 3 < NT:
                    loads3(t + 3)
                E("act", lambda e, t=t, k=k: e.activation(out=JK.t[:, :], in_=ACC[k].t[:, :], func=AF.Square, accum_out=S3.t[:, 4 * t:4 * t + 1]),
                  reads=[ACC[k], S3], writes=[JK, S3])
                rs = S3.t[:, 4 * t + 1:4 * t + 2]
                E("dve", lambda e, t=t, rs=rs: e.tensor_scalar(out=rs, in0=S3.t[:, 4 * t:4 * t + 1], scalar1=1.0 / D, scalar2=EPS, op0=OP.mult, op1=OP.add),
                  reads=[S3], writes=[S3])
                E("act", lambda e, rs=rs: e.activation(out=rs, in_=rs, func=AF.Ln), reads=[S3], writes=[S3])
                E("act", lambda e, rs=rs: e.activation(out=rs, in_=rs, func=AF.Exp, scale=-0.5), reads=[S3], writes=[S3])
                E("dve", lambda e, k=k, rs=rs: e.scalar_tensor_tensor(out=OUTT[k].t[:, :], in0=ACC[k].t[:, :], scalar=rs, in1=FIN.t[:, :], op0=OP.mult, op1=OP.mult),
                  reads=[ACC[k], S3, FIN], writes=[OUTT[k]])
                E("sp", lambda e, t=t, k=k: e.dma_start(out=out[t * 128:(t + 1) * 128, :], in_=OUTT[k].t[:, :]), reads=[OUTT[k]], dma=osm[k])
        fw.finish()
    nc._dump_names = list(dumps.keys())
    return nc


def _consts():
    f = np.float32
    i = np.arange(128)
    same = (i[:, None] // 64) == (i[None, :] // 64)
    cfm = np.zeros((128, NCF), f)

    def put(name, arr):
        a, b = _cfo[name]
        cfm[:, a:b] = arr
    put("ident", np.eye(128, dtype=f))
    put("ub", ((i[:, None] <= i[None, :]) & same).astype(f))
    put("slb", ((i[:, None] > i[None, :]) & same).astype(f))
    put("bb", same.astype(f))
    put("ones", np.ones((128, 128), f))
    inc = (i[None, :] >= i[:, None]) & same
    strict = (i[None, :] > i[:, None]) & same
    put("negm4", np.tile(np.where(inc, 0.0, -30000.0).astype(f), (1, 4)))
    put("strn4", np.where(strict, -1.0, 0.0).astype(f))
    e4 = np.zeros((128, 512), np.float64)
    kd = np.zeros((128, 4), np.float64)
    osc = np.zeros((128, 4), np.float64)
    for h in range(4):
        g = GAM[h]
        m = (i[None, :] >= i[:, None])
        e4[:, h * 128:(h + 1) * 128] = np.where(m, (128.0 ** -0.5) * g ** (-(i[:, None] + 1.0)), 0.0)
        kd[:, h] = (128.0 ** -0.5) * g ** (127.0 - i)
        osc[:, h] = g ** (i + 1.0)
    put("e4", e4.astype(f))
    put("su", (i[:, None] < i[None, :]).astype(f))
    pm = np.zeros((128, 128), f)
    pm[(i + 64) % 128, i] = 1.0
    put("pm", pm)
    put("cm", np.stack([(i < 64), (i >= 64)], 1).astype(f))
    put("kdsc", kd.astype(f))
    put("osc", osc.astype(f))
    put("iotacap", np.tile((np.arange(NE) * CAP).astype(f)[None, :], (128, 1)))
    pos = np.arange(S, dtype=f)
    inv = (f(10000.0) ** (-(np.arange(0, 128, 2, dtype=f)) / f(128.0))).astype(f)
    ang = (pos[:, None] * inv[None, :]).astype(f)
    cos = np.cos(ang).astype(f).T
    sin = np.sin(ang).astype(f).T
    rope = np.zeros((128, 2, S), f)
    rope[0:64, 0] = cos
    rope[64:128, 0] = cos
    rope[0:64, 1] = -sin
    rope[64:128, 1] = sin
    return cfm, rope


_CACHE = {}


def kernel(x, attn_norm, w_in, conv_a, a_log, dt_bias, norm_a, norm_b, w_out, ffn_norm,
           w_router_group, w_router_expert, w_gate, w_up, w_down, final_norm, _debug=False):
    f = np.float32
    x = np.asarray(x, f)
    w_in_l = np.ascontiguousarray(np.asarray(w_in, f)[0].reshape(8, 128, DIN).transpose(1, 0, 2))
    w_out_l = np.ascontiguousarray(np.asarray(w_out, f)[0].reshape(8, 128, D).transpose(1, 0, 2))
    wr = np.concatenate([np.asarray(w_router_group, f)[0], np.asarray(w_router_expert, f)[0]], axis=1)
    w_r_l = np.ascontiguousarray(wr.reshape(8, 128, 72).transpose(1, 0, 2).reshape(128, 8 * 72))
    wg = np.asarray(w_gate, f)[0].reshape(NE, 8, 128, 256).transpose(0, 2, 1, 3).reshape(NE, 128, 2048)
    wu = np.asarray(w_up, f)[0].reshape(NE, 8, 128, 256).transpose(0, 2, 1, 3).reshape(NE, 128, 2048)
    wgu_l = np.ascontiguousarray(np.concatenate([wg, wu], axis=2))
    wd_l = np.ascontiguousarray(np.asarray(w_down, f)[0].reshape(NE, 2, 128, D).transpose(0, 2, 1, 3).reshape(NE, 128, 2048))
    cols = np.zeros((128, NCOL), f)
    ca = np.asarray(conv_a, f)[0]
    cols[:, 0:48] = ca.reshape(4, 12, 128).transpose(2, 1, 0).reshape(128, 48)
    normfull = np.concatenate([np.tile(np.asarray(norm_a, f)[0], 4), np.asarray(norm_b, f)[0]])
    cols[:, 48:56] = normfull.reshape(8, 128).T
    cols[:, 56:64] = np.asarray(ffn_norm, f)[0].reshape(8, 128).T
    cols[:, 64:72] = np.asarray(attn_norm, f)[0].reshape(8, 128).T
    rows = np.concatenate([np.asarray(a_log, f)[0], np.asarray(dt_bias, f)[0]])[None, :].astype(f)
    fin = np.asarray(final_norm, f)[None, :]
    cfm, rope = _consts()
    key = bool(_debug)
    if key not in _CACHE:
        _CACHE[key] = build(debug=_debug)
    nc = _CACHE[key]
    in_maps = []
    ncores = 1 if _debug else NCORES
    for c in range(ncores):
        in_maps.append({"x": np.ascontiguousarray(x[c]), "w_in": w_in_l, "w_out": w_out_l, "w_r": w_r_l, "wgu": wgu_l, "wd": wd_l,
                        "cols": cols, "rows": rows, "fin": fin, "cf": cfm, "rope": rope})
    res = run_bass_kernel_spmd(nc, in_maps, core_ids=list(range(ncores)))
    outp = np.stack([np.asarray(r["out"], f) for r in res.results], axis=0)
    if _debug:
        kernel.dbg = [{k: np.asarray(r[k]) for k in ["x1_scr", "dbg_o", "offs_d", "cw_d"] + ["dd_" + n for n in nc._dump_names]} for r in res.results]
    return outp
```

```python
import os
import numpy as np
from contextlib import ExitStack
import concourse.bass as bass
import concourse.mybir as mybir
from concourse.bass_utils import run_bass_kernel_spmd

F32 = mybir.dt.float32
BF16 = mybir.dt.bfloat16
I32 = mybir.dt.int32
AF = mybir.ActivationFunctionType
OP = mybir.AluOpType
AX = mybir.AxisListType

S = 4096
D = 1024
NT = 32
NB = 8
TPB = 4
BLK = 512
NE = 64
CAP = 256
DIN = 4104
EPS = 1e-6
NCORES = 8
GAM = [1.0 - 2.0 ** (-5.0 - h) for h in range(4)]

_cfo = {}
_o = 0
for _n, _w in (("ident", 128), ("ub", 128), ("slb", 128), ("bb", 128), ("ones", 128), ("negm4", 512),
               ("strn4", 128), ("e4", 512), ("su", 128), ("pm", 128), ("cm", 2),
               ("kdsc", 4), ("osc", 4), ("iotacap", 64)):
    _cfo[_n] = (_o, _o + _w)
    _o += _w
NCF = _o
NCOL = 72
NROW = 8


import types


def _freeze(fn):
    if fn.__closure__ is None:
        return fn
    cells = []
    for c in fn.__closure__:
        try:
            cells.append(types.CellType(c.cell_contents))
        except ValueError:
            cells.append(c)
    return types.FunctionType(fn.__code__, fn.__globals__, fn.__name__, fn.__defaults__, tuple(cells))


COST = {"pe": 0.12, "act": 0.7, "dve": 0.6, "pool": 1.6, "sp": 0.05}
LAT = 0.25
DMA_LAT = 2.5


class Eng:
    def __init__(s, name, sem):
        s.name = name; s.sem = sem; s.cnt = 0; s.waited = {}; s.prog = []; s.tfree = 0.0


class DSem:
    def __init__(s, sem):
        s.sem = sem; s.val = 0


class Buf:
    def __init__(s, name="", excl=False):
        s.name = name; s.w = None; s.r = {}; s.excl = excl
        s.tw = 0.0; s.tr = 0.0


class TB:
    def __init__(s, t, name=""):
        s.t = t; s.b = Buf(name)


class FW:
    def __init__(s, nc, stack):
        s.nc = nc
        s.stack = stack
        s.E = {}
        for n in ("pe", "act", "dve", "pool", "sp"):
            s.E[n] = Eng(n, stack.enter_context(nc.semaphore("sem_" + n)))
        s.dsems = []

    def dsem(s):
        d = DSem(s.stack.enter_context(s.nc.semaphore("dsem%d" % len(s.dsems))))
        s.dsems.append(d)
        return d

    def est_start(s, en, reads, writes):
        reads = [b.b if isinstance(b, TB) else b for b in reads]
        writes = [b.b if isinstance(b, TB) else b for b in writes]
        t = s.E[en].tfree
        for b in reads:
            t = max(t, (b.tw + LAT) if not b.excl else (max(b.tw, b.tr) + LAT))
        for b in writes:
            t = max(t, max(b.tw, b.tr) + LAT)
        return t

    def emit(s, en, fn, reads=(), writes=(), dma=None, cost=None, frozen=False):
        eng = s.E[en]
        deps = []
        reads = [b.b if isinstance(b, TB) else b for b in reads]
        writes = [b.b if isinstance(b, TB) else b for b in writes]
        t0 = s.est_start(en, reads, writes)
        c = COST[en] if cost is None else cost
        eng.tfree = t0 + c
        tfin = t0 + c + (DMA_LAT if dma is not None else 0.0)
        for b in reads:
            if b.excl:
                b.tw = max(b.tw, tfin)
            else:
                b.tr = max(b.tr, tfin)
        for b in writes:
            b.tw = max(b.tw, tfin); b.tr = 0.0
        writes = writes + [b for b in reads if b.excl]
        reads = [b for b in reads if not b.excl]
        for b in reads:
            if b.w is not None:
                deps.append(b.w)
        for b in writes:
            if b.w is not None:
                deps.append(b.w)
            deps.extend(b.r.values())
        waits = []
        for (sem, val) in deps:
            if sem is eng.sem and en in ("pe", "sp"):
                continue
            k = id(sem)
            if eng.waited.get(k, 0) >= val:
                continue
            eng.waited[k] = val
            waits.append((sem, val))
        if dma is None:
            eng.cnt += 1
            tok = (eng.sem, eng.cnt)
            inc = 1
        else:
            dma.val += 16
            tok = (dma.sem, dma.val)
            inc = 16
        eng.prog.append((waits, fn if frozen else _freeze(fn), tok[0], inc))
        for b in reads:
            if isinstance(b, TB):
                b = b.b
            b.r[id(tok[0])] = tok
        for b in writes:
            if isinstance(b, TB):
                b = b.b
            b.w = tok
            b.r = {}
        return tok

    def barrier(s):
        toks = [(e.sem, e.cnt) for e in s.E.values() if e.cnt > 0]
        toks += [(d.sem, d.val) for d in s.dsems if d.val > 0]
        for en, eng in s.E.items():
            waits = []
            for (sem, val) in toks:
                if sem is eng.sem:
                    continue
                if eng.waited.get(id(sem), 0) >= val:
                    continue
                eng.waited[id(sem)] = val
                waits.append((sem, val))
            if waits:
                eng.cnt += 1
                eng.prog.append((waits, (lambda e: e.nop()), eng.sem, 1))

    def finish(s):
        nc = s.nc
        finals = [(d.sem, d.val) for d in s.dsems if d.val > 0]
        with nc.Block() as block:
            def run(eng, e):
                for (waits, fn, sem, inc) in eng.prog:
                    for (ws, wv) in waits:
                        e.wait_ge(ws, wv)
                    fn(e).then_inc(sem, inc)

            @block.tensor
            def _(e):
                run(s.E["pe"], e)

            @block.scalar
            def _(e):
                run(s.E["act"], e)

            @block.vector
            def _(e):
                run(s.E["dve"], e)

            @block.gpsimd
            def _(e):
                run(s.E["pool"], e)

            @block.sync
            def _(e):
                run(s.E["sp"], e)
                for (ws, wv) in finals:
                    e.wait_ge(ws, wv)


def build(debug=False):
    nc = bass.Bass("TRN2", target_bir_lowering=False)

    def dram(name, shape, ty, kind="ExternalInput"):
        return nc.dram_tensor(name, shape, ty, kind=kind).ap()

    x = dram("x", [S, D], F32)
    w_in = dram("w_in", [128, 8, DIN], F32)
    w_out = dram("w_out", [128, 8, D], F32)
    w_r = dram("w_r", [128, 8 * 72], F32)
    wgu = dram("wgu", [NE, 128, 4096], F32)
    wd = dram("wd", [NE, 128, 2048], F32)
    cols_d = dram("cols", [128, NCOL], F32)
    rows_d = dram("rows", [1, NROW], F32)
    fin_d = dram("fin", [1, D], F32)
    cf_d = dram("cf", [128, NCF], F32)
    rope_d = dram("rope", [128, 2, S], F32)
    out = dram("out", [S, D], F32, "ExternalOutput")
    xs_all = dram("xs_all", [NE * CAP, D], BF16, "Internal")
    y_all = dram("y_all", [NE * CAP, D], BF16, "Internal")
    x1_scr = dram("x1_scr", [S, D], F32, "ExternalOutput" if debug else "Internal")
    wgu_bf = dram("wgu_bf", [NE, 128, 4096], BF16, "Internal")
    wd_bf = dram("wd_bf", [NE, 128, 2048], BF16, "Internal")
    dbg_o = dram("dbg_o", [S, D], F32, "ExternalOutput") if debug else None

    with ExitStack() as st:
        fw = FW(nc, st)
        sink = [None]

        def E(en, fn, reads=(), writes=(), dma=None, cost=None):
            if sink[0] is None:
                fw.emit(en, fn, reads, writes, dma, cost)
            else:
                sink[0].append((en, _freeze(fn), list(reads), list(writes), dma, cost))
        dumps = {}

        def dump(name, ap, shape, ty, rbufs):
            if not debug or name in dumps:
                return
            dumps[name] = dram("dd_" + name, list(shape), ty, "ExternalOutput")
            ds_ = fw.dsem()
            E("sp", lambda e: e.dma_start(out=dumps[name], in_=ap), reads=rbufs, dma=ds_)

        def sb(name, shape, ty, stack=st):
            return TB(stack.enter_context(nc.sbuf_tensor(name, shape, ty)), name)

        PB = [TB(st.enter_context(nc.psum_tensor("pb%d" % i, [128, 512], F32)), "pb%d" % i) for i in range(8)]
        for _pb in PB:
            _pb.b.excl = True
        prot = {"mm": [0, 1], "tr": [2], "cv": [3], "dn": [4], "nm": [5], "st": [6], "o": [7], "tr2": [2, 3], "dw": [6, 7], "mm4": [0, 1, 4, 5], "n0": [0], "g": [1, 3], "g2": [6, 7], "dnA": [2], "nmA": [4], "dnB": [0], "nmB": [5], "rmm": [1], "rcv": [3]}
        pidx = {k: 0 for k in prot}

        def bank(role):
            l = prot[role]
            i = l[pidx[role] % len(l)]
            pidx[role] += 1
            return PB[i]

        def bfv(pb):
            return pb.t[:, :].bitcast(BF16)

        def v3(ap, a):
            return ap.rearrange("p (a b) -> p a b", a=a)

        def bc3(ap2, a, b):
            return ap2.unsqueeze(2).to_broadcast([ap2.shape[0], a, b])

        def bcm(ap2, a, b):
            return ap2.unsqueeze(1).to_broadcast([ap2.shape[0], a, b])

        CF = sb("CF", [128, NCF], F32)
        COLS = sb("COLS", [128, NCOL], F32)
        ROWS = sb("ROWS", [128, NROW], F32)
        CBF = sb("CBF", [128, 512], BF16)
        W_R = sb("W_R", [128, 8 * 72], F32)
        NEGA = sb("NEGA", [128, 4], F32)
        CW = sb("CW", [128, NT * 2], F32)
        OFFS = sb("OFFS", [128, NT * 2], I32)
        BASECAP = sb("BASECAP", [128, NE], F32)

        def cf(name, lo=None, hi=None):
            a, b = _cfo[name]
            return CF.t[:, a:b]

        IDF = cf("ident")
        IDB = CBF.t[:, 0:128]
        SUB = CBF.t[:, 128:256]
        PMB = CBF.t[:, 256:384]
        ONB = CBF.t[:, 384:512]

        dq = [fw.dsem() for _ in range(4)]
        E("sp", lambda e: e.dma_start(out=CF.t[:, :], in_=cf_d), writes=[CF], dma=dq[0])
        E("sp", lambda e: e.dma_start(out=COLS.t[:, :], in_=cols_d), writes=[COLS], dma=dq[1])
        E("sp", lambda e: e.dma_start(out=ROWS.t[:, :], in_=bass.AP(tensor=rows_d.tensor, offset=0, ap=[[0, 128], [1, NROW]])),
          writes=[ROWS], dma=dq[2])
        E("sp", lambda e: e.dma_start(out=W_R.t[:, :], in_=w_r), writes=[W_R], dma=dq[3])
        for i, nm in enumerate(("ident", "su", "pm", "ones")):
            E("dve", lambda e, i=i, nm=nm: e.tensor_copy(out=CBF.t[:, i * 128:(i + 1) * 128], in_=cf(nm)),
              reads=[CF], writes=[CBF])
        for kc in range(8):
            E("dve", lambda e, kc=kc: e.tensor_scalar(out=W_R.t[:, kc * 72:(kc + 1) * 72], in0=W_R.t[:, kc * 72:(kc + 1) * 72],
                                                     scalar1=COLS.t[:, 56 + kc:57 + kc], scalar2=None, op0=OP.mult),
              reads=[W_R, COLS], writes=[W_R])
        E("act", lambda e: e.activation(out=NEGA.t[:, :], in_=ROWS.t[:, 0:4], func=AF.Exp), reads=[ROWS], writes=[NEGA])
        E("dve", lambda e: e.tensor_scalar(out=NEGA.t[:, :], in0=NEGA.t[:, :], scalar1=-1.0, scalar2=None, op0=OP.mult),
          reads=[NEGA], writes=[NEGA])
        E("dve", lambda e: e.tensor_copy(out=BASECAP.t[:, :], in_=cf("iotacap")), reads=[CF], writes=[BASECAP])

        ZT = sb("ZT", [128, 1024], BF16)
        E("pool", lambda e: e.memset(ZT.t[:, :], 0.0), writes=[ZT])
        XSB = Buf("xs_all")
        zsem = fw.dsem()
        zsem0 = fw.dsem()
        xs_c = xs_all.rearrange("(c q r) d -> c q (r d)", c=16, q=16)
        xs_v = xs_all.rearrange("(e p r) d -> e p (r d)", e=2 * NE, p=128)

        with ExitStack() as p1:
            def sb1(name, shape, ty):
                return sb(name, shape, ty, p1)

            WIN = sb1("WIN", [128, 8, DIN], BF16)
            WOUT = sb1("WOUT", [128, 8, D], BF16)
            CDT = [sb1("CDT%d" % i, [128, 4, 128], BF16) for i in range(2)]
            wsem = [fw.dsem()]
            for kc in range(8):
                E("pool", lambda e, kc=kc: e.dma_start(out=WIN.t[:, kc, :], in_=w_in[:, kc, :]), writes=[WIN], dma=wsem[0])

            def wg(c0):
                return WIN

            pcs = fw.dsem()
            def precast(ex):
                E("pool", lambda e, ex=ex: e.dma_start(out=wgu_bf[ex], in_=wgu[ex]), dma=pcs)
                E("pool", lambda e, ex=ex: e.dma_start(out=wd_bf[ex], in_=wd[ex]), dma=pcs)
            XT = [sb1("XT%d" % i, [128, D], F32) for i in range(2)]
            xsem = [fw.dsem() for _ in range(2)]
            xrsem = fw.dsem()
            SS = sb1("SS", [128, 8], F32)
            SSG = sb1("SSG", [128, 8], F32)
            SS4 = [sb1("SSx%d" % i, [128, 2], F32) for i in range(TPB)]
            HT = sb1("HT", [128, 8, BLK], BF16)
            PRE = [sb1("PRE%d" % i, [128, 3 + BLK], BF16) for i in range(2)]
            HIST = sb1("HIST", [128, 12, 3], BF16)
            VT = sb1("VT", [128, 4, BLK], BF16)
            SQ = sb1("SQ", [128, BLK], BF16)
            QTA = sb1("QTA", [128, 4, BLK], BF16)
            KTA = sb1("KTA", [128, 4, BLK], BF16)
            QTB = sb1("QTB", [128, 4, BLK], BF16)
            KTB = sb1("KTB", [128, 4, BLK], BF16)
            rsem = fw.dsem()
            SG = [sb1("SG%d" % i, [128, D], BF16) for i in range(TPB)]
            VB = [sb1("VB%d" % i, [128, 512], BF16) for i in range(2)] * 2
            VA2 = [sb1("VA%d" % i, [128, 512], BF16) for i in range(2)]
            KDEC2 = [sb1("KDEC%d" % i, [128, 512], BF16) for i in range(2)]
            KDECB = [sb1("KDECB%d" % i, [128, 512], BF16) for i in range(2)] * 2
            YF3 = [sb1("YF%d" % i, [128, 512], BF16) for i in range(3)]
            BA = sb1("BA", [128, TPB, 8], F32)
            BETA = sb1("BETA", [128, 16], F32)
            GG = sb1("GG", [128, 16], F32)
            GM = sb1("GM", [128, TPB, 8], F32)
            EGC = sb1("EGC", [128, 16], F32)
            NEGC = sb1("NEGC", [128, 16], F32)
            EDEC = sb1("EDEC", [128, 16], F32)
            LASTB = sb1("LASTB", [128, TPB * 8], F32)
            TMPS = sb1("TMPS", [128, 16], F32)
            RR = sb1("RR", [128, 512], F32)
            DECT = sb1("DECT", [128, 512], F32)
            WW = sb1("WW", [128, 512], F32)
            TT = [sb1("TT%d" % i, [128, 512], BF16) for i in range(2)]
            NN = [sb1("NN%d" % i, [128, 512], BF16) for i in range(2)]
            YY = [sb1("YY%d" % i, [128, 512], BF16) for i in range(2)]
            ATT2 = [sb1("ATT%d" % i, [128, 512], BF16) for i in range(2)]
            TMPZ = sb1("TMPZ", [128, 512], F32)
            ZZ = sb1("ZZ", [128, 512], BF16)
            VN = sb1("VN", [128, 512], BF16)
            SA = sb1("SA", [128, 512], F32)
            SAB = sb1("SAB", [128, 512], BF16)
            SBS = sb1("SBS", [128, 512], F32)
            SBB = sb1("SBB", [128, 512], BF16)
            ATB = sb1("ATB", [128, 512], BF16)
            OALLL = [sb1("OALL%d" % i, [128, D], F32) for i in range(2)] * 2
            RSTD8 = sb1("RSTD8", [128, 8], F32)
            MIX = sb1("MIX", [128, D], BF16)
            MIXT = sb1("MIXT", [128, 8, 128], BF16)
            X1 = sb1("X1", [128, D], F32)
            x1sem = fw.dsem()
            H2B = sb1("H2B", [128, D], BF16)
            H2T = sb1("H2T", [128, 8, 128], F32)
            OSQ = TB.__new__(TB); OSQ.b = H2T.b; OSQ.t = H2T.t[:, :, :].rearrange("p a b -> p (a b)")
            HBF = sb1("HBF1", [128, D], BF16)
            RINV = DECT
            RA = TB.__new__(TB); RA.b = X1.b; RA.t = X1.t[:, 0:512]
            RB = TMPZ
            SQ_B = TB.__new__(TB); SQ_B.b = PRE[0].b; SQ_B.t = PRE[0].t[:, 0:512]
            RINV_B = WW
            JUNKS = [TB.__new__(TB), TB.__new__(TB)]
            JUNKS[0].b = QTB.b; JUNKS[0].t = QTB.t[:, 0:2, :].rearrange("p a b -> p (a b)")
            JUNKS[1].b = KTB.b; JUNKS[1].t = KTB.t[:, 0:2, :].rearrange("p a b -> p (a b)")

            def alias(base, ap):
                a_ = TB.__new__(TB); a_.b = base.b; a_.t = ap
                return a_
            xt1bf = XT[1].t[:, 512:1024].bitcast(BF16)
            hbfv = HBF.t[:, :]
            NSETS = [
                (RR, DECT, WW, TT, NN, YY),
                (alias(XT[0], XT[0].t[:, 0:512]), alias(XT[0], XT[0].t[:, 512:1024]), alias(XT[1], XT[1].t[:, 0:512]),
                 [alias(XT[1], xt1bf[:, 0:512]), alias(XT[1], xt1bf[:, 512:1024])],
                 [alias(HBF, hbfv[:, 0:512]), alias(HBF, hbfv[:, 512:1024])],
                 [alias(PRE[0], PRE[0].t[:, 0:512]), alias(PRE[1], PRE[1].t[:, 0:512])]),
            ]
            NBANKS = [("dnA", "nmA"), ("dnB", "nmB")]
            s3 = lambda l2, third: [l2[0], l2[1], third, l2[0]]
            KDEC = s3(KDEC2, alias(CDT[0], CDT[0].t[:, :, :].rearrange("p a b -> p (a b)")))
            VA = s3(VA2, alias(CDT[1], CDT[1].t[:, :, :].rearrange("p a b -> p (a b)")))
            ATTL = s3(ATT2, alias(SQ, SQ.t[:, :]))
            YFL = [YF3[0], YF3[1], YF3[2], YF3[0]]
            TBF = SQ

            ROPE = TB.__new__(TB); ROPE.b = H2T.b; ROPE.t = H2T.t[:, :, :].rearrange("p (a c) b -> p a (c b)", a=2)
            wss = [fw.dsem() for _ in range(2)]
            for kc in range(8):
                stg = (X1, OSQ)[kc % 2]
                E("sp", lambda e, kc=kc, stg=stg: e.dma_start(out=stg.t[:, :], in_=w_out[:, kc, :]), writes=[stg], dma=wss[kc % 2])
                E("dve", lambda e, kc=kc, stg=stg: e.tensor_scalar(out=WOUT.t[:, kc, :], in0=stg.t[:, :], scalar1=COLS.t[:, 48 + kc:49 + kc],
                                                                   scalar2=None, op0=OP.mult),
                  reads=[stg, COLS], writes=[WOUT])
            LG = sb1("LG", [128, 72], F32)
            RT = sb1("RT", [128, 16], F32)
            GMASK = sb1("GMASK", [128, 8], F32)
            PEN = sb1("PEN", [128, 8], F32)
            EL = sb1("EL", [128, 64], F32)
            EL2 = sb1("EL2", [128, 64], F32)
            OH1 = sb1("OH1", [128, 64], F32)
            OH2 = sb1("OH2", [128, 64], F32)
            OH12 = sb1("OH12", [128, 64], BF16)
            POSM = sb1("POSM", [128, 64], F32)
            PRD = sb1("PRD", [128, 64], F32)
            OFF_F = sb1("OFF_F", [128, 2], F32)
            scsem = fw.dsem()
            dbgsem = fw.dsem() if debug else None

            for z in (SA, SBS):
                E("pool", lambda e, z=z: e.memset(z.t[:, :], 0.0), writes=[z])
            for z in (SAB, SBB):
                E("pool", lambda e, z=z: e.memset(z.t[:, :], 0.0), writes=[z])
            E("pool", lambda e: e.memset(HIST.t[:, :, :], 0.0), writes=[HIST])

            def rstd_from_ss(ss_ap, out_ap, n, rbufs, wbufs):
                E("dve", lambda e: e.tensor_scalar(out=out_ap, in0=ss_ap, scalar1=1.0 / n, scalar2=EPS, op0=OP.mult, op1=OP.add),
                  reads=rbufs, writes=wbufs)
                E("act", lambda e: e.activation(out=out_ap, in_=out_ap, func=AF.Ln), reads=wbufs, writes=wbufs)
                E("act", lambda e: e.activation(out=out_ap, in_=out_ap, func=AF.Exp, scale=-0.5), reads=wbufs, writes=wbufs)

            pendG = [None]

            from collections import deque

            def run_rr(gens, stop_on_first=False, stop_on=(), must_finish=(), after=None):
                gens = [g for g in gens if g is not None]
                for g in gens:
                    if g not in pend:
                        pend[g] = deque()
                alive = {g: True for g in gens}

                def fill(g):
                    q = pend[g]
                    while not q and alive[g]:
                        sink[0] = q
                        try:
                            next(g)
                        except StopIteration:
                            alive[g] = False
                        sink[0] = None
                    return bool(q)

                watch = [gens[0]] if stop_on_first else [gens[i] for i in stop_on]
                need = [gens[i] for i in must_finish]
                active = list(gens)
                while True:
                    best = None
                    for pi, g in enumerate(active):
                        if after and g in after and any(alive[x] or pend[x] for x in after[g]):
                            continue
                        if fill(g):
                            op = pend[g][0]
                            t = fw.est_start(op[0], op[2], op[3])
                            if best is None or t < best[0] - 1e-9:
                                best = (t, pi, g)
                    if best is None:
                        break
                    g = best[2]
                    en, fn, rd, wr, dma, cost = pend[g].popleft()
                    fw.emit(en, fn, rd, wr, dma, cost, frozen=True)
                    if watch and not any(alive[w] or pend[w] for w in watch):
                        active = [g2 for g2 in need if alive[g2] or pend[g2]]
                        watch = []
                        if not active:
                            break
                        stopping = True
                for g in gens:
                    if not alive[g] and not pend[g]:
                        pend.pop(g, None)

            pend = {}

            def ldx_blk(bb, tl):
                t = bb * TPB + tl
                xt = XT[tl % 2]
                E("sp", lambda e, t=t, xt=xt: e.dma_start(out=xt.t[:, :], in_=x[t * 128:(t + 1) * 128, :]), writes=[xt], dma=xsem[tl % 2])

            for b in range(NB):
                def thrP1():
                    def ldx(tl):
                        ldx_blk(b, tl)
                    if b == 0:
                        ldx(0)
                    for tl in range(TPB):
                        if tl + 1 < TPB and not (b > 0 and tl == 0):
                            ldx(tl + 1)
                        xt = XT[tl % 2]
                        ss = SS4[tl]
                        jk = JUNKS[tl % 2]
                        E("pool", lambda e, ss=ss: e.memset(ss.t[:, 0:1], 0.0), writes=[ss], cost=0.2)
                        yield
                        E("act", lambda e, xt=xt, ss=ss, jk=jk: e.activation(out=jk.t, in_=xt.t[:, :], func=AF.Square, accum_out=ss.t[:, 0:1]),
                          reads=[xt, ss], writes=[jk, ss], cost=1.1)
                        yield
                        rstd_from_ss(ss.t[:, 0:1], ss.t[:, 1:2], D, [ss], [ss])
                        E("dve", lambda e, xt=xt, ss=ss: e.tensor_scalar(out=HBF.t[:, :], in0=xt.t[:, :], scalar1=ss.t[:, 1:2], scalar2=None, op0=OP.mult),
                          reads=[xt, ss], writes=[HBF], cost=1.1)
                        yield
                        pt = bank("tr")
                        for kc in range(8):
                            E("pe", lambda e, kc=kc, pt=pt: e.transpose(out=bfv(pt)[:, kc * 128:(kc + 1) * 128], in_=HBF.t[:, kc * 128:(kc + 1) * 128],
                                                                        identity=IDB),
                              reads=[HBF, CBF], writes=[pt])
                            yield
                        E("dve", lambda e, pt=pt, tl=tl: e.tensor_tensor(out=HT.t[:, :, tl * 128:(tl + 1) * 128], in0=v3(bfv(pt)[:, :], 8),
                                                                         in1=bc3(COLS.t[:, 64:72], 8, 128), op=OP.mult),
                          reads=[pt, COLS], writes=[HT])
                        yield

                    if b == 0:
                        XS0 = Buf("xs_chunk0")
                        for ex in range(8):
                            E("sp", lambda e, ex=ex: e.dma_start(out=xs_v[ex], in_=ZT.t[:, :]), reads=[ZT], writes=[XS0], dma=zsem0)
                            yield
                        for k in range(1, 16):
                            E("sp", lambda e, k=k: e.dma_start(out=xs_c[k], in_=xs_c[0]), reads=[XS0], writes=[XSB], dma=zsem)
                            yield
                        dump("ht", HT.t[:, :, :], [128, 8, BLK], BF16, [HT])
                    def proj_fm(c0):
                        pm = bank("mm")
                        for kc in range(8):
                            E("pe", lambda e, kc=kc, pm=pm: e.matmul(pm.t[:, :], lhsT=WIN.t[:, kc, c0:c0 + 128], rhs=HT.t[:, kc, :],
                                                                     start=(kc == 0), stop=(kc == 7)),
                              reads=[wg(c0), HT], writes=[pm])
                        return pm

                    for c in range(12):
                        pm = proj_fm(c * 128)
                        pre = PRE[c % 2]
                        E("pool", lambda e, c=c, pre=pre: e.tensor_copy(out=pre.t[:, 0:3], in_=HIST.t[:, c, :]), reads=[HIST], writes=[pre])
                        yield
                        E("act", lambda e, pm=pm, pre=pre: e.activation(out=pre.t[:, 3:3 + BLK], in_=pm.t[:, :], func=AF.Copy), reads=[pm], writes=[pre])
                        yield
                        E("pool", lambda e, c=c, pre=pre: e.tensor_copy(out=HIST.t[:, c, :], in_=pre.t[:, BLK:BLK + 3]), reads=[pre], writes=[HIST])
                        yield
                        pc = bank("cv")
                        cd = CDT[c % 2]
                        for j in range(4):
                            E("dve", lambda e, j=j, c=c, cd=cd: e.tensor_scalar(out=cd.t[:, j, :], in0=IDF, scalar1=COLS.t[:, c * 4 + j:c * 4 + j + 1], scalar2=None, op0=OP.mult),
                              reads=[CF, COLS], writes=[cd])
                            yield
                        for j in range(4):
                            E("pe", lambda e, j=j, c=c, pc=pc, pre=pre, cd=cd: e.matmul(pc.t[:, :], lhsT=cd.t[:, j, :], rhs=pre.t[:, j:j + BLK],
                                                                                       start=(j == 0), stop=(j == 3)),
                              reads=[cd, pre], writes=[pc])
                            yield
                        if c < 8:
                            sd = QTA if c < 4 else KTA
                            E("act", lambda e, c=c, pc=pc, sd=sd: e.activation(out=sd.t[:, c % 4, :], in_=pc.t[:, :], func=AF.Silu), reads=[pc], writes=[sd])
                            yield
                        else:
                            E("act", lambda e, c=c, pc=pc: e.activation(out=VT.t[:, c - 8, :], in_=pc.t[:, :], func=AF.Silu), reads=[pc], writes=[VT])
                            yield
                    for tl in range(TPB):
                        for (c0, n, kind) in ((1536, 512, "ga"), (3592, 512, "gb"), (2048, 8, "ba")):
                            pm = bank("mm")
                            for kc in range(8):
                                E("pe", lambda e, kc=kc, pm=pm, c0=c0, n=n, tl=tl: e.matmul(pm.t[:, 0:n], lhsT=HT.t[:, kc, tl * 128:(tl + 1) * 128],
                                                                                           rhs=WIN.t[:, kc, c0:c0 + n], start=(kc == 0), stop=(kc == 7)),
                                  reads=[wg(c0), HT], writes=[pm])
                                yield
                            if kind == "ga":
                                E("act", lambda e, pm=pm, tl=tl: e.activation(out=SG[tl].t[:, 0:512], in_=pm.t[:, :], func=AF.Silu), reads=[pm], writes=[SG[tl]])
                                yield
                            elif kind == "gb":
                                E("act", lambda e, pm=pm, tl=tl: e.activation(out=SG[tl].t[:, 512:1024], in_=pm.t[:, :], func=AF.Silu), reads=[pm], writes=[SG[tl]])
                                yield
                            else:
                                E("dve", lambda e, pm=pm, tl=tl: e.tensor_copy(out=BA.t[:, tl, :], in_=pm.t[:, 0:8]), reads=[pm], writes=[BA])
                                yield

                    yield
                def thrL(par, SQ, RINV, lbank):
                    for c in range(par, 8, 2):
                        dst = QTA if c < 4 else KTA
                        E("pool", lambda e, c=c, dst=dst: e.tensor_tensor(out=SQ.t[:, :], in0=dst.t[:, c % 4, :], in1=dst.t[:, c % 4, :], op=OP.mult),
                          reads=[dst], writes=[SQ])
                        yield
                        pc = bank(lbank)
                        E("pe", lambda e, pc=pc: e.matmul(pc.t[:, :], lhsT=ONB, rhs=SQ.t[:, :], start=True, stop=True), reads=[CBF, SQ], writes=[pc])
                        yield
                        E("act", lambda e, pc=pc: e.activation(out=RINV.t[:, :], in_=pc.t[:, :], func=AF.Ln, bias=EPS), reads=[pc], writes=[RINV])
                        yield
                        lb = float(np.log(128.0 ** -0.5)) if c < 4 else 0.0
                        E("act", lambda e, lb=lb: e.activation(out=RINV.t[:, :], in_=RINV.t[:, :], func=AF.Exp, scale=-0.5, bias=lb),
                          reads=[RINV], writes=[RINV])
                        yield
                        E("dve", lambda e, c=c, dst=dst: e.tensor_tensor(out=dst.t[:, c % 4, :], in0=dst.t[:, c % 4, :], in1=RINV.t[:, :], op=OP.mult),
                          reads=[dst, RINV], writes=[dst])
                        yield
                    yield
                def thrRope():
                    def proj_fm(c0):
                        pm = bank("rmm")
                        for kc in range(8):
                            E("pe", lambda e, kc=kc, pm=pm: e.matmul(pm.t[:, :], lhsT=WIN.t[:, kc, c0:c0 + 128], rhs=HT.t[:, kc, :],
                                                                     start=(kc == 0), stop=(kc == 7)),
                              reads=[wg(c0), HT], writes=[pm])
                        return pm
                    for c in range(8):
                        c0 = 2056 + c * 128
                        pm = proj_fm(c0)
                        E("act", lambda e, pm=pm: e.activation(out=TBF.t[:, :], in_=pm.t[:, :], func=AF.Copy), reads=[pm], writes=[TBF])
                        yield
                        pc = bank("rcv")
                        E("pe", lambda e, pc=pc: e.matmul(pc.t[:, :], lhsT=PMB, rhs=TBF.t[:, :], start=True, stop=True), reads=[CBF, TBF], writes=[pc])
                        yield
                        E("dve", lambda e, pm=pm: e.tensor_tensor(out=RA.t[:, :], in0=pm.t[:, :], in1=ROPE.t[:, 0, :], op=OP.mult),
                          reads=[pm, ROPE], writes=[RA])
                        yield
                        E("dve", lambda e, pc=pc: e.tensor_tensor(out=RB.t[:, :], in0=pc.t[:, :], in1=ROPE.t[:, 1, :], op=OP.mult),
                          reads=[pc, ROPE], writes=[RB])
                        yield
                        dst = QTB if c < 4 else KTB
                        E("pool", lambda e, c=c, dst=dst: e.tensor_tensor(out=dst.t[:, c % 4, :], in0=RA.t[:, :], in1=RB.t[:, :], op=OP.add),
                          reads=[RA, RB], writes=[dst])
                        yield

                    yield
                def thrScal():
                    ba3 = BA.t[:, :, :]
                    b16 = v3(BETA.t[:, :], TPB)
                    g16 = v3(GG.t[:, :], TPB)
                    t16 = v3(TMPS.t[:, :], TPB)
                    E("act", lambda e: e.activation(out=b16, in_=ba3[:, :, 0:4], func=AF.Exp, scale=-1.0), reads=[BA], writes=[BETA])
                    yield
                    E("dve", lambda e: e.tensor_scalar(out=BETA.t[:, :], in0=BETA.t[:, :], scalar1=1.0, scalar2=None, op0=OP.add), reads=[BETA], writes=[BETA])
                    yield
                    E("dve", lambda e: e.reciprocal(out=BETA.t[:, :], in_=BETA.t[:, :]), reads=[BETA], writes=[BETA])
                    yield
                    E("dve", lambda e: e.tensor_tensor(out=g16, in0=ba3[:, :, 4:8], in1=bcm(ROWS.t[:, 4:8], TPB, 4), op=OP.add),
                      reads=[BA, ROWS], writes=[GG])
                    yield
                    E("act", lambda e: e.activation(out=GG.t[:, :], in_=GG.t[:, :], func=AF.Exp), reads=[GG], writes=[GG])
                    yield
                    E("act", lambda e: e.activation(out=GG.t[:, :], in_=GG.t[:, :], func=AF.Ln, bias=1.0), reads=[GG], writes=[GG])
                    yield
                    E("dve", lambda e: e.tensor_tensor(out=g16, in0=g16, in1=bcm(NEGA.t[:, :], TPB, 4), op=OP.mult), reads=[GG, NEGA], writes=[GG])
                    yield
                    ps = bank("st")
                    E("pe", lambda e, ps=ps: e.matmul(ps.t[:, 0:16], lhsT=cf("ub"), rhs=GG.t[:, :], start=True, stop=True), reads=[CF, GG], writes=[ps])
                    yield
                    E("pe", lambda e, ps=ps: e.matmul(ps.t[:, 16:32], lhsT=cf("bb"), rhs=GG.t[:, :], start=True, stop=True), reads=[CF, GG], writes=[ps])
                    yield
                    for cc in range(2):
                        E("dve", lambda e, cc=cc: e.tensor_scalar(out=GM.t[:, :, cc * 4:(cc + 1) * 4], in0=g16, scalar1=cf("cm")[:, cc:cc + 1],
                                                                  scalar2=None, op0=OP.mult),
                          reads=[GG, CF], writes=[GM])
                        yield
                    E("pe", lambda e, ps=ps: e.matmul(ps.t[:, 32:64], lhsT=cf("ones"), rhs=GM.t[:, :, :].rearrange("p a b -> p (a b)"),
                                                      start=True, stop=True), reads=[CF, GM], writes=[ps])
                    yield
                    E("act", lambda e, ps=ps: e.activation(out=EGC.t[:, :], in_=ps.t[:, 0:16], func=AF.Exp), reads=[ps], writes=[EGC])
                    yield
                    E("dve", lambda e: e.tensor_scalar(out=NEGC.t[:, :], in0=EGC.t[:, :], scalar1=-1.0, scalar2=None, op0=OP.mult), reads=[EGC], writes=[NEGC])
                    yield
                    E("act", lambda e, ps=ps: e.activation(out=TMPS.t[:, :], in_=ps.t[:, 16:32], func=AF.Copy), reads=[ps], writes=[TMPS])
                    yield
                    E("dve", lambda e, ps=ps: e.tensor_tensor(out=TMPS.t[:, :], in0=TMPS.t[:, :], in1=ps.t[:, 0:16], op=OP.subtract),
                      reads=[ps, TMPS], writes=[TMPS])
                    yield
                    E("act", lambda e: e.activation(out=EDEC.t[:, :], in_=TMPS.t[:, :], func=AF.Exp), reads=[TMPS], writes=[EDEC])
                    yield
                    E("act", lambda e, ps=ps: e.activation(out=LASTB.t[:, :], in_=ps.t[:, 32:64], func=AF.Exp), reads=[ps], writes=[LASTB])
                    yield

                    yield
                def dumps23():
                    if b == 0:
                        dump("qta", QTA.t[:, :, :], [128, 4, BLK], BF16, [QTA])
                        dump("kta", KTA.t[:, :, :], [128, 4, BLK], BF16, [KTA])
                        dump("vt", VT.t[:, :, :], [128, 4, BLK], BF16, [VT])
                        dump("qtb", QTB.t[:, :, :], [128, 4, BLK], BF16, [QTB])
                        dump("ktb", KTB.t[:, :, :], [128, 4, BLK], BF16, [KTB])
                        dump("sg0", SG[0].t[:, :], [128, D], BF16, [SG[0]])
                        dump("ba", BA.t[:, :, :], [128, TPB, 8], F32, [BA])
                    if b == 0:
                        dump("beta", BETA.t[:, :], [128, 16], F32, [BETA])
                        dump("gg", GG.t[:, :], [128, 16], F32, [GG])
                        dump("egc", EGC.t[:, :], [128, 16], F32, [EGC])
                        dump("edec", EDEC.t[:, :], [128, 16], F32, [EDEC])
                        dump("lastb", LASTB.t[:, :], [128, 32], F32, [LASTB])
                    pass
                def tok_major(tl, trb):
                    cs = slice(tl * 128, (tl + 1) * 128)
                    pt = bank(trb)
                    for h in range(4):
                        E("pe", lambda e, h=h, pt=pt, cs=cs: e.transpose(out=bfv(pt)[:, h * 128:(h + 1) * 128], in_=KTA.t[:, h, cs], identity=IDB),
                          reads=[KTA, CBF], writes=[pt])
                    E("dve", lambda e, pt=pt, tl=tl: e.tensor_tensor(out=v3(KDEC[tl].t[:, :], 4), in0=v3(bfv(pt)[:, 0:512], 4),
                                                                     in1=bc3(EDEC.t[:, tl * 4:(tl + 1) * 4], 4, 128), op=OP.mult),
                      reads=[pt, EDEC], writes=[KDEC[tl]])
                    pt = bank(trb)
                    for h in range(4):
                        E("pe", lambda e, h=h, pt=pt, cs=cs: e.transpose(out=bfv(pt)[:, h * 128:(h + 1) * 128], in_=VT.t[:, h, cs], identity=IDB),
                          reads=[VT, CBF], writes=[pt])
                    E("act", lambda e, pt=pt, tl=tl: e.activation(out=VA[tl].t[:, :], in_=bfv(pt)[:, 0:512], func=AF.Copy), reads=[pt], writes=[VA[tl]])

                def tok_major_s(tl):
                    cs = slice(tl * 128, (tl + 1) * 128)
                    pm = bank("st")
                    for kc in range(8):
                        E("pe", lambda e, kc=kc, pm=pm: e.matmul(pm.t[:, :], lhsT=HT.t[:, kc, cs], rhs=WIN.t[:, kc, 3080:3592], start=(kc == 0), stop=(kc == 7)),
                          reads=[wg(3080), HT], writes=[pm])
                    E("act", lambda e, pm=pm: e.activation(out=VB[tl].t[:, :], in_=pm.t[:, :], func=AF.Copy), reads=[pm], writes=[VB[tl]])
                    pt = bank("o")
                    for h in range(4):
                        E("pe", lambda e, h=h, pt=pt, cs=cs: e.transpose(out=bfv(pt)[:, h * 128:(h + 1) * 128], in_=KTB.t[:, h, cs], identity=IDB),
                          reads=[KTB, CBF], writes=[pt])
                    E("dve", lambda e, pt=pt, tl=tl: e.tensor_tensor(out=v3(KDECB[tl].t[:, :], 4), in0=v3(bfv(pt)[:, 0:512], 4),
                                                                     in1=bc3(cf("kdsc"), 4, 128), op=OP.mult),
                      reads=[pt, CF], writes=[KDECB[tl]])


                def thrN(tl, ns=0):
                    t = b * TPB + tl
                    cs = slice(tl * 128, (tl + 1) * 128)
                    sc = slice(tl * 4, (tl + 1) * 4)
                    hc = [slice(h * 128, (h + 1) * 128) for h in range(4)]
                    RR, DECT, WW, TT, NN, YY = NSETS[ns]
                    dnb, nmb = NBANKS[ns]
                    tok_major(tl, dnb)
                    yield
                    E("pool", lambda e, sc=sc: e.tensor_tensor(out=v3(RR.t[:, :], 4), in0=bcm(cf("ub"), 4, 128), in1=bc3(GG.t[:, sc], 4, 128), op=OP.mult),
                      reads=[CF, GG], writes=[RR])
                    yield
                    pd = bank(dnb)
                    E("pe", lambda e, pd=pd: e.matmul(pd.t[:, :], lhsT=cf("slb"), rhs=RR.t[:, :], start=True, stop=False), reads=[CF, RR], writes=[pd])
                    yield
                    E("pe", lambda e, pd=pd: e.matmul(pd.t[:, :], lhsT=IDF, rhs=cf("negm4"), start=False, stop=True), reads=[CF], writes=[pd])
                    yield
                    E("act", lambda e, pd=pd: e.activation(out=DECT.t[:, :], in_=pd.t[:, :], func=AF.Exp), reads=[pd], writes=[DECT])
                    yield
                    E("pool", lambda e: e.tensor_tensor(out=v3(WW.t[:, :], 4), in0=v3(DECT.t[:, :], 4), in1=bcm(cf("strn4"), 4, 128), op=OP.mult), reads=[DECT, CF], writes=[WW])
                    yield
                    E("pool", lambda e, sc=sc: e.tensor_tensor(out=v3(WW.t[:, :], 4), in0=v3(WW.t[:, :], 4), in1=bc3(BETA.t[:, sc], 4, 128), op=OP.mult),
                      reads=[WW, BETA], writes=[WW])
                    yield
                    pk = bank(dnb)
                    for h in range(4):
                        E("pe", lambda e, h=h, pk=pk: e.matmul(pk.t[:, hc[h]], lhsT=KTA.t[:, h, cs], rhs=KTA.t[:, h, cs], start=True, stop=True),
                          reads=[KTA], writes=[pk])
                        yield
                    E("dve", lambda e, pk=pk: e.tensor_tensor(out=TT[0].t[:, :], in0=pk.t[:, :], in1=WW.t[:, :], op=OP.mult), reads=[pk, WW], writes=[TT[0]])
                    yield
                    pq = bank(dnb)
                    for h in range(4):
                        E("pe", lambda e, h=h, pq=pq: e.matmul(pq.t[:, hc[h]], lhsT=KTA.t[:, h, cs], rhs=QTA.t[:, h, cs], start=True, stop=True),
                          reads=[KTA, QTA], writes=[pq])
                        yield
                    E("dve", lambda e, pq=pq: e.tensor_tensor(out=ATTL[tl].t[:, :], in0=pq.t[:, :], in1=DECT.t[:, :], op=OP.mult), reads=[pq, DECT], writes=[ATTL[tl]])
                    yield
                    pt = bank(dnb)
                    for h in range(4):
                        E("pe", lambda e, h=h, pt=pt: e.transpose(out=bfv(pt)[:, hc[h]], in_=TT[0].t[:, hc[h]], identity=IDB), reads=[TT[0], CBF], writes=[pt])
                        yield
                    E("act", lambda e, pt=pt: e.activation(out=NN[0].t[:, :], in_=bfv(pt)[:, 0:512], func=AF.Copy), reads=[pt], writes=[NN[0]])
                    yield
                    E("pool", lambda e: e.tensor_tensor(out=v3(YY[0].t[:, :], 4), in0=v3(TT[0].t[:, :], 4), in1=bcm(IDF, 4, 128), op=OP.add), reads=[TT[0], CF], writes=[YY[0]])
                    yield
                    for p in range(5):
                        Tp, Np, Tn, Nn = TT[p % 2], NN[p % 2], TT[(p + 1) % 2], NN[(p + 1) % 2]
                        Yp, Yn = YY[p % 2], (YY[(p + 1) % 2] if p < 4 else YFL[tl])
                        pn = bank(nmb)
                        for h in range(4):
                            E("pe", lambda e, h=h, pn=pn, Tp=Tp, Np=Np: e.matmul(pn.t[:, hc[h]], lhsT=Tp.t[:, hc[h]], rhs=Np.t[:, hc[h]], start=True, stop=True),
                              reads=[Tp, Np], writes=[pn])
                            yield
                        if p < 4:
                            pt2 = bank(dnb)
                            for h in range(4):
                                E("pe", lambda e, h=h, pt2=pt2, Tp=Tp, Np=Np: e.matmul(pt2.t[:, hc[h]], lhsT=Np.t[:, hc[h]], rhs=Tp.t[:, hc[h]], start=True, stop=True),
                                  reads=[Tp, Np], writes=[pt2])
                                yield
                        E("act", lambda e, pn=pn, Nn=Nn: e.activation(out=Nn.t[:, :], in_=pn.t[:, :], func=AF.Copy), reads=[pn], writes=[Nn])
                        yield
                        if p < 4:
                            E("dve", lambda e, pt2=pt2, Tn=Tn: e.tensor_copy(out=Tn.t[:, :], in_=pt2.t[:, :]), reads=[pt2], writes=[Tn])
                            yield
                        py = bank(nmb)
                        for h in range(4):
                            E("pe", lambda e, h=h, py=py, Nn=Nn, Yp=Yp: e.matmul(py.t[:, hc[h]], lhsT=Nn.t[:, hc[h]], rhs=Yp.t[:, hc[h]], start=True, stop=True),
                              reads=[Nn, Yp], writes=[py])
                            yield
                        E("dve", lambda e, py=py, Yp=Yp, Yn=Yn: e.tensor_tensor(out=Yn.t[:, :], in0=py.t[:, :], in1=Yp.t[:, :], op=OP.add),
                          reads=[py, Yp], writes=[Yn])
                        yield
                    YF = YFL[tl]

                    if t == 0:
                        dump("dect", DECT.t[:, :], [128, 512], F32, [DECT])
                        dump("att", ATTL[tl].t[:, :], [128, 512], BF16, [ATTL[tl]])
                        dump("yf", YF.t[:, :], [128, 512], BF16, [YF])
                        dump("vb", VB[0].t[:, :], [128, 512], BF16, [VB[0]])
                        dump("va", VA[0].t[:, :], [128, 512], BF16, [VA[0]])
                        dump("kdec", KDEC[0].t[:, :], [128, 512], BF16, [KDEC[0]])
                        dump("kdecb", KDECB[0].t[:, :], [128, 512], BF16, [KDECB[0]])
                    yield
                def thrS(tl):
                    t = b * TPB + tl
                    cs = slice(tl * 128, (tl + 1) * 128)
                    sc = slice(tl * 4, (tl + 1) * 4)
                    hc = [slice(h * 128, (h + 1) * 128) for h in range(4)]
                    precast(2 * t)
                    precast(2 * t + 1)
                    yield
                    tok_major_s(tl)
                    yield
                    for c in range(2):
                        rw = slice(c * 64, (c + 1) * 64)
                        pks = bank("st")
                        for h in range(4):
                            E("pe", lambda e, h=h, pks=pks: e.matmul(pks.t[:, hc[h]], lhsT=KTA.t[:, h, cs], rhs=SAB.t[:, hc[h]], start=True, stop=True),
                              reads=[KTA, SAB], writes=[pks])
                            yield
                        pqs = bank("o")
                        for h in range(4):
                            E("pe", lambda e, h=h, pqs=pqs: e.matmul(pqs.t[:, hc[h]], lhsT=QTA.t[:, h, cs], rhs=SAB.t[:, hc[h]], start=True, stop=True),
                              reads=[QTA, SAB], writes=[pqs])
                            yield
                        E("dve", lambda e, pks=pks, rw=rw, sc=sc: e.tensor_tensor(out=v3(TMPZ.t[rw, :], 4), in0=v3(pks.t[rw, :], 4),
                                                                                  in1=bc3(NEGC.t[rw, sc], 4, 128), op=OP.mult),
                          reads=[pks, NEGC], writes=[TMPZ])
                        yield
                        E("dve", lambda e, rw=rw, tl=tl: e.tensor_tensor(out=ZZ.t[rw, :], in0=TMPZ.t[rw, :], in1=VA[tl].t[rw, :], op=OP.add),
                          reads=[TMPZ, VA[tl]], writes=[ZZ])
                        yield
                        E("dve", lambda e, pqs=pqs, rw=rw, sc=sc: e.tensor_tensor(out=v3(OALLL[tl].t[rw, 0:512], 4), in0=v3(pqs.t[rw, :], 4),
                                                                                  in1=bc3(EGC.t[rw, sc], 4, 128), op=OP.mult),
                          reads=[pqs, EGC], writes=[OALLL[tl]])
                        yield
                        pxz = bank("st")
                        for h in range(4):
                            E("pe", lambda e, h=h, pxz=pxz, rw=rw: e.matmul(pxz.t[:, hc[h]], lhsT=YFL[tl].t[rw, hc[h]], rhs=ZZ.t[rw, hc[h]], start=True, stop=True),
                              reads=[YFL[tl], ZZ], writes=[pxz])
                            yield
                        E("dve", lambda e, pxz=pxz, rw=rw, sc=sc: e.tensor_tensor(out=v3(VN.t[rw, :], 4), in0=v3(pxz.t[rw, :], 4),
                                                                                  in1=bc3(BETA.t[rw, sc], 4, 128), op=OP.mult),
                          reads=[pxz, BETA], writes=[VN])
                        yield
                        pkv = bank("st")
                        for h in range(4):
                            E("pe", lambda e, h=h, pkv=pkv, rw=rw, tl=tl: e.matmul(pkv.t[:, hc[h]], lhsT=KDEC[tl].t[rw, hc[h]], rhs=VN.t[rw, hc[h]],
                                                                                  start=True, stop=True),
                              reads=[KDEC[tl], VN], writes=[pkv])
                            yield
                        for h in range(4):
                            li = tl * 8 + c * 4 + h
                            E("dve", lambda e, h=h, pkv=pkv, li=li: e.scalar_tensor_tensor(out=SA.t[:, hc[h]], in0=SA.t[:, hc[h]], scalar=LASTB.t[:, li:li + 1],
                                                                                           in1=pkv.t[:, hc[h]], op0=OP.mult, op1=OP.add),
                              reads=[SA, LASTB, pkv], writes=[SA])
                            yield
                        E("act", lambda e: e.activation(out=SAB.t[:, :], in_=SA.t[:, :], func=AF.Copy), reads=[SA], writes=[SAB])
                        yield
                    pav = bank("o")
                    for h in range(4):
                        E("pe", lambda e, h=h, pav=pav: e.matmul(pav.t[:, hc[h]], lhsT=ATTL[tl].t[:, hc[h]], rhs=VN.t[:, hc[h]], start=True, stop=True),
                          reads=[ATTL[tl], VN], writes=[pav])
                        yield
                    E("dve", lambda e, pav=pav: e.tensor_tensor(out=OALLL[tl].t[:, 0:512], in0=pav.t[:, :], in1=OALLL[tl].t[:, 0:512], op=OP.add),
                      reads=[pav, OALLL[tl]], writes=[OALLL[tl]])
                    yield

                    pq = bank("st")
                    for h in range(4):
                        E("pe", lambda e, h=h, pq=pq: e.matmul(pq.t[:, hc[h]], lhsT=KTB.t[:, h, cs], rhs=QTB.t[:, h, cs], start=True, stop=True),
                          reads=[KTB, QTB], writes=[pq])
                        yield
                    E("dve", lambda e, pq=pq: e.tensor_tensor(out=ATB.t[:, :], in0=pq.t[:, :], in1=cf("e4"), op=OP.mult), reads=[pq, CF], writes=[ATB])
                    yield
                    pob = bank("o")
                    for h in range(4):
                        E("pe", lambda e, h=h, pob=pob: e.matmul(pob.t[:, hc[h]], lhsT=QTB.t[:, h, cs], rhs=SBB.t[:, hc[h]], start=True, stop=False),
                          reads=[QTB, SBB], writes=[pob])
                        yield
                        E("pe", lambda e, h=h, pob=pob, tl=tl: e.matmul(pob.t[:, hc[h]], lhsT=ATB.t[:, hc[h]], rhs=VB[tl].t[:, hc[h]], start=False, stop=True),
                          reads=[ATB, VB[tl]], writes=[pob])
                        yield
                    E("dve", lambda e, pob=pob: e.tensor_tensor(out=v3(OALLL[tl].t[:, 512:1024], 4), in0=v3(pob.t[:, :], 4), in1=bc3(cf("osc"), 4, 128), op=OP.mult),
                      reads=[pob, CF], writes=[OALLL[tl]])
                    yield
                    psb = bank("st")
                    for h in range(4):
                        E("pe", lambda e, h=h, psb=psb, tl=tl: e.matmul(psb.t[:, hc[h]], lhsT=KDECB[tl].t[:, hc[h]], rhs=VB[tl].t[:, hc[h]], start=True, stop=True),
                          reads=[KDECB[tl], VB[tl]], writes=[psb])
                        yield
                    for h in range(4):
                        E("dve", lambda e, h=h, psb=psb: e.scalar_tensor_tensor(out=SBS.t[:, hc[h]], in0=SBS.t[:, hc[h]], scalar=float(GAM[h] ** 128),
                                                                                in1=psb.t[:, hc[h]], op0=OP.mult, op1=OP.add),
                          reads=[SBS, psb], writes=[SBS])
                        yield
                    E("act", lambda e: e.activation(out=SBB.t[:, :], in_=SBS.t[:, :], func=AF.Copy), reads=[SBS], writes=[SBB])
                    yield

                    if debug:
                        E("sp", lambda e, t=t: e.dma_start(out=dbg_o[t * 128:(t + 1) * 128, :], in_=OALLL[tl].t[:, :]), reads=[OALLL[tl]], dma=dbgsem)
                        yield

                    yield
                def thrG(tl, b=b, gb="g"):
                    t = b * TPB + tl
                    cs = slice(tl * 128, (tl + 1) * 128)
                    sc = slice(tl * 4, (tl + 1) * 4)
                    hc = [slice(h * 128, (h + 1) * 128) for h in range(4)]
                    E("sp", lambda e, t=t: e.dma_start(out=X1.t[:, :], in_=x[t * 128:(t + 1) * 128, :]), writes=[X1], dma=xrsem)
                    yield
                    E("pool", lambda e: e.tensor_tensor(out=OSQ.t[:, :], in0=OALLL[tl].t[:, :], in1=OALLL[tl].t[:, :], op=OP.mult), reads=[OALLL[tl]], writes=[OSQ])
                    yield
                    E("dve", lambda e: e.tensor_reduce(out=RSTD8.t[:, :], in_=v3(OSQ.t[:, :], 8), axis=AX.X, op=OP.add), reads=[OSQ], writes=[RSTD8])
                    yield
                    rstd_from_ss(RSTD8.t[:, :], RSTD8.t[:, :], 128, [RSTD8], [RSTD8])
                    E("dve", lambda e: e.tensor_tensor(out=v3(OSQ.t[:, :], 8), in0=v3(OALLL[tl].t[:, :], 8), in1=bc3(RSTD8.t[:, :], 8, 128), op=OP.mult),
                      reads=[OALLL[tl], RSTD8], writes=[OSQ])
                    yield
                    E("dve", lambda e, tl=tl: e.tensor_tensor(out=MIX.t[:, :], in0=OSQ.t[:, :], in1=SG[tl].t[:, :], op=OP.mult),
                      reads=[OSQ, SG[tl]], writes=[MIX])
                    yield
                    pt = bank(gb)
                    for kc in range(8):
                        E("pe", lambda e, kc=kc, pt=pt: e.transpose(out=bfv(pt)[:, kc * 128:(kc + 1) * 128], in_=MIX.t[:, kc * 128:(kc + 1) * 128], identity=IDB),
                          reads=[MIX, CBF], writes=[pt])
                        yield
                    E("act", lambda e, pt=pt: e.activation(out=MIXT.t[:, :, :], in_=v3(bfv(pt)[:, :], 8), func=AF.Copy), reads=[pt], writes=[MIXT])
                    yield
                    for half in range(2):
                        pm = bank(gb)
                        for kc in range(8):
                            E("pe", lambda e, kc=kc, pm=pm, half=half: e.matmul(pm.t[:, :], lhsT=MIXT.t[:, kc, :], rhs=WOUT.t[:, kc, half * 512:(half + 1) * 512],
                                                                               start=(kc == 0), stop=(kc == 7)),
                              reads=[MIXT, WOUT], writes=[pm])
                            yield
                        E("dve", lambda e, pm=pm, half=half, tl=tl: e.tensor_tensor(out=X1.t[:, half * 512:(half + 1) * 512], in0=pm.t[:, :],
                                                                                   in1=X1.t[:, half * 512:(half + 1) * 512], op=OP.add),
                          reads=[pm, X1], writes=[X1])
                        yield
                    E("sp", lambda e, t=t: e.dma_start(out=x1_scr[t * 128:(t + 1) * 128, :], in_=X1.t[:, :]), reads=[X1], dma=x1sem)
                    yield
                    if t == 0:
                        dump("mix", MIX.t[:, :], [128, D], BF16, [MIX])
                    E("pool", lambda e: e.memset(SSG.t[:, 2:3], 0.0), writes=[SSG])
                    yield
                    E("act", lambda e: e.activation(out=H2B.t[:, :], in_=X1.t[:, :], func=AF.Square, accum_out=SSG.t[:, 2:3]),
                      reads=[X1, SSG], writes=[H2B, SSG])
                    yield
                    rstd_from_ss(SSG.t[:, 2:3], SSG.t[:, 3:4], D, [SSG], [SSG])
                    E("act", lambda e: e.activation(out=H2B.t[:, :], in_=X1.t[:, :], func=AF.Copy, scale=SSG.t[:, 3:4]), reads=[X1, SSG], writes=[H2B])
                    yield
                    for half in range(2):
                        pc = bank(gb)
                        for q in range(4):
                            kc = half * 4 + q
                            E("pe", lambda e, kc=kc, q=q, pc=pc: e.transpose(out=pc.t[:, q * 128:(q + 1) * 128], in_=X1.t[:, kc * 128:(kc + 1) * 128], identity=IDF),
                              reads=[X1, CF], writes=[pc])
                            yield
                        E("act", lambda e, pc=pc, half=half: e.activation(out=H2T.t[:, half * 4:(half + 1) * 4, :], in_=v3(pc.t[:, :], 4), func=AF.Copy),
                          reads=[pc], writes=[H2T])
                        yield
                    pl = bank(gb)
                    for kc in range(8):
                        E("pe", lambda e, kc=kc, pl=pl: e.matmul(pl.t[:, 0:72], lhsT=H2T.t[:, kc, :], rhs=W_R.t[:, kc * 72:(kc + 1) * 72],
                                                                 start=(kc == 0), stop=(kc == 7)),
                          reads=[H2T, W_R], writes=[pl])
                        yield
                    E("dve", lambda e, pl=pl: e.tensor_scalar(out=LG.t[:, :], in0=pl.t[:, 0:72], scalar1=SSG.t[:, 3:4], scalar2=None, op0=OP.mult), reads=[pl, SSG], writes=[LG])
                    yield
                    R = RT.t
                    E("dve", lambda e: e.tensor_reduce(out=R[:, 0:1], in_=LG.t[:, 0:8], axis=AX.X, op=OP.max), reads=[LG], writes=[RT])
                    yield
                    E("dve", lambda e: e.tensor_scalar(out=GMASK.t[:, :], in0=LG.t[:, 0:8], scalar1=R[:, 0:1], scalar2=None, op0=OP.is_equal),
                      reads=[LG, RT], writes=[GMASK])
                    yield
                    E("dve", lambda e: e.tensor_scalar(out=R[:, 1:2], in0=R[:, 0:1], scalar1=-1.0, scalar2=None, op0=OP.mult), reads=[RT], writes=[RT])
                    yield
                    E("pool", lambda e: e.memset(R[:, 2:3], 0.0), writes=[RT])
                    yield
                    E("act", lambda e: e.activation(out=PEN.t[:, :], in_=LG.t[:, 0:8], func=AF.Exp, bias=R[:, 1:2], accum_out=R[:, 2:3]),
                      reads=[LG, RT], writes=[PEN, RT])
                    yield
                    E("dve", lambda e: e.reciprocal(out=R[:, 3:4], in_=R[:, 2:3]), reads=[RT], writes=[RT])
                    yield
                    E("dve", lambda e: e.tensor_scalar(out=PEN.t[:, :], in0=GMASK.t[:, :], scalar1=1e30, scalar2=-1e30, op0=OP.mult, op1=OP.add),
                      reads=[GMASK], writes=[PEN])
                    yield
                    E("dve", lambda e: e.tensor_tensor(out=v3(EL.t[:, :], 8), in0=v3(LG.t[:, 8:72], 8), in1=bc3(PEN.t[:, :], 8, 8), op=OP.add),
                      reads=[LG, PEN], writes=[EL])
                    yield
                    E("dve", lambda e: e.tensor_reduce(out=R[:, 4:5], in_=EL.t[:, :], axis=AX.X, op=OP.max), reads=[EL], writes=[RT])
                    yield
                    E("dve", lambda e: e.tensor_scalar(out=OH1.t[:, :], in0=EL.t[:, :], scalar1=R[:, 4:5], scalar2=None, op0=OP.is_equal),
                      reads=[EL, RT], writes=[OH1])
                    yield
                    E("dve", lambda e: e.scalar_tensor_tensor(out=EL2.t[:, :], in0=OH1.t[:, :], scalar=-1e30, in1=EL.t[:, :], op0=OP.mult, op1=OP.add),
                      reads=[OH1, EL], writes=[EL2])
                    yield
                    E("dve", lambda e: e.tensor_reduce(out=R[:, 5:6], in_=EL2.t[:, :], axis=AX.X, op=OP.max), reads=[EL2], writes=[RT])
                    yield
                    E("dve", lambda e: e.tensor_scalar(out=OH2.t[:, :], in0=EL2.t[:, :], scalar1=R[:, 5:6], scalar2=None, op0=OP.is_equal),
                      reads=[EL2, RT], writes=[OH2])
                    yield
                    E("dve", lambda e: e.tensor_tensor(out=R[:, 6:7], in0=R[:, 5:6], in1=R[:, 4:5], op=OP.subtract), reads=[RT], writes=[RT])
                    yield
                    E("act", lambda e: e.activation(out=R[:, 7:8], in_=R[:, 6:7], func=AF.Exp), reads=[RT], writes=[RT])
                    yield
                    E("dve", lambda e: e.tensor_scalar(out=R[:, 8:9], in0=R[:, 7:8], scalar1=1.0, scalar2=None, op0=OP.add), reads=[RT], writes=[RT])
                    yield
                    E("dve", lambda e: e.reciprocal(out=R[:, 9:10], in_=R[:, 8:9]), reads=[RT], writes=[RT])
                    yield
                    E("dve", lambda e: e.tensor_tensor(out=R[:, 10:11], in0=R[:, 7:8], in1=R[:, 9:10], op=OP.mult), reads=[RT], writes=[RT])
                    yield
                    E("dve", lambda e, t=t: e.tensor_scalar(out=CW.t[:, 2 * t:2 * t + 2], in0=R[:, 9:11], scalar1=R[:, 3:4], scalar2=None, op0=OP.mult),
                      reads=[RT], writes=[CW])
                    yield
                    if t == 0:
                        dump("lg", LG.t[:, :], [128, 72], F32, [LG])
                        dump("rt", RT.t[:, :], [128, 16], F32, [RT])
                    E("dve", lambda e: e.tensor_tensor(out=OH12.t[:, :], in0=OH1.t[:, :], in1=OH2.t[:, :], op=OP.add), reads=[OH1, OH2], writes=[OH12])
                    yield
                    pcn = bank(gb)
                    E("pe", lambda e, pcn=pcn: e.matmul(pcn.t[:, 0:64], lhsT=SUB, rhs=OH12.t[:, :], start=True, stop=True), reads=[CBF, OH12], writes=[pcn])
                    yield
                    E("pe", lambda e, pcn=pcn: e.matmul(pcn.t[:, 64:128], lhsT=ONB, rhs=OH12.t[:, :], start=True, stop=True), reads=[CBF, OH12], writes=[pcn])
                    yield
                    E("dve", lambda e, pcn=pcn: e.tensor_tensor(out=POSM.t[:, :], in0=pcn.t[:, 0:64], in1=BASECAP.t[:, :], op=OP.add),
                      reads=[pcn, BASECAP], writes=[POSM])
                    yield
                    E("dve", lambda e, pcn=pcn: e.tensor_tensor(out=BASECAP.t[:, :], in0=pcn.t[:, 64:128], in1=BASECAP.t[:, :], op=OP.add),
                      reads=[pcn, BASECAP], writes=[BASECAP])
                    yield
                    for k, oh in enumerate((OH1, OH2)):
                        E("dve", lambda e, oh=oh: e.tensor_tensor(out=PRD.t[:, :], in0=oh.t[:, :], in1=POSM.t[:, :], op=OP.mult), reads=[oh, POSM], writes=[PRD])
                        yield
                        E("dve", lambda e, k=k: e.tensor_reduce(out=OFF_F.t[:, k:k + 1], in_=PRD.t[:, :], axis=AX.X, op=OP.add), reads=[PRD], writes=[OFF_F])
                        yield
                    E("dve", lambda e, t=t: e.tensor_copy(out=OFFS.t[:, 2 * t:2 * t + 2], in_=OFF_F.t[:, :]), reads=[OFF_F], writes=[OFFS])
                    yield
                    for k in range(2):
                        E("pool", lambda e, t=t, k=k: e.indirect_dma_start(out=xs_all[:, :], out_offset=bass.IndirectOffsetOnAxis(ap=OFFS.t[:, 2 * t + k:2 * t + k + 1], axis=0),
                                                                          in_=H2B.t[:, :], in_offset=None),
                          reads=[H2B, OFFS, XSB], dma=scsem)
                        yield
                    yield
                if b == 0:
                    E("sp", lambda e: e.dma_start(out=ROPE.t[:, :, :], in_=rope_d[:, :, 0:BLK]), writes=[ROPE], dma=rsem)
                run_rr([thrP1(), pendG[0]])
                if b > 0:
                    E("sp", lambda e, b=b: e.dma_start(out=ROPE.t[:, :, :], in_=rope_d[:, :, b * BLK:(b + 1) * BLK]), writes=[ROPE], dma=rsem)
                run_rr([thrL(0, SQ, RINV, "cv"), thrL(1, SQ_B, RINV_B, "tr"), thrScal()])
                dumps23()
                n0 = thrN(0, 0); n1 = thrN(1, 1); rp = thrRope(); s0 = thrS(0)
                nA = thrN(2, 0)
                nB = thrN(3, 1)
                run_rr([s0, rp, n0, n1, nA], after={s0: [n0, rp], nA: [n0]}, stop_on=(0,), must_finish=(1, 2, 3))
                run_rr([thrS(1), nA, nB, thrG(0)], stop_on=(0,), must_finish=(1, 3))
                run_rr([thrS(2), nB, thrG(1)])
                if b + 1 < NB:
                    ldx_blk(b + 1, 0)
                    ldx_blk(b + 1, 1)
                run_rr([thrS(3), thrG(2)])
                pendG[0] = thrG(TPB - 1, gb="g2")
            run_rr([pendG[0]])
            if debug:
                offs_d = dram("offs_d", [128, NT * 2], I32, "ExternalOutput")
                cw_d = dram("cw_d", [128, NT * 2], F32, "ExternalOutput")
                E("sp", lambda e: e.dma_start(out=offs_d, in_=OFFS.t[:, :]), reads=[OFFS], dma=fw.dsem())
                E("sp", lambda e: e.dma_start(out=cw_d, in_=CW.t[:, :]), reads=[CW], dma=fw.dsem())
            fw.barrier()

        with ExitStack() as p2:
            def sb2(name, shape, ty):
                return sb(name, shape, ty, p2)
            NWB = 6
            WGU = [sb2("WGU%d" % i, [128, 4096], BF16) for i in range(NWB)]
            WD = [sb2("WD%d" % i, [128, 2048], BF16) for i in range(NWB)]
            wsm = [fw.dsem() for _ in range(NWB)]
            wsm2 = [fw.dsem() for _ in range(NWB)]
            XS = [sb2("XS%d" % i, [128, 2, D], BF16) for i in range(4)]
            xsm = [fw.dsem() for _ in range(4)]
            XST = [sb2("XST%d" % i, [128, 8, CAP], BF16) for i in range(2)]
            GS = [sb2("GS%d" % i, [128, CAP], F32) for i in range(2)]
            ACTT = [sb2("ACTT%d" % i, [128, 2, CAP], BF16) for i in range(2)]
            YS = [sb2("YS%d" % i, [128, 2, D], BF16) for i in range(2)]
            ysm = [fw.dsem() for _ in range(2)]
            YAB = Buf("y_all")
            xs_e = xs_all.rearrange("(e r p) d -> e p r d", e=NE, p=128)
            y_e = y_all.rearrange("(e r p) d -> e p r d", e=NE, p=128)

            def load_w(ex):
                i = ex % NWB
                E("pool", lambda e: e.dma_start(out=WGU[i].t[:, :], in_=wgu_bf[ex]), writes=[WGU[i]], dma=wsm[i])
                E("pool", lambda e: e.dma_start(out=WD[i].t[:, :], in_=wd_bf[ex]), writes=[WD[i]], dma=wsm2[i])

            def ldxs(ex):
                j4 = ex % 4
                E("sp", lambda e: e.dma_start(out=XS[j4].t[:, :, :], in_=xs_e[ex]), reads=[XSB], writes=[XS[j4]], dma=xsm[j4])

            def stA(ex):
                j = ex % 2
                j4 = ex % 4
                for r in range(2):
                    pt = bank("tr2")
                    for kc in range(8):
                        E("pe", lambda e, kc=kc, pt=pt, r=r: e.transpose(out=bfv(pt)[:, kc * 128:(kc + 1) * 128], in_=XS[j4].t[:, r, kc * 128:(kc + 1) * 128], identity=IDB),
                          reads=[XS[j4], CBF], writes=[pt])
                    E("dve", lambda e, pt=pt, r=r: e.tensor_tensor(out=XST[j].t[:, :, r * 128:(r + 1) * 128], in0=v3(bfv(pt)[:, :], 8),
                                                                   in1=bc3(COLS.t[:, 56:64], 8, 128), op=OP.mult),
                      reads=[pt, COLS], writes=[XST[j]])

            def stB(ex):
                i = ex % NWB
                j = ex % 2
                for fc in range(2):
                    pg = bank("mm4")
                    for kc in range(8):
                        E("pe", lambda e, kc=kc, pg=pg, fc=fc: e.matmul(pg.t[:, 0:CAP], lhsT=WGU[i].t[:, kc * 256 + fc * 128:kc * 256 + fc * 128 + 128],
                                                                       rhs=XST[j].t[:, kc, :], start=(kc == 0), stop=(kc == 7)),
                          reads=[WGU[i], XST[j]], writes=[pg])
                    pu = bank("mm4")
                    for kc in range(8):
                        E("pe", lambda e, kc=kc, pu=pu, fc=fc: e.matmul(pu.t[:, 0:CAP], lhsT=WGU[i].t[:, 2048 + kc * 256 + fc * 128:2048 + kc * 256 + fc * 128 + 128],
                                                                       rhs=XST[j].t[:, kc, :], start=(kc == 0), stop=(kc == 7)),
                          reads=[WGU[i], XST[j]], writes=[pu])
                    gs = GS[fc]
                    E("act", lambda e, pg=pg, gs=gs: e.activation(out=gs.t[:, :], in_=pg.t[:, 0:CAP], func=AF.Silu), reads=[pg], writes=[gs])
                    E("dve", lambda e, pu=pu, fc=fc, gs=gs: e.tensor_tensor(out=ACTT[j].t[:, fc, :], in0=pu.t[:, 0:CAP], in1=gs.t[:, :], op=OP.mult),
                      reads=[pu, gs], writes=[ACTT[j]])

            def stC(ex):
                i = ex % NWB
                j = ex % 2
                for r in range(2):
                    for half in range(2):
                        py = bank("dw")
                        for fc in range(2):
                            E("pe", lambda e, fc=fc, py=py, r=r, half=half: e.matmul(py.t[:, :], lhsT=ACTT[j].t[:, fc, r * 128:(r + 1) * 128],
                                                                                    rhs=WD[i].t[:, fc * 1024 + half * 512:fc * 1024 + half * 512 + 512],
                                                                                    start=(fc == 0), stop=(fc == 1)),
                              reads=[ACTT[j], WD[i]], writes=[py])
                        if half == 0:
                            E("act", lambda e, py=py, r=r: e.activation(out=YS[j].t[:, r, 0:512], in_=py.t[:, :], func=AF.Copy), reads=[py], writes=[YS[j]])
                        else:
                            E("dve", lambda e, py=py, r=r: e.tensor_copy(out=YS[j].t[:, r, 512:1024], in_=py.t[:, :]), reads=[py], writes=[YS[j]])
                E("sp", lambda e: e.dma_start(out=y_e[ex], in_=YS[j].t[:, :, :]), reads=[YS[j]], dma=ysm[j])

            for ex in range(4):
                load_w(ex)
            for ex in range(3):
                ldxs(ex)
            for it in range(NE + 2):
                if it < NE:
                    stA(it)
                if it + 3 < NE:
                    ldxs(it + 3)
                if 0 <= it - 1 < NE:
                    stB(it - 1)
                if 0 <= it - 2 < NE:
                    stC(it - 2)
                if it + 4 < NE:
                    load_w(it + 4)
            fw.barrier()

        with ExitStack() as p3:
            def sb3(name, shape, ty):
                return sb(name, shape, ty, p3)
            FIN = sb3("FIN", [128, D], F32)
            fsem = fw.dsem()
            E("sp", lambda e: e.dma_start(out=FIN.t[:, :], in_=bass.AP(tensor=fin_d.tensor, offset=0, ap=[[0, 128], [1, D]])), writes=[FIN], dma=fsem)
            NB3 = 4
            X1L = [sb3("X1L%d" % i, [128, D], F32) for i in range(NB3)]
            Y1 = [sb3("Y1_%d" % i, [128, D], BF16) for i in range(NB3)]
            Y2 = [sb3("Y2_%d" % i, [128, D], BF16) for i in range(NB3)]
            l1 = [fw.dsem() for _ in range(NB3)]
            l2 = [fw.dsem() for _ in range(NB3)]
            l3 = [fw.dsem() for _ in range(NB3)]
            ACC = [sb3("ACC%d" % i, [128, D], F32) for i in range(2)]
            OUTT = [sb3("OUTT%d" % i, [128, D], F32) for i in range(2)]
            osm = [fw.dsem() for _ in range(2)]
            JK = sb3("JK", [128, D], BF16)
            S3 = sb3("S3", [128, 4 * NT], F32)
            E("pool", lambda e: e.memset(S3.t[:, :], 0.0), writes=[S3])

            def loads3(t):
                j = t % NB3
                E("sp", lambda e, t=t, j=j: e.dma_start(out=X1L[j].t[:, :], in_=x1_scr[t * 128:(t + 1) * 128, :]), writes=[X1L[j]], dma=l1[j])
                E("pool", lambda e, t=t, j=j: e.indirect_dma_start(out=Y1[j].t[:, :], out_offset=None, in_=y_all[:, :],
                                                                  in_offset=bass.IndirectOffsetOnAxis(ap=OFFS.t[:, 2 * t:2 * t + 1], axis=0)),
                  reads=[OFFS], writes=[Y1[j]], dma=l2[j])
                E("pool", lambda e, t=t, j=j: e.indirect_dma_start(out=Y2[j].t[:, :], out_offset=None, in_=y_all[:, :],
                                                                  in_offset=bass.IndirectOffsetOnAxis(ap=OFFS.t[:, 2 * t + 1:2 * t + 2], axis=0)),
                  reads=[OFFS], writes=[Y2[j]], dma=l3[j])

            for t in range(min(3, NT)):
                loads3(t)
            for t in range(NT):
                j = t % NB3
                k = t % 2
                E("dve", lambda e, t=t, j=j, k=k: e.scalar_tensor_tensor(out=ACC[k].t[:, :], in0=Y1[j].t[:, :], scalar=CW.t[:, 2 * t:2 * t + 1], in1=X1L[j].t[:, :],
                                                                        op0=OP.mult, op1=OP.add),
                  reads=[Y1[j], CW, X1L[j]], writes=[ACC[k]])
                E("dve", lambda e, t=t, j=j, k=k: e.scalar_tensor_tensor(out=ACC[k].t[:, :], in0=Y2[j].t[:, :], scalar=CW.t[:, 2 * t + 1:2 * t + 2], in1=ACC[k].t[:, :],
                                                                        op0=OP.mult, op1=OP.add),
                  reads=[Y2[j], CW, ACC[k]], writes=[ACC[k]])
                if t + 3 < NT:
                    loads3(t + 3)
                E("act", lambda e, t=t, k=k: e.activation(out=JK.t[:, :], in_=ACC[k].t[:, :], func=AF.Square, accum_out=S3.t[:, 4 * t:4 * t + 1]),
                  reads=[ACC[k], S3], writes=[JK, S3])
                rs = S3.t[:, 4 * t + 1:4 * t + 2]
                E("dve", lambda e, t=t, rs=rs: e.tensor_scalar(out=rs, in0=S3.t[:, 4 * t:4 * t + 1], scalar1=1.0 / D, scalar2=EPS, op0=OP.mult, op1=OP.add),
                  reads=[S3], writes=[S3])
                E("act", lambda e, rs=rs: e.activation(out=rs, in_=rs, func=AF.Ln), reads=[S3], writes=[S3])
                E("act", lambda e, rs=rs: e.activation(out=rs, in_=rs, func=AF.Exp, scale=-0.5), reads=[S3], writes=[S3])
                E("dve", lambda e, k=k, rs=rs: e.scalar_tensor_tensor(out=OUTT[k].t[:, :], in0=ACC[k].t[:, :], scalar=rs, in1=FIN.t[:, :], op0=OP.mult, op1=OP.mult),
                  reads=[ACC[k], S3, FIN], writes=[OUTT[k]])
                E("sp", lambda e, t=t, k=k: e.dma_start(out=out[t * 128:(t + 1) * 128, :], in_=OUTT[k].t[:, :]), reads=[OUTT[k]], dma=osm[k])
        fw.finish()
    nc._dump_names = list(dumps.keys())
    return nc


def _consts():
    f = np.float32
    i = np.arange(128)
    same = (i[:, None] // 64) == (i[None, :] // 64)
    cfm = np.zeros((128, NCF), f)

    def put(name, arr):
        a, b = _cfo[name]
        cfm[:, a:b] = arr
    put("ident", np.eye(128, dtype=f))
    put("ub", ((i[:, None] <= i[None, :]) & same).astype(f))
    put("slb", ((i[:, None] > i[None, :]) & same).astype(f))
    put("bb", same.astype(f))
    put("ones", np.ones((128, 128), f))
    inc = (i[None, :] >= i[:, None]) & same
    strict = (i[None, :] > i[:, None]) & same
    put("negm4", np.tile(np.where(inc, 0.0, -30000.0).astype(f), (1, 4)))
    put("strn4", np.where(strict, -1.0, 0.0).astype(f))
    e4 = np.zeros((128, 512), np.float64)
    kd = np.zeros((128, 4), np.float64)
    osc = np.zeros((128, 4), np.float64)
    for h in range(4):
        g = GAM[h]
        m = (i[None, :] >= i[:, None])
        e4[:, h * 128:(h + 1) * 128] = np.where(m, (128.0 ** -0.5) * g ** (-(i[:, None] + 1.0)), 0.0)
        kd[:, h] = (128.0 ** -0.5) * g ** (127.0 - i)
        osc[:, h] = g ** (i + 1.0)
    put("e4", e4.astype(f))
    put("su", (i[:, None] < i[None, :]).astype(f))
    pm = np.zeros((128, 128), f)
    pm[(i + 64) % 128, i] = 1.0
    put("pm", pm)
    put("cm", np.stack([(i < 64), (i >= 64)], 1).astype(f))
    put("kdsc", kd.astype(f))
    put("osc", osc.astype(f))
    put("iotacap", np.tile((np.arange(NE) * CAP).astype(f)[None, :], (128, 1)))
    pos = np.arange(S, dtype=f)
    inv = (f(10000.0) ** (-(np.arange(0, 128, 2, dtype=f)) / f(128.0))).astype(f)
    ang = (pos[:, None] * inv[None, :]).astype(f)
    cos = np.cos(ang).astype(f).T
    sin = np.sin(ang).astype(f).T
    rope = np.zeros((128, 2, S), f)
    rope[0:64, 0] = cos
    rope[64:128, 0] = cos
    rope[0:64, 1] = -sin
    rope[64:128, 1] = sin
    return cfm, rope


_CACHE = {}


def kernel(x, attn_norm, w_in, conv_a, a_log, dt_bias, norm_a, norm_b, w_out, ffn_norm,
           w_router_group, w_router_expert, w_gate, w_up, w_down, final_norm, _debug=False):
    f = np.float32
    x = np.asarray(x, f)
    w_in_l = np.ascontiguousarray(np.asarray(w_in, f)[0].reshape(8, 128, DIN).transpose(1, 0, 2))
    w_out_l = np.ascontiguousarray(np.asarray(w_out, f)[0].reshape(8, 128, D).transpose(1, 0, 2))
    wr = np.concatenate([np.asarray(w_router_group, f)[0], np.asarray(w_router_expert, f)[0]], axis=1)
    w_r_l = np.ascontiguousarray(wr.reshape(8, 128, 72).transpose(1, 0, 2).reshape(128, 8 * 72))
    wg = np.asarray(w_gate, f)[0].reshape(NE, 8, 128, 256).transpose(0, 2, 1, 3).reshape(NE, 128, 2048)
    wu = np.asarray(w_up, f)[0].reshape(NE, 8, 128, 256).transpose(0, 2, 1, 3).reshape(NE, 128, 2048)
    wgu_l = np.ascontiguousarray(np.concatenate([wg, wu], axis=2))
    wd_l = np.ascontiguousarray(np.asarray(w_down, f)[0].reshape(NE, 2, 128, D).transpose(0, 2, 1, 3).reshape(NE, 128, 2048))
    cols = np.zeros((128, NCOL), f)
    ca = np.asarray(conv_a, f)[0]
    cols[:, 0:48] = ca.reshape(4, 12, 128).transpose(2, 1, 0).reshape(128, 48)
    normfull = np.concatenate([np.tile(np.asarray(norm_a, f)[0], 4), np.asarray(norm_b, f)[0]])
    cols[:, 48:56] = normfull.reshape(8, 128).T
    cols[:, 56:64] = np.asarray(ffn_norm, f)[0].reshape(8, 128).T
    cols[:, 64:72] = np.asarray(attn_norm, f)[0].reshape(8, 128).T
    rows = np.concatenate([np.asarray(a_log, f)[0], np.asarray(dt_bias, f)[0]])[None, :].astype(f)
    fin = np.asarray(final_norm, f)[None, :]
    cfm, rope = _consts()
    key = bool(_debug)
    if key not in _CACHE:
        _CACHE[key] = build(debug=_debug)
    nc = _CACHE[key]
    in_maps = []
    ncores = 1 if _debug else NCORES
    for c in range(ncores):
        in_maps.append({"x": np.ascontiguousarray(x[c]), "w_in": w_in_l, "w_out": w_out_l, "w_r": w_r_l, "wgu": wgu_l, "wd": wd_l,
                        "cols": cols, "rows": rows, "fin": fin, "cf": cfm, "rope": rope})
    res = run_bass_kernel_spmd(nc, in_maps, core_ids=list(range(ncores)))
    outp = np.stack([np.asarray(r["out"], f) for r in res.results], axis=0)
    if _debug:
        kernel.dbg = [{k: np.asarray(r[k]) for k in ["x1_scr", "dbg_o", "offs_d", "cw_d"] + ["dd_" + n for n in nc._dump_names]} for r in res.results]
    return outp
```

```python
import os
import numpy as np
from contextlib import ExitStack
import concourse.bass as bass
import concourse.mybir as mybir
from concourse.bass_utils import run_bass_kernel_spmd

F32 = mybir.dt.float32
BF16 = mybir.dt.bfloat16
I32 = mybir.dt.int32
AF = mybir.ActivationFunctionType
OP = mybir.AluOpType
AX = mybir.AxisListType

S = 4096
D = 1024
NT = 32
NB = 8
TPB = 4
BLK = 512
NE = 64
CAP = 256
DIN = 4104
EPS = 1e-6
NCORES = 8
GAM = [1.0 - 2.0 ** (-5.0 - h) for h in range(4)]

_cfo = {}
_o = 0
for _n, _w in (("ident", 128), ("ub", 128), ("slb", 128), ("bb", 128), ("ones", 128), ("negm4", 512),
               ("strn4", 128), ("e4", 512), ("su", 128), ("pm", 128), ("cm", 2),
               ("kdsc", 4), ("osc", 4), ("iotacap", 64)):
    _cfo[_n] = (_o, _o + _w)
    _o += _w
NCF = _o
NCOL = 72
NROW = 8


import types


def _freeze(fn):
    if fn.__closure__ is None:
        return fn
    cells = []
    for c in fn.__closure__:
        try:
            cells.append(types.CellType(c.cell_contents))
        except ValueError:
            cells.append(c)
    return types.FunctionType(fn.__code__, fn.__globals__, fn.__name__, fn.__defaults__, tuple(cells))


COST = {"pe": 0.12, "act": 0.7, "dve": 0.6, "pool": 1.6, "sp": 0.05}
LAT = 0.25
DMA_LAT = 2.5


class Eng:
    def __init__(s, name, sem):
        s.name = name; s.sem = sem; s.cnt = 0; s.waited = {}; s.prog = []; s.tfree = 0.0


class DSem:
    def __init__(s, sem):
        s.sem = sem; s.val = 0


class Buf:
    def __init__(s, name="", excl=False):
        s.name = name; s.w = None; s.r = {}; s.excl = excl
        s.tw = 0.0; s.tr = 0.0


class TB:
    def __init__(s, t, name=""):
        s.t = t; s.b = Buf(name)


class FW:
    def __init__(s, nc, stack):
        s.nc = nc
        s.stack = stack
        s.E = {}
        for n in ("pe", "act", "dve", "pool", "sp"):
            s.E[n] = Eng(n, stack.enter_context(nc.semaphore("sem_" + n)))
        s.dsems = []

    def dsem(s):
        d = DSem(s.stack.enter_context(s.nc.semaphore("dsem%d" % len(s.dsems))))
        s.dsems.append(d)
        return d

    def est_start(s, en, reads, writes):
        reads = [b.b if isinstance(b, TB) else b for b in reads]
        writes = [b.b if isinstance(b, TB) else b for b in writes]
        t = s.E[en].tfree
        for b in reads:
            t = max(t, (b.tw + LAT) if not b.excl else (max(b.tw, b.tr) + LAT))
        for b in writes:
            t = max(t, max(b.tw, b.tr) + LAT)
        return t

    def emit(s, en, fn, reads=(), writes=(), dma=None, cost=None, frozen=False):
        eng = s.E[en]
        deps = []
        reads = [b.b if isinstance(b, TB) else b for b in reads]
        writes = [b.b if isinstance(b, TB) else b for b in writes]
        t0 = s.est_start(en, reads, writes)
        c = COST[en] if cost is None else cost
        eng.tfree = t0 + c
        tfin = t0 + c + (DMA_LAT if dma is not None else 0.0)
        for b in reads:
            if b.excl:
                b.tw = max(b.tw, tfin)
            else:
                b.tr = max(b.tr, tfin)
        for b in writes:
            b.tw = max(b.tw, tfin); b.tr = 0.0
        writes = writes + [b for b in reads if b.excl]
        reads = [b for b in reads if not b.excl]
        for b in reads:
            if b.w is not None:
                deps.append(b.w)
        for b in writes:
            if b.w is not None:
                deps.append(b.w)
            deps.extend(b.r.values())
        waits = []
        for (sem, val) in deps:
            if sem is eng.sem and en in ("pe", "sp"):
                continue
            k = id(sem)
            if eng.waited.get(k, 0) >= val:
                continue
            eng.waited[k] = val
            waits.append((sem, val))
        if dma is None:
            eng.cnt += 1
            tok = (eng.sem, eng.cnt)
            inc = 1
        else:
            dma.val += 16
            tok = (dma.sem, dma.val)
            inc = 16
        eng.prog.append((waits, fn if frozen else _freeze(fn), tok[0], inc))
        for b in reads:
            if isinstance(b, TB):
                b = b.b
            b.r[id(tok[0])] = tok
        for b in writes:
            if isinstance(b, TB):
                b = b.b
            b.w = tok
            b.r = {}
        return tok

    def barrier(s):
        toks = [(e.sem, e.cnt) for e in s.E.values() if e.cnt > 0]
        toks += [(d.sem, d.val) for d in s.dsems if d.val > 0]
        for en, eng in s.E.items():
            waits = []
            for (sem, val) in toks:
                if sem is eng.sem:
                    continue
                if eng.waited.get(id(sem), 0) >= val:
                    continue
                eng.waited[id(sem)] = val
                waits.append((sem, val))
            if waits:
                eng.cnt += 1
                eng.prog.append((waits, (lambda e: e.nop()), eng.sem, 1))

    def finish(s):
        nc = s.nc
        finals = [(d.sem, d.val) for d in s.dsems if d.val > 0]
        with nc.Block() as block:
            def run(eng, e):
                for (waits, fn, sem, inc) in eng.prog:
                    for (ws, wv) in waits:
                        e.wait_ge(ws, wv)
                    fn(e).then_inc(sem, inc)

            @block.tensor
            def _(e):
                run(s.E["pe"], e)

            @block.scalar
            def _(e):
                run(s.E["act"], e)

            @block.vector
            def _(e):
                run(s.E["dve"], e)

            @block.gpsimd
            def _(e):
                run(s.E["pool"], e)

            @block.sync
            def _(e):
                run(s.E["sp"], e)
                for (ws, wv) in finals:
                    e.wait_ge(ws, wv)


def build(debug=False):
    nc = bass.Bass("TRN2", target_bir_lowering=False)

    def dram(name, shape, ty, kind="ExternalInput"):
        return nc.dram_tensor(name, shape, ty, kind=kind).ap()

    x = dram("x", [S, D], F32)
    w_in = dram("w_in", [128, 8, DIN], F32)
    w_out = dram("w_out", [128, 8, D], F32)
    w_r = dram("w_r", [128, 8 * 72], F32)
    wgu = dram("wgu", [NE, 128, 4096], F32)
    wd = dram("wd", [NE, 128, 2048], F32)
    cols_d = dram("cols", [128, NCOL], F32)
    rows_d = dram("rows", [1, NROW], F32)
    fin_d = dram("fin", [1, D], F32)
    cf_d = dram("cf", [128, NCF], F32)
    rope_d = dram("rope", [128, 2, S], F32)
    out = dram("out", [S, D], F32, "ExternalOutput")
    xs_all = dram("xs_all", [NE * CAP, D], BF16, "Internal")
    y_all = dram("y_all", [NE * CAP, D], BF16, "Internal")
    x1_scr = dram("x1_scr", [S, D], F32, "ExternalOutput" if debug else "Internal")
    wgu_bf = dram("wgu_bf", [NE, 128, 4096], BF16, "Internal")
    wd_bf = dram("wd_bf", [NE, 128, 2048], BF16, "Internal")
    dbg_o = dram("dbg_o", [S, D], F32, "ExternalOutput") if debug else None

    with ExitStack() as st:
        fw = FW(nc, st)
        sink = [None]

        def E(en, fn, reads=(), writes=(), dma=None, cost=None):
            if sink[0] is None:
                fw.emit(en, fn, reads, writes, dma, cost)
            else:
                sink[0].append((en, _freeze(fn), list(reads), list(writes), dma, cost))
        dumps = {}

        def dump(name, ap, shape, ty, rbufs):
            if not debug or name in dumps:
                return
            dumps[name] = dram("dd_" + name, list(shape), ty, "ExternalOutput")
            ds_ = fw.dsem()
            E("sp", lambda e: e.dma_start(out=dumps[name], in_=ap), reads=rbufs, dma=ds_)

        def sb(name, shape, ty, stack=st):
            return TB(stack.enter_context(nc.sbuf_tensor(name, shape, ty)), name)

        PB = [TB(st.enter_context(nc.psum_tensor("pb%d" % i, [128, 512], F32)), "pb%d" % i) for i in range(8)]
        for _pb in PB:
            _pb.b.excl = True
        prot = {"mm": [0, 1], "tr": [2], "cv": [3], "dn": [4], "nm": [5], "st": [6], "o": [7], "tr2": [2, 3], "dw": [6, 7], "mm4": [0, 1, 4, 5], "n0": [0], "g": [1, 3], "g2": [6, 7], "dnA": [2], "nmA": [4], "dnB": [0], "nmB": [5], "rmm": [1], "rcv": [3]}
        pidx = {k: 0 for k in prot}

        def bank(role):
            l = prot[role]
            i = l[pidx[role] % len(l)]
            pidx[role] += 1
            return PB[i]

        def bfv(pb):
            return pb.t[:, :].bitcast(BF16)

        def v3(ap, a):
            return ap.rearrange("p (a b) -> p a b", a=a)

        def bc3(ap2, a, b):
            return ap2.unsqueeze(2).to_broadcast([ap2.shape[0], a, b])

        def bcm(ap2, a, b):
            return ap2.unsqueeze(1).to_broadcast([ap2.shape[0], a, b])

        CF = sb("CF", [128, NCF], F32)
        COLS = sb("COLS", [128, NCOL], F32)
        ROWS = sb("ROWS", [128, NROW], F32)
        CBF = sb("CBF", [128, 512], BF16)
        W_R = sb("W_R", [128, 8 * 72], F32)
        NEGA = sb("NEGA", [128, 4], F32)
        CW = sb("CW", [128, NT * 2], F32)
        OFFS = sb("OFFS", [128, NT * 2], I32)
        BASECAP = sb("BASECAP", [128, NE], F32)

        def cf(name, lo=None, hi=None):
            a, b = _cfo[name]
            return CF.t[:, a:b]

        IDF = cf("ident")
        IDB = CBF.t[:, 0:128]
        SUB = CBF.t[:, 128:256]
        PMB = CBF.t[:, 256:384]
        ONB = CBF.t[:, 384:512]

        dq = [fw.dsem() for _ in range(4)]
        E("sp", lambda e: e.dma_start(out=CF.t[:, :], in_=cf_d), writes=[CF], dma=dq[0])
        E("sp", lambda e: e.dma_start(out=COLS.t[:, :], in_=cols_d), writes=[COLS], dma=dq[1])
        E("sp", lambda e: e.dma_start(out=ROWS.t[:, :], in_=bass.AP(tensor=rows_d.tensor, offset=0, ap=[[0, 128], [1, NROW]])),
          writes=[ROWS], dma=dq[2])
        E("sp", lambda e: e.dma_start(out=W_R.t[:, :], in_=w_r), writes=[W_R], dma=dq[3])
        for i, nm in enumerate(("ident", "su", "pm", "ones")):
            E("dve", lambda e, i=i, nm=nm: e.tensor_copy(out=CBF.t[:, i * 128:(i + 1) * 128], in_=cf(nm)),
              reads=[CF], writes=[CBF])
        for kc in range(8):
            E("dve", lambda e, kc=kc: e.tensor_scalar(out=W_R.t[:, kc * 72:(kc + 1) * 72], in0=W_R.t[:, kc * 72:(kc + 1) * 72],
                                                     scalar1=COLS.t[:, 56 + kc:57 + kc], scalar2=None, op0=OP.mult),
              reads=[W_R, COLS], writes=[W_R])
        E("act", lambda e: e.activation(out=NEGA.t[:, :], in_=ROWS.t[:, 0:4], func=AF.Exp), reads=[ROWS], writes=[NEGA])
        E("dve", lambda e: e.tensor_scalar(out=NEGA.t[:, :], in0=NEGA.t[:, :], scalar1=-1.0, scalar2=None, op0=OP.mult),
          reads=[NEGA], writes=[NEGA])
        E("dve", lambda e: e.tensor_copy(out=BASECAP.t[:, :], in_=cf("iotacap")), reads=[CF], writes=[BASECAP])

        ZT = sb("ZT", [128, 1024], BF16)
        E("pool", lambda e: e.memset(ZT.t[:, :], 0.0), writes=[ZT])
        XSB = Buf("xs_all")
        zsem = fw.dsem()
        zsem0 = fw.dsem()
        xs_c = xs_all.rearrange("(c q r) d -> c q (r d)", c=16, q=16)
        xs_v = xs_all.rearrange("(e p r) d -> e p (r d)", e=2 * NE, p=128)

        with ExitStack() as p1:
            def sb1(name, shape, ty):
                return sb(name, shape, ty, p1)

            WIN = sb1("WIN", [128, 8, DIN], BF16)
            WOUT = sb1("WOUT", [128, 8, D], BF16)
            CDT = [sb1("CDT%d" % i, [128, 4, 128], BF16) for i in range(2)]
            wsem = [fw.dsem()]
            for kc in range(8):
                E("pool", lambda e, kc=kc: e.dma_start(out=WIN.t[:, kc, :], in_=w_in[:, kc, :]), writes=[WIN], dma=wsem[0])

            def wg(c0):
                return WIN

            pcs = fw.dsem()
            def precast(ex):
                E("pool", lambda e, ex=ex: e.dma_start(out=wgu_bf[ex], in_=wgu[ex]), dma=pcs)
                E("pool", lambda e, ex=ex: e.dma_start(out=wd_bf[ex], in_=wd[ex]), dma=pcs)
            XT = [sb1("XT%d" % i, [128, D], F32) for i in range(2)]
            xsem = [fw.dsem() for _ in range(2)]
            xrsem = fw.dsem()
            SS = sb1("SS", [128, 8], F32)
            SSG = sb1("SSG", [128, 8], F32)
            SS4 = [sb1("SSx%d" % i, [128, 2], F32) for i in range(TPB)]
            HT = sb1("HT", [128, 8, BLK], BF16)
            PRE = [sb1("PRE%d" % i, [128, 3 + BLK], BF16) for i in range(2)]
            HIST = sb1("HIST", [128, 12, 3], BF16)
            VT = sb1("VT", [128, 4, BLK], BF16)
            SQ = sb1("SQ", [128, BLK], BF16)
            QTA = sb1("QTA", [128, 4, BLK], BF16)
            KTA = sb1("KTA", [128, 4, BLK], BF16)
            QTB = sb1("QTB", [128, 4, BLK], BF16)
            KTB = sb1("KTB", [128, 4, BLK], BF16)
            rsem = fw.dsem()
            SG = [sb1("SG%d" % i, [128, D], BF16) for i in range(TPB)]
            VB = [sb1("VB%d" % i, [128, 512], BF16) for i in range(2)] * 2
            VA2 = [sb1("VA%d" % i, [128, 512], BF16) for i in range(2)]
            KDEC2 = [sb1("KDEC%d" % i, [128, 512], BF16) for i in range(2)]
            KDECB = [sb1("KDECB%d" % i, [128, 512], BF16) for i in range(2)] * 2
            YF3 = [sb1("YF%d" % i, [128, 512], BF16) for i in range(3)]
            BA = sb1("BA", [128, TPB, 8], F32)
            BETA = sb1("BETA", [128, 16], F32)
            GG = sb1("GG", [128, 16], F32)
            GM = sb1("GM", [128, TPB, 8], F32)
            EGC = sb1("EGC", [128, 16], F32)
            NEGC = sb1("NEGC", [128, 16], F32)
            EDEC = sb1("EDEC", [128, 16], F32)
            LASTB = sb1("LASTB", [128, TPB * 8], F32)
            TMPS = sb1("TMPS", [128, 16], F32)
            RR = sb1("RR", [128, 512], F32)
            DECT = sb1("DECT", [128, 512], F32)
            WW = sb1("WW", [128, 512], F32)
            TT = [sb1("TT%d" % i, [128, 512], BF16) for i in range(2)]
            NN = [sb1("NN%d" % i, [128, 512], BF16) for i in range(2)]
            YY = [sb1("YY%d" % i, [128, 512], BF16) for i in range(2)]
            ATT2 = [sb1("ATT%d" % i, [128, 512], BF16) for i in range(2)]
            TMPZ = sb1("TMPZ", [128, 512], F32)
            ZZ = sb1("ZZ", [128, 512], BF16)
            VN = sb1("VN", [128, 512], BF16)
            SA = sb1("SA", [128, 512], F32)
            SAB = sb1("SAB", [128, 512], BF16)
            SBS = sb1("SBS", [128, 512], F32)
            SBB = sb1("SBB", [128, 512], BF16)
            ATB = sb1("ATB", [128, 512], BF16)
            OALLL = [sb1("OALL%d" % i, [128, D], F32) for i in range(2)] * 2
            RSTD8 = sb1("RSTD8", [128, 8], F32)
            MIX = sb1("MIX", [128, D], BF16)
            MIXT = sb1("MIXT", [128, 8, 128], BF16)
            X1 = sb1("X1", [128, D], F32)
            x1sem = fw.dsem()
            H2B = sb1("H2B", [128, D], BF16)
            H2T = sb1("H2T", [128, 8, 128], F32)
            OSQ = TB.__new__(TB); OSQ.b = H2T.b; OSQ.t = H2T.t[:, :, :].rearrange("p a b -> p (a b)")
            HBF = sb1("HBF1", [128, D], BF16)
            RINV = DECT
            RA = TB.__new__(TB); RA.b = X1.b; RA.t = X1.t[:, 0:512]
            RB = TMPZ
            SQ_B = TB.__new__(TB); SQ_B.b = PRE[0].b; SQ_B.t = PRE[0].t[:, 0:512]
            RINV_B = WW
            JUNKS = [TB.__new__(TB), TB.__new__(TB)]
            JUNKS[0].b = QTB.b; JUNKS[0].t = QTB.t[:, 0:2, :].rearrange("p a b -> p (a b)")
            JUNKS[1].b = KTB.b; JUNKS[1].t = KTB.t[:, 0:2, :].rearrange("p a b -> p (a b)")

            def alias(base, ap):
                a_ = TB.__new__(TB); a_.b = base.b; a_.t = ap
                return a_
            xt1bf = XT[1].t[:, 512:1024].bitcast(BF16)
            hbfv = HBF.t[:, :]
            NSETS = [
                (RR, DECT, WW, TT, NN, YY),
                (alias(XT[0], XT[0].t[:, 0:512]), alias(XT[0], XT[0].t[:, 512:1024]), alias(XT[1], XT[1].t[:, 0:512]),
                 [alias(XT[1], xt1bf[:, 0:512]), alias(XT[1], xt1bf[:, 512:1024])],
                 [alias(HBF, hbfv[:, 0:512]), alias(HBF, hbfv[:, 512:1024])],
                 [alias(PRE[0], PRE[0].t[:, 0:512]), alias(PRE[1], PRE[1].t[:, 0:512])]),
            ]
            NBANKS = [("dnA", "nmA"), ("dnB", "nmB")]
            s3 = lambda l2, third: [l2[0], l2[1], third, l2[0]]
            KDEC = s3(KDEC2, alias(CDT[0], CDT[0].t[:, :, :].rearrange("p a b -> p (a b)")))
            VA = s3(VA2, alias(CDT[1], CDT[1].t[:, :, :].rearrange("p a b -> p (a b)")))
            ATTL = s3(ATT2, alias(SQ, SQ.t[:, :]))
            YFL = [YF3[0], YF3[1], YF3[2], YF3[0]]
            TBF = SQ

            ROPE = TB.__new__(TB); ROPE.b = H2T.b; ROPE.t = H2T.t[:, :, :].rearrange("p (a c) b -> p a (c b)", a=2)
            wss = [fw.dsem() for _ in range(2)]
            for kc in range(8):
                stg = (X1, OSQ)[kc % 2]
                E("sp", lambda e, kc=kc, stg=stg: e.dma_start(out=stg.t[:, :], in_=w_out[:, kc, :]), writes=[stg], dma=wss[kc % 2])
                E("dve", lambda e, kc=kc, stg=stg: e.tensor_scalar(out=WOUT.t[:, kc, :], in0=stg.t[:, :], scalar1=COLS.t[:, 48 + kc:49 + kc],
                                                                   scalar2=None, op0=OP.mult),
                  reads=[stg, COLS], writes=[WOUT])
            LG = sb1("LG", [128, 72], F32)
            RT = sb1("RT", [128, 16], F32)
            GMASK = sb1("GMASK", [128, 8], F32)
            PEN = sb1("PEN", [128, 8], F32)
            EL = sb1("EL", [128, 64], F32)
            EL2 = sb1("EL2", [128, 64], F32)
            OH1 = sb1("OH1", [128, 64], F32)
            OH2 = sb1("OH2", [128, 64], F32)
            OH12 = sb1("OH12", [128, 64], BF16)
            POSM = sb1("POSM", [128, 64], F32)
            PRD = sb1("PRD", [128, 64], F32)
            OFF_F = sb1("OFF_F", [128, 2], F32)
            scsem = fw.dsem()
            dbgsem = fw.dsem() if debug else None

            for z in (SA, SBS):
                E("pool", lambda e, z=z: e.memset(z.t[:, :], 0.0), writes=[z])
            for z in (SAB, SBB):
                E("pool", lambda e, z=z: e.memset(z.t[:, :], 0.0), writes=[z])
            E("pool", lambda e: e.memset(HIST.t[:, :, :], 0.0), writes=[HIST])

            def rstd_from_ss(ss_ap, out_ap, n, rbufs, wbufs):
                E("dve", lambda e: e.tensor_scalar(out=out_ap, in0=ss_ap, scalar1=1.0 / n, scalar2=EPS, op0=OP.mult, op1=OP.add),
                  reads=rbufs, writes=wbufs)
                E("act", lambda e: e.activation(out=out_ap, in_=out_ap, func=AF.Ln), reads=wbufs, writes=wbufs)
                E("act", lambda e: e.activation(out=out_ap, in_=out_ap, func=AF.Exp, scale=-0.5), reads=wbufs, writes=wbufs)

            pendG = [None]

            from collections import deque

            def run_rr(gens, stop_on_first=False, stop_on=(), must_finish=(), after=None):
                gens = [g for g in gens if g is not None]
                for g in gens:
                    if g not in pend:
                        pend[g] = deque()
                alive = {g: True for g in gens}

                def fill(g):
                    q = pend[g]
                    while not q and alive[g]:
                        sink[0] = q
                        try:
                            next(g)
                        except StopIteration:
                            alive[g] = False
                        sink[0] = None
                    return bool(q)

                watch = [gens[0]] if stop_on_first else [gens[i] for i in stop_on]
                need = [gens[i] for i in must_finish]
                active = list(gens)
                while True:
                    best = None
                    for pi, g in enumerate(active):
                        if after and g in after and any(alive[x] or pend[x] for x in after[g]):
                            continue
                        if fill(g):
                            op = pend[g][0]
                            t = fw.est_start(op[0], op[2], op[3])
                            if best is None or t < best[0] - 1e-9:
                                best = (t, pi, g)
                    if best is None:
                        break
                    g = best[2]
                    en, fn, rd, wr, dma, cost = pend[g].popleft()
                    fw.emit(en, fn, rd, wr, dma, cost, frozen=True)
                    if watch and not any(alive[w] or pend[w] for w in watch):
                        active = [g2 for g2 in need if alive[g2] or pend[g2]]
                        watch = []
                        if not active:
                            break
                        stopping = True
                for g in gens:
                    if not alive[g] and not pend[g]:
                        pend.pop(g, None)

            pend = {}

            def ldx_blk(bb, tl):
                t = bb * TPB + tl
                xt = XT[tl % 2]
                E("sp", lambda e, t=t, xt=xt: e.dma_start(out=xt.t[:, :], in_=x[t * 128:(t + 1) * 128, :]), writes=[xt], dma=xsem[tl % 2])

            for b in range(NB):
                def thrP1():
                    def ldx(tl):
                        ldx_blk(b, tl)
                    if b == 0:
                        ldx(0)
                    for tl in range(TPB):
                        if tl + 1 < TPB and not (b > 0 and tl == 0):
                            ldx(tl + 1)
                        xt = XT[tl % 2]
                        ss = SS4[tl]
                        jk = JUNKS[tl % 2]
                        E("pool", lambda e, ss=ss: e.memset(ss.t[:, 0:1], 0.0), writes=[ss], cost=0.2)
                        yield
                        E("act", lambda e, xt=xt, ss=ss, jk=jk: e.activation(out=jk.t, in_=xt.t[:, :], func=AF.Square, accum_out=ss.t[:, 0:1]),
                          reads=[xt, ss], writes=[jk, ss], cost=1.1)
                        yield
                        rstd_from_ss(ss.t[:, 0:1], ss.t[:, 1:2], D, [ss], [ss])
                        E("dve", lambda e, xt=xt, ss=ss: e.tensor_scalar(out=HBF.t[:, :], in0=xt.t[:, :], scalar1=ss.t[:, 1:2], scalar2=None, op0=OP.mult),
                          reads=[xt, ss], writes=[HBF], cost=1.1)
                        yield
                        pt = bank("tr")
                        for kc in range(8):
                            E("pe", lambda e, kc=kc, pt=pt: e.transpose(out=bfv(pt)[:, kc * 128:(kc + 1) * 128], in_=HBF.t[:, kc * 128:(kc + 1) * 128],
                                                                        identity=IDB),
                              reads=[HBF, CBF], writes=[pt])
                            yield
                        E("dve", lambda e, pt=pt, tl=tl: e.tensor_tensor(out=HT.t[:, :, tl * 128:(tl + 1) * 128], in0=v3(bfv(pt)[:, :], 8),
                                                                         in1=bc3(COLS.t[:, 64:72], 8, 128), op=OP.mult),
                          reads=[pt, COLS], writes=[HT])
                        yield

                    if b == 0:
                        XS0 = Buf("xs_chunk0")
                        for ex in range(8):
                            E("sp", lambda e, ex=ex: e.dma_start(out=xs_v[ex], in_=ZT.t[:, :]), reads=[ZT], writes=[XS0], dma=zsem0)
                            yield
                        for k in range(1, 16):
                            E("sp", lambda e, k=k: e.dma_start(out=xs_c[k], in_=xs_c[0]), reads=[XS0], writes=[XSB], dma=zsem)
                            yield
                        dump("ht", HT.t[:, :, :], [128, 8, BLK], BF16, [HT])
                    def proj_fm(c0):
                        pm = bank("mm")
                        for kc in range(8):
                            E("pe", lambda e, kc=kc, pm=pm: e.matmul(pm.t[:, :], lhsT=WIN.t[:, kc, c0:c0 + 128], rhs=HT.t[:, kc, :],
                                                                     start=(kc == 0), stop=(kc == 7)),
                              reads=[wg(c0), HT], writes=[pm])
                        return pm

                    for c in range(12):
                        pm = proj_fm(c * 128)
                        pre = PRE[c % 2]
                        E("pool", lambda e, c=c, pre=pre: e.tensor_copy(out=pre.t[:, 0:3], in_=HIST.t[:, c, :]), reads=[HIST], writes=[pre])
                        yield
                        E("act", lambda e, pm=pm, pre=pre: e.activation(out=pre.t[:, 3:3 + BLK], in_=pm.t[:, :], func=AF.Copy), reads=[pm], writes=[pre])
                        yield
                        E("pool", lambda e, c=c, pre=pre: e.tensor_copy(out=HIST.t[:, c, :], in_=pre.t[:, BLK:BLK + 3]), reads=[pre], writes=[HIST])
                        yield
                        pc = bank("cv")
                        cd = CDT[c % 2]
                        for j in range(4):
                            E("dve", lambda e, j=j, c=c, cd=cd: e.tensor_scalar(out=cd.t[:, j, :], in0=IDF, scalar1=COLS.t[:, c * 4 + j:c * 4 + j + 1], scalar2=None, op0=OP.mult),
                              reads=[CF, COLS], writes=[cd])
                            yield
                        for j in range(4):
                            E("pe", lambda e, j=j, c=c, pc=pc, pre=pre, cd=cd: e.matmul(pc.t[:, :], lhsT=cd.t[:, j, :], rhs=pre.t[:, j:j + BLK],
                                                                                       start=(j == 0), stop=(j == 3)),
                              reads=[cd, pre], writes=[pc])
                            yield
                        if c < 8:
                            sd = QTA if c < 4 else KTA
                            E("act", lambda e, c=c, pc=pc, sd=sd: e.activation(out=sd.t[:, c % 4, :], in_=pc.t[:, :], func=AF.Silu), reads=[pc], writes=[sd])
                            yield
                        else:
                            E("act", lambda e, c=c, pc=pc: e.activation(out=VT.t[:, c - 8, :], in_=pc.t[:, :], func=AF.Silu), reads=[pc], writes=[VT])
                            yield
                    for tl in range(TPB):
                        for (c0, n, kind) in ((1536, 512, "ga"), (3592, 512, "gb"), (2048, 8, "ba")):
                            pm = bank("mm")
                            for kc in range(8):
                                E("pe", lambda e, kc=kc, pm=pm, c0=c0, n=n, tl=tl: e.matmul(pm.t[:, 0:n], lhsT=HT.t[:, kc, tl * 128:(tl + 1) * 128],
                                                                                           rhs=WIN.t[:, kc, c0:c0 + n], start=(kc == 0), stop=(kc == 7)),
                                  reads=[wg(c0), HT], writes=[pm])
                                yield
                            if kind == "ga":
                                E("act", lambda e, pm=pm, tl=tl: e.activation(out=SG[tl].t[:, 0:512], in_=pm.t[:, :], func=AF.Silu), reads=[pm], writes=[SG[tl]])
                                yield
                            elif kind == "gb":
                                E("act", lambda e, pm=pm, tl=tl: e.activation(out=SG[tl].t[:, 512:1024], in_=pm.t[:, :], func=AF.Silu), reads=[pm], writes=[SG[tl]])
                                yield
                            else:
                                E("dve", lambda e, pm=pm, tl=tl: e.tensor_copy(out=BA.t[:, tl, :], in_=pm.t[:, 0:8]), reads=[pm], writes=[BA])
                                yield

                    yield
                def thrL(par, SQ, RINV, lbank):
                    for c in range(par, 8, 2):
                        dst = QTA if c < 4 else KTA
                        E("pool", lambda e, c=c, dst=dst: e.tensor_tensor(out=SQ.t[:, :], in0=dst.t[:, c % 4, :], in1=dst.t[:, c % 4, :], op=OP.mult),
                          reads=[dst], writes=[SQ])
                        yield
                        pc = bank(lbank)
                        E("pe", lambda e, pc=pc: e.matmul(pc.t[:, :], lhsT=ONB, rhs=SQ.t[:, :], start=True, stop=True), reads=[CBF, SQ], writes=[pc])
                        yield
                        E("act", lambda e, pc=pc: e.activation(out=RINV.t[:, :], in_=pc.t[:, :], func=AF.Ln, bias=EPS), reads=[pc], writes=[RINV])
                        yield
                        lb = float(np.log(128.0 ** -0.5)) if c < 4 else 0.0
                        E("act", lambda e, lb=lb: e.activation(out=RINV.t[:, :], in_=RINV.t[:, :], func=AF.Exp, scale=-0.5, bias=lb),
                          reads=[RINV], writes=[RINV])
                        yield
                        E("dve", lambda e, c=c, dst=dst: e.tensor_tensor(out=dst.t[:, c % 4, :], in0=dst.t[:, c % 4, :], in1=RINV.t[:, :], op=OP.mult),
                          reads=[dst, RINV], writes=[dst])
                        yield
                    yield
                def thrRope():
                    def proj_fm(c0):
                        pm = bank("rmm")
                        for kc in range(8):
                            E("pe", lambda e, kc=kc, pm=pm: e.matmul(pm.t[:, :], lhsT=WIN.t[:, kc, c0:c0 + 128], rhs=HT.t[:, kc, :],
                                                                     start=(kc == 0), stop=(kc == 7)),
                              reads=[wg(c0), HT], writes=[pm])
                        return pm
                    for c in range(8):
                        c0 = 2056 + c * 128
                        pm = proj_fm(c0)
                        E("act", lambda e, pm=pm: e.activation(out=TBF.t[:, :], in_=pm.t[:, :], func=AF.Copy), reads=[pm], writes=[TBF])
                        yield
                        pc = bank("rcv")
                        E("pe", lambda e, pc=pc: e.matmul(pc.t[:, :], lhsT=PMB, rhs=TBF.t[:, :], start=True, stop=True), reads=[CBF, TBF], writes=[pc])
                        yield
                        E("dve", lambda e, pm=pm: e.tensor_tensor(out=RA.t[:, :], in0=pm.t[:, :], in1=ROPE.t[:, 0, :], op=OP.mult),
                          reads=[pm, ROPE], writes=[RA])
                        yield
                        E("dve", lambda e, pc=pc: e.tensor_tensor(out=RB.t[:, :], in0=pc.t[:, :], in1=ROPE.t[:, 1, :], op=OP.mult),
                          reads=[pc, ROPE], writes=[RB])
                        yield
                        dst = QTB if c < 4 else KTB
                        E("pool", lambda e, c=c, dst=dst: e.tensor_tensor(out=dst.t[:, c % 4, :], in0=RA.t[:, :], in1=RB.t[:, :], op=OP.add),
                          reads=[RA, RB], writes=[dst])
                        yield

                    yield
                def thrScal():
                    ba3 = BA.t[:, :, :]
                    b16 = v3(BETA.t[:, :], TPB)
                    g16 = v3(GG.t[:, :], TPB)
                    t16 = v3(TMPS.t[:, :], TPB)
                    E("act", lambda e: e.activation(out=b16, in_=ba3[:, :, 0:4], func=AF.Exp, scale=-1.0), reads=[BA], writes=[BETA])
                    yield
                    E("dve", lambda e: e.tensor_scalar(out=BETA.t[:, :], in0=BETA.t[:, :], scalar1=1.0, scalar2=None, op0=OP.add), reads=[BETA], writes=[BETA])
                    yield
                    E("dve", lambda e: e.reciprocal(out=BETA.t[:, :], in_=BETA.t[:, :]), reads=[BETA], writes=[BETA])
                    yield
                    E("dve", lambda e: e.tensor_tensor(out=g16, in0=ba3[:, :, 4:8], in1=bcm(ROWS.t[:, 4:8], TPB, 4), op=OP.add),
                      reads=[BA, ROWS], writes=[GG])
                    yield
                    E("act", lambda e: e.activation(out=GG.t[:, :], in_=GG.t[:, :], func=AF.Exp), reads=[GG], writes=[GG])
                    yield
                    E("act", lambda e: e.activation(out=GG.t[:, :], in_=GG.t[:, :], func=AF.Ln, bias=1.0), reads=[GG], writes=[GG])
                    yield
                    E("dve", lambda e: e.tensor_tensor(out=g16, in0=g16, in1=bcm(NEGA.t[:, :], TPB, 4), op=OP.mult), reads=[GG, NEGA], writes=[GG])
                    yield
                    ps = bank("st")
                    E("pe", lambda e, ps=ps: e.matmul(ps.t[:, 0:16], lhsT=cf("ub"), rhs=GG.t[:, :], start=True, stop=True), reads=[CF, GG], writes=[ps])
                    yield
                    E("pe", lambda e, ps=ps: e.matmul(ps.t[:, 16:32], lhsT=cf("bb"), rhs=GG.t[:, :], start=True, stop=True), reads=[CF, GG], writes=[ps])
                    yield
                    for cc in range(2):
                        E("dve", lambda e, cc=cc: e.tensor_scalar(out=GM.t[:, :, cc * 4:(cc + 1) * 4], in0=g16, scalar1=cf("cm")[:, cc:cc + 1],
                                                                  scalar2=None, op0=OP.mult),
                          reads=[GG, CF], writes=[GM])
                        yield
                    E("pe", lambda e, ps=ps: e.matmul(ps.t[:, 32:64], lhsT=cf("ones"), rhs=GM.t[:, :, :].rearrange("p a b -> p (a b)"),
                                                      start=True, stop=True), reads=[CF, GM], writes=[ps])
                    yield
                    E("act", lambda e, ps=ps: e.activation(out=EGC.t[:, :], in_=ps.t[:, 0:16], func=AF.Exp), reads=[ps], writes=[EGC])
                    yield
                    E("dve", lambda e: e.tensor_scalar(out=NEGC.t[:, :], in0=EGC.t[:, :], scalar1=-1.0, scalar2=None, op0=OP.mult), reads=[EGC], writes=[NEGC])
                    yield
                    E("act", lambda e, ps=ps: e.activation(out=TMPS.t[:, :], in_=ps.t[:, 16:32], func=AF.Copy), reads=[ps], writes=[TMPS])
                    yield
                    E("dve", lambda e, ps=ps: e.tensor_tensor(out=TMPS.t[:, :], in0=TMPS.t[:, :], in1=ps.t[:, 0:16], op=OP.subtract),
                      reads=[ps, TMPS], writes=[TMPS])
                    yield
                    E("act", lambda e: e.activation(out=EDEC.t[:, :], in_=TMPS.t[:, :], func=AF.Exp), reads=[TMPS], writes=[EDEC])
                    yield
                    E("act", lambda e, ps=ps: e.activation(out=LASTB.t[:, :], in_=ps.t[:, 32:64], func=AF.Exp), reads=[ps], writes=[LASTB])
                    yield

                    yield
                def dumps23():
                    if b == 0:
                        dump("qta", QTA.t[:, :, :], [128, 4, BLK], BF16, [QTA])
                        dump("kta", KTA.t[:, :, :], [128, 4, BLK], BF16, [KTA])
                        dump("vt", VT.t[:, :, :], [128, 4, BLK], BF16, [VT])
                        dump("qtb", QTB.t[:, :, :], [128, 4, BLK], BF16, [QTB])
                        dump("ktb", KTB.t[:, :, :], [128, 4, BLK], BF16, [KTB])
                        dump("sg0", SG[0].t[:, :], [128, D], BF16, [SG[0]])
                        dump("ba", BA.t[:, :, :], [128, TPB, 8], F32, [BA])
                    if b == 0:
                        dump("beta", BETA.t[:, :], [128, 16], F32, [BETA])
                        dump("gg", GG.t[:, :], [128, 16], F32, [GG])
                        dump("egc", EGC.t[:, :], [128, 16], F32, [EGC])
                        dump("edec", EDEC.t[:, :], [128, 16], F32, [EDEC])
                        dump("lastb", LASTB.t[:, :], [128, 32], F32, [LASTB])
                    pass
                def tok_major(tl, trb):
                    cs = slice(tl * 128, (tl + 1) * 128)
                    pt = bank(trb)
                    for h in range(4):
                        E("pe", lambda e, h=h, pt=pt, cs=cs: e.transpose(out=bfv(pt)[:, h * 128:(h + 1) * 128], in_=KTA.t[:, h, cs], identity=IDB),
                          reads=[KTA, CBF], writes=[pt])
                    E("dve", lambda e, pt=pt, tl=tl: e.tensor_tensor(out=v3(KDEC[tl].t[:, :], 4), in0=v3(bfv(pt)[:, 0:512], 4),
                                                                     in1=bc3(EDEC.t[:, tl * 4:(tl + 1) * 4], 4, 128), op=OP.mult),
                      reads=[pt, EDEC], writes=[KDEC[tl]])
                    pt = bank(trb)
                    for h in range(4):
                        E("pe", lambda e, h=h, pt=pt, cs=cs: e.transpose(out=bfv(pt)[:, h * 128:(h + 1) * 128], in_=VT.t[:, h, cs], identity=IDB),
                          reads=[VT, CBF], writes=[pt])
                    E("act", lambda e, pt=pt, tl=tl: e.activation(out=VA[tl].t[:, :], in_=bfv(pt)[:, 0:512], func=AF.Copy), reads=[pt], writes=[VA[tl]])

                def tok_major_s(tl):
                    cs = slice(tl * 128, (tl + 1) * 128)
                    pm = bank("st")
                    for kc in range(8):
                        E("pe", lambda e, kc=kc, pm=pm: e.matmul(pm.t[:, :], lhsT=HT.t[:, kc, cs], rhs=WIN.t[:, kc, 3080:3592], start=(kc == 0), stop=(kc == 7)),
                          reads=[wg(3080), HT], writes=[pm])
                    E("act", lambda e, pm=pm: e.activation(out=VB[tl].t[:, :], in_=pm.t[:, :], func=AF.Copy), reads=[pm], writes=[VB[tl]])
                    pt = bank("o")
                    for h in range(4):
                        E("pe", lambda e, h=h, pt=pt, cs=cs: e.transpose(out=bfv(pt)[:, h * 128:(h + 1) * 128], in_=KTB.t[:, h, cs], identity=IDB),
                          reads=[KTB, CBF], writes=[pt])
                    E("dve", lambda e, pt=pt, tl=tl: e.tensor_tensor(out=v3(KDECB[tl].t[:, :], 4), in0=v3(bfv(pt)[:, 0:512], 4),
                                                                     in1=bc3(cf("kdsc"), 4, 128), op=OP.mult),
                      reads=[pt, CF], writes=[KDECB[tl]])


                def thrN(tl, ns=0):
                    t = b * TPB + tl
                    cs = slice(tl * 128, (tl + 1) * 128)
                    sc = slice(tl * 4, (tl + 1) * 4)
                    hc = [slice(h * 128, (h + 1) * 128) for h in range(4)]
                    RR, DECT, WW, TT, NN, YY = NSETS[ns]
                    dnb, nmb = NBANKS[ns]
                    tok_major(tl, dnb)
                    yield
                    E("pool", lambda e, sc=sc: e.tensor_tensor(out=v3(RR.t[:, :], 4), in0=bcm(cf("ub"), 4, 128), in1=bc3(GG.t[:, sc], 4, 128), op=OP.mult),
                      reads=[CF, GG], writes=[RR])
                    yield
                    pd = bank(dnb)
                    E("pe", lambda e, pd=pd: e.matmul(pd.t[:, :], lhsT=cf("slb"), rhs=RR.t[:, :], start=True, stop=False), reads=[CF, RR], writes=[pd])
                    yield
                    E("pe", lambda e, pd=pd: e.matmul(pd.t[:, :], lhsT=IDF, rhs=cf("negm4"), start=False, stop=True), reads=[CF], writes=[pd])
                    yield
                    E("act", lambda e, pd=pd: e.activation(out=DECT.t[:, :], in_=pd.t[:, :], func=AF.Exp), reads=[pd], writes=[DECT])
                    yield
                    E("pool", lambda e: e.tensor_tensor(out=v3(WW.t[:, :], 4), in0=v3(DECT.t[:, :], 4), in1=bcm(cf("strn4"), 4, 128), op=OP.mult), reads=[DECT, CF], writes=[WW])
                    yield
                    E("pool", lambda e, sc=sc: e.tensor_tensor(out=v3(WW.t[:, :], 4), in0=v3(WW.t[:, :], 4), in1=bc3(BETA.t[:, sc], 4, 128), op=OP.mult),
                      reads=[WW, BETA], writes=[WW])
                    yield
                    pk = bank(dnb)
                    for h in range(4):
                        E("pe", lambda e, h=h, pk=pk: e.matmul(pk.t[:, hc[h]], lhsT=KTA.t[:, h, cs], rhs=KTA.t[:, h, cs], start=True, stop=True),
                          reads=[KTA], writes=[pk])
                        yield
                    E("dve", lambda e, pk=pk: e.tensor_tensor(out=TT[0].t[:, :], in0=pk.t[:, :], in1=WW.t[:, :], op=OP.mult), reads=[pk, WW], writes=[TT[0]])
                    yield
                    pq = bank(dnb)
                    for h in range(4):
                        E("pe", lambda e, h=h, pq=pq: e.matmul(pq.t[:, hc[h]], lhsT=KTA.t[:, h, cs], rhs=QTA.t[:, h, cs], start=True, stop=True),
                          reads=[KTA, QTA], writes=[pq])
                        yield
                    E("dve", lambda e, pq=pq: e.tensor_tensor(out=ATTL[tl].t[:, :], in0=pq.t[:, :], in1=DECT.t[:, :], op=OP.mult), reads=[pq, DECT], writes=[ATTL[tl]])
                    yield
                    pt = bank(dnb)
                    for h in range(4):
                        E("pe", lambda e, h=h, pt=pt: e.transpose(out=bfv(pt)[:, hc[h]], in_=TT[0].t[:, hc[h]], identity=IDB), reads=[TT[0], CBF], writes=[pt])
                        yield
                    E("act", lambda e, pt=pt: e.activation(out=NN[0].t[:, :], in_=bfv(pt)[:, 0:512], func=AF.Copy), reads=[pt], writes=[NN[0]])
                    yield
                    E("pool", lambda e: e.tensor_tensor(out=v3(YY[0].t[:, :], 4), in0=v3(TT[0].t[:, :], 4), in1=bcm(IDF, 4, 128), op=OP.add), reads=[TT[0], CF], writes=[YY[0]])
                    yield
                    for p in range(5):
                        Tp, Np, Tn, Nn = TT[p % 2], NN[p % 2], TT[(p + 1) % 2], NN[(p + 1) % 2]
                        Yp, Yn = YY[p % 2], (YY[(p + 1) % 2] if p < 4 else YFL[tl])
                        pn = bank(nmb)
                        for h in range(4):
                            E("pe", lambda e, h=h, pn=pn, Tp=Tp, Np=Np: e.matmul(pn.t[:, hc[h]], lhsT=Tp.t[:, hc[h]], rhs=Np.t[:, hc[h]], start=True, stop=True),
                              reads=[Tp, Np], writes=[pn])
                            yield
                        if p < 4:
                            pt2 = bank(dnb)
                            for h in range(4):
                                E("pe", lambda e, h=h, pt2=pt2, Tp=Tp, Np=Np: e.matmul(pt2.t[:, hc[h]], lhsT=Np.t[:, hc[h]], rhs=Tp.t[:, hc[h]], start=True, stop=True),
                                  reads=[Tp, Np], writes=[pt2])
                                yield
                        E("act", lambda e, pn=pn, Nn=Nn: e.activation(out=Nn.t[:, :], in_=pn.t[:, :], func=AF.Copy), reads=[pn], writes=[Nn])
                        yield
                        if p < 4:
                            E("dve", lambda e, pt2=pt2, Tn=Tn: e.tensor_copy(out=Tn.t[:, :], in_=pt2.t[:, :]), reads=[pt2], writes=[Tn])
                            yield
                        py = bank(nmb)
                        for h in range(4):
                            E("pe", lambda e, h=h, py=py, Nn=Nn, Yp=Yp: e.matmul(py.t[:, hc[h]], lhsT=Nn.t[:, hc[h]], rhs=Yp.t[:, hc[h]], start=True, stop=True),
                              reads=[Nn, Yp], writes=[py])
                            yield
                        E("dve", lambda e, py=py, Yp=Yp, Yn=Yn: e.tensor_tensor(out=Yn.t[:, :], in0=py.t[:, :], in1=Yp.t[:, :], op=OP.add),
                          reads=[py, Yp], writes=[Yn])
                        yield
                    YF = YFL[tl]

                    if t == 0:
                        dump("dect", DECT.t[:, :], [128, 512], F32, [DECT])
                        dump("att", ATTL[tl].t[:, :], [128, 512], BF16, [ATTL[tl]])
                        dump("yf", YF.t[:, :], [128, 512], BF16, [YF])
                        dump("vb", VB[0].t[:, :], [128, 512], BF16, [VB[0]])
                        dump("va", VA[0].t[:, :], [128, 512], BF16, [VA[0]])
                        dump("kdec", KDEC[0].t[:, :], [128, 512], BF16, [KDEC[0]])
                        dump("kdecb", KDECB[0].t[:, :], [128, 512], BF16, [KDECB[0]])
                    yield
                def thrS(tl):
                    t = b * TPB + tl
                    cs = slice(tl * 128, (tl + 1) * 128)
                    sc = slice(tl * 4, (tl + 1) * 4)
                    hc = [slice(h * 128, (h + 1) * 128) for h in range(4)]
                    tok_major_s(tl)
                    yield
                    for c in range(2):
                        rw = slice(c * 64, (c + 1) * 64)
                        pks = bank("st")
                        for h in range(4):
                            E("pe", lambda e, h=h, pks=pks: e.matmul(pks.t[:, hc[h]], lhsT=KTA.t[:, h, cs], rhs=SAB.t[:, hc[h]], start=True, stop=True),
                              reads=[KTA, SAB], writes=[pks])
                            yield
                        pqs = bank("o")
                        for h in range(4):
                            E("pe", lambda e, h=h, pqs=pqs: e.matmul(pqs.t[:, hc[h]], lhsT=QTA.t[:, h, cs], rhs=SAB.t[:, hc[h]], start=True, stop=True),
                              reads=[QTA, SAB], writes=[pqs])
                            yield
                        E("dve", lambda e, pks=pks, rw=rw, sc=sc: e.tensor_tensor(out=v3(TMPZ.t[rw, :], 4), in0=v3(pks.t[rw, :], 4),
                                                                                  in1=bc3(NEGC.t[rw, sc], 4, 128), op=OP.mult),
                          reads=[pks, NEGC], writes=[TMPZ])
                        yield
                        E("dve", lambda e, rw=rw, tl=tl: e.tensor_tensor(out=ZZ.t[rw, :], in0=TMPZ.t[rw, :], in1=VA[tl].t[rw, :], op=OP.add),
                          reads=[TMPZ, VA[tl]], writes=[ZZ])
                        yield
                        E("dve", lambda e, pqs=pqs, rw=rw, sc=sc: e.tensor_tensor(out=v3(OALLL[tl].t[rw, 0:512], 4), in0=v3(pqs.t[rw, :], 4),
                                                                                  in1=bc3(EGC.t[rw, sc], 4, 128), op=OP.mult),
                          reads=[pqs, EGC], writes=[OALLL[tl]])
                        yield
                        pxz = bank("st")
                        for h in range(4):
                            E("pe", lambda e, h=h, pxz=pxz, rw=rw: e.matmul(pxz.t[:, hc[h]], lhsT=YFL[tl].t[rw, hc[h]], rhs=ZZ.t[rw, hc[h]], start=True, stop=True),
                              reads=[YFL[tl], ZZ], writes=[pxz])
                            yield
                        E("dve", lambda e, pxz=pxz, rw=rw, sc=sc: e.tensor_tensor(out=v3(VN.t[rw, :], 4), in0=v3(pxz.t[rw, :], 4),
                                                                                  in1=bc3(BETA.t[rw, sc], 4, 128), op=OP.mult),
                          reads=[pxz, BETA], writes=[VN])
                        yield
                        pkv = bank("st")
                        for h in range(4):
                            E("pe", lambda e, h=h, pkv=pkv, rw=rw, tl=tl: e.matmul(pkv.t[:, hc[h]], lhsT=KDEC[tl].t[rw, hc[h]], rhs=VN.t[rw, hc[h]],
                                                                                  start=True, stop=True),
                              reads=[KDEC[tl], VN], writes=[pkv])
                            yield
                        for h in range(4):
                            li = tl * 8 + c * 4 + h
                            E("dve", lambda e, h=h, pkv=pkv, li=li: e.scalar_tensor_tensor(out=SA.t[:, hc[h]], in0=SA.t[:, hc[h]], scalar=LASTB.t[:, li:li + 1],
                                                                                           in1=pkv.t[:, hc[h]], op0=OP.mult, op1=OP.add),
                              reads=[SA, LASTB, pkv], writes=[SA])
                            yield
                        E("act", lambda e: e.activation(out=SAB.t[:, :], in_=SA.t[:, :], func=AF.Copy), reads=[SA], writes=[SAB])
                        yield
                    pav = bank("o")
                    for h in range(4):
                        E("pe", lambda e, h=h, pav=pav: e.matmul(pav.t[:, hc[h]], lhsT=ATTL[tl].t[:, hc[h]], rhs=VN.t[:, hc[h]], start=True, stop=True),
                          reads=[ATTL[tl], VN], writes=[pav])
                        yield
                    E("dve", lambda e, pav=pav: e.tensor_tensor(out=OALLL[tl].t[:, 0:512], in0=pav.t[:, :], in1=OALLL[tl].t[:, 0:512], op=OP.add),
                      reads=[pav, OALLL[tl]], writes=[OALLL[tl]])
                    yield

                    pq = bank("st")
                    for h in range(4):
                        E("pe", lambda e, h=h, pq=pq: e.matmul(pq.t[:, hc[h]], lhsT=KTB.t[:, h, cs], rhs=QTB.t[:, h, cs], start=True, stop=True),
                          reads=[KTB, QTB], writes=[pq])
                        yield
                    E("dve", lambda e, pq=pq: e.tensor_tensor(out=ATB.t[:, :], in0=pq.t[:, :], in1=cf("e4"), op=OP.mult), reads=[pq, CF], writes=[ATB])
                    yield
                    pob = bank("o")
                    for h in range(4):
                        E("pe", lambda e, h=h, pob=pob: e.matmul(pob.t[:, hc[h]], lhsT=QTB.t[:, h, cs], rhs=SBB.t[:, hc[h]], start=True, stop=False),
                          reads=[QTB, SBB], writes=[pob])
                        yield
                        E("pe", lambda e, h=h, pob=pob, tl=tl: e.matmul(pob.t[:, hc[h]], lhsT=ATB.t[:, hc[h]], rhs=VB[tl].t[:, hc[h]], start=False, stop=True),
                          reads=[ATB, VB[tl]], writes=[pob])
                        yield
                    E("dve", lambda e, pob=pob: e.tensor_tensor(out=v3(OALLL[tl].t[:, 512:1024], 4), in0=v3(pob.t[:, :], 4), in1=bc3(cf("osc"), 4, 128), op=OP.mult),
                      reads=[pob, CF], writes=[OALLL[tl]])
                    yield
                    psb = bank("st")
                    for h in range(4):
                        E("pe", lambda e, h=h, psb=psb, tl=tl: e.matmul(psb.t[:, hc[h]], lhsT=KDECB[tl].t[:, hc[h]], rhs=VB[tl].t[:, hc[h]], start=True, stop=True),
                          reads=[KDECB[tl], VB[tl]], writes=[psb])
                        yield
                    for h in range(4):
                        E("dve", lambda e, h=h, psb=psb: e.scalar_tensor_tensor(out=SBS.t[:, hc[h]], in0=SBS.t[:, hc[h]], scalar=float(GAM[h] ** 128),
                                                                                in1=psb.t[:, hc[h]], op0=OP.mult, op1=OP.add),
                          reads=[SBS, psb], writes=[SBS])
                        yield
                    E("act", lambda e: e.activation(out=SBB.t[:, :], in_=SBS.t[:, :], func=AF.Copy), reads=[SBS], writes=[SBB])
                    yield

                    if debug:
                        E("sp", lambda e, t=t: e.dma_start(out=dbg_o[t * 128:(t + 1) * 128, :], in_=OALLL[tl].t[:, :]), reads=[OALLL[tl]], dma=dbgsem)
                        yield

                    precast(2 * t)
                    precast(2 * t + 1)
                    yield
                    yield
                def thrG(tl, b=b, gb="g"):
                    t = b * TPB + tl
                    cs = slice(tl * 128, (tl + 1) * 128)
                    sc = slice(tl * 4, (tl + 1) * 4)
                    hc = [slice(h * 128, (h + 1) * 128) for h in range(4)]
                    E("pool", lambda e: e.tensor_tensor(out=OSQ.t[:, :], in0=OALLL[tl].t[:, :], in1=OALLL[tl].t[:, :], op=OP.mult), reads=[OALLL[tl]], writes=[OSQ])
                    yield
                    E("dve", lambda e: e.tensor_reduce(out=RSTD8.t[:, :], in_=v3(OSQ.t[:, :], 8), axis=AX.X, op=OP.add), reads=[OSQ], writes=[RSTD8])
                    yield
                    rstd_from_ss(RSTD8.t[:, :], RSTD8.t[:, :], 128, [RSTD8], [RSTD8])
                    E("dve", lambda e: e.tensor_tensor(out=v3(OSQ.t[:, :], 8), in0=v3(OALLL[tl].t[:, :], 8), in1=bc3(RSTD8.t[:, :], 8, 128), op=OP.mult),
                      reads=[OALLL[tl], RSTD8], writes=[OSQ])
                    yield
                    E("dve", lambda e, tl=tl: e.tensor_tensor(out=MIX.t[:, :], in0=OSQ.t[:, :], in1=SG[tl].t[:, :], op=OP.mult),
                      reads=[OSQ, SG[tl]], writes=[MIX])
                    yield
                    pt = bank(gb)
                    for kc in range(8):
                        E("pe", lambda e, kc=kc, pt=pt: e.transpose(out=bfv(pt)[:, kc * 128:(kc + 1) * 128], in_=MIX.t[:, kc * 128:(kc + 1) * 128], identity=IDB),
                          reads=[MIX, CBF], writes=[pt])
                        yield
                    E("act", lambda e, pt=pt: e.activation(out=MIXT.t[:, :, :], in_=v3(bfv(pt)[:, :], 8), func=AF.Copy), reads=[pt], writes=[MIXT])
                    yield
                    E("sp", lambda e, t=t: e.dma_start(out=X1.t[:, :], in_=x[t * 128:(t + 1) * 128, :]), writes=[X1], dma=xrsem)
                    yield
                    for half in range(2):
                        pm = bank(gb)
                        for kc in range(8):
                            E("pe", lambda e, kc=kc, pm=pm, half=half: e.matmul(pm.t[:, :], lhsT=MIXT.t[:, kc, :], rhs=WOUT.t[:, kc, half * 512:(half + 1) * 512],
                                                                               start=(kc == 0), stop=(kc == 7)),
                              reads=[MIXT, WOUT], writes=[pm])
                            yield
                        E("dve", lambda e, pm=pm, half=half, tl=tl: e.tensor_tensor(out=X1.t[:, half * 512:(half + 1) * 512], in0=pm.t[:, :],
                                                                                   in1=X1.t[:, half * 512:(half + 1) * 512], op=OP.add),
                          reads=[pm, X1], writes=[X1])
                        yield
                    E("sp", lambda e, t=t: e.dma_start(out=x1_scr[t * 128:(t + 1) * 128, :], in_=X1.t[:, :]), reads=[X1], dma=x1sem)
                    yield
                    if t == 0:
                        dump("mix", MIX.t[:, :], [128, D], BF16, [MIX])
                    E("pool", lambda e: e.memset(SSG.t[:, 2:3], 0.0), writes=[SSG])
                    yield
                    E("act", lambda e: e.activation(out=H2B.t[:, :], in_=X1.t[:, :], func=AF.Square, accum_out=SSG.t[:, 2:3]),
                      reads=[X1, SSG], writes=[H2B, SSG])
                    yield
                    rstd_from_ss(SSG.t[:, 2:3], SSG.t[:, 3:4], D, [SSG], [SSG])
                    E("act", lambda e: e.activation(out=H2B.t[:, :], in_=X1.t[:, :], func=AF.Copy, scale=SSG.t[:, 3:4]), reads=[X1, SSG], writes=[H2B])
                    yield
                    for half in range(2):
                        pc = bank(gb)
                        for q in range(4):
                            kc = half * 4 + q
                            E("pe", lambda e, kc=kc, q=q, pc=pc: e.transpose(out=pc.t[:, q * 128:(q + 1) * 128], in_=X1.t[:, kc * 128:(kc + 1) * 128], identity=IDF),
                              reads=[X1, CF], writes=[pc])
                            yield
                        E("act", lambda e, pc=pc, half=half: e.activation(out=H2T.t[:, half * 4:(half + 1) * 4, :], in_=v3(pc.t[:, :], 4), func=AF.Copy),
                          reads=[pc], writes=[H2T])
                        yield
                    pl = bank(gb)
                    for kc in range(8):
                        E("pe", lambda e, kc=kc, pl=pl: e.matmul(pl.t[:, 0:72], lhsT=H2T.t[:, kc, :], rhs=W_R.t[:, kc * 72:(kc + 1) * 72],
                                                                 start=(kc == 0), stop=(kc == 7)),
                          reads=[H2T, W_R], writes=[pl])
                        yield
                    E("dve", lambda e, pl=pl: e.tensor_scalar(out=LG.t[:, :], in0=pl.t[:, 0:72], scalar1=SSG.t[:, 3:4], scalar2=None, op0=OP.mult), reads=[pl, SSG], writes=[LG])
                    yield
                    R = RT.t
                    E("dve", lambda e: e.tensor_reduce(out=R[:, 0:1], in_=LG.t[:, 0:8], axis=AX.X, op=OP.max), reads=[LG], writes=[RT])
                    yield
                    E("dve", lambda e: e.tensor_scalar(out=GMASK.t[:, :], in0=LG.t[:, 0:8], scalar1=R[:, 0:1], scalar2=None, op0=OP.is_equal),
                      reads=[LG, RT], writes=[GMASK])
                    yield
                    E("dve", lambda e: e.tensor_scalar(out=R[:, 1:2], in0=R[:, 0:1], scalar1=-1.0, scalar2=None, op0=OP.mult), reads=[RT], writes=[RT])
                    yield
                    E("pool", lambda e: e.memset(R[:, 2:3], 0.0), writes=[RT])
                    yield
                    E("act", lambda e: e.activation(out=PEN.t[:, :], in_=LG.t[:, 0:8], func=AF.Exp, bias=R[:, 1:2], accum_out=R[:, 2:3]),
                      reads=[LG, RT], writes=[PEN, RT])
                    yield
                    E("dve", lambda e: e.reciprocal(out=R[:, 3:4], in_=R[:, 2:3]), reads=[RT], writes=[RT])
                    yield
                    E("dve", lambda e: e.tensor_scalar(out=PEN.t[:, :], in0=GMASK.t[:, :], scalar1=1e30, scalar2=-1e30, op0=OP.mult, op1=OP.add),
                      reads=[GMASK], writes=[PEN])
                    yield
                    E("dve", lambda e: e.tensor_tensor(out=v3(EL.t[:, :], 8), in0=v3(LG.t[:, 8:72], 8), in1=bc3(PEN.t[:, :], 8, 8), op=OP.add),
                      reads=[LG, PEN], writes=[EL])
                    yield
                    E("dve", lambda e: e.tensor_reduce(out=R[:, 4:5], in_=EL.t[:, :], axis=AX.X, op=OP.max), reads=[EL], writes=[RT])
                    yield
                    E("dve", lambda e: e.tensor_scalar(out=OH1.t[:, :], in0=EL.t[:, :], scalar1=R[:, 4:5], scalar2=None, op0=OP.is_equal),
                      reads=[EL, RT], writes=[OH1])
                    yield
                    E("dve", lambda e: e.scalar_tensor_tensor(out=EL2.t[:, :], in0=OH1.t[:, :], scalar=-1e30, in1=EL.t[:, :], op0=OP.mult, op1=OP.add),
                      reads=[OH1, EL], writes=[EL2])
                    yield
                    E("dve", lambda e: e.tensor_reduce(out=R[:, 5:6], in_=EL2.t[:, :], axis=AX.X, op=OP.max), reads=[EL2], writes=[RT])
                    yield
                    E("dve", lambda e: e.tensor_scalar(out=OH2.t[:, :], in0=EL2.t[:, :], scalar1=R[:, 5:6], scalar2=None, op0=OP.is_equal),
                      reads=[EL2, RT], writes=[OH2])
                    yield
                    E("dve", lambda e: e.tensor_tensor(out=R[:, 6:7], in0=R[:, 5:6], in1=R[:, 4:5], op=OP.subtract), reads=[RT], writes=[RT])
                    yield
                    E("act", lambda e: e.activation(out=R[:, 7:8], in_=R[:, 6:7], func=AF.Exp), reads=[RT], writes=[RT])
                    yield
                    E("dve", lambda e: e.tensor_scalar(out=R[:, 8:9], in0=R[:, 7:8], scalar1=1.0, scalar2=None, op0=OP.add), reads=[RT], writes=[RT])
                    yield
                    E("dve", lambda e: e.reciprocal(out=R[:, 9:10], in_=R[:, 8:9]), reads=[RT], writes=[RT])
                    yield
                    E("dve", lambda e: e.tensor_tensor(out=R[:, 10:11], in0=R[:, 7:8], in1=R[:, 9:10], op=OP.mult), reads=[RT], writes=[RT])
                    yield
                    E("dve", lambda e, t=t: e.tensor_scalar(out=CW.t[:, 2 * t:2 * t + 2], in0=R[:, 9:11], scalar1=R[:, 3:4], scalar2=None, op0=OP.mult),
                      reads=[RT], writes=[CW])
                    yield
                    if t == 0:
                        dump("lg", LG.t[:, :], [128, 72], F32, [LG])
                        dump("rt", RT.t[:, :], [128, 16], F32, [RT])
                    E("dve", lambda e: e.tensor_tensor(out=OH12.t[:, :], in0=OH1.t[:, :], in1=OH2.t[:, :], op=OP.add), reads=[OH1, OH2], writes=[OH12])
                    yield
                    pcn = bank(gb)
                    E("pe", lambda e, pcn=pcn: e.matmul(pcn.t[:, 0:64], lhsT=SUB, rhs=OH12.t[:, :], start=True, stop=True), reads=[CBF, OH12], writes=[pcn])
                    yield
                    E("pe", lambda e, pcn=pcn: e.matmul(pcn.t[:, 64:128], lhsT=ONB, rhs=OH12.t[:, :], start=True, stop=True), reads=[CBF, OH12], writes=[pcn])
                    yield
                    E("dve", lambda e, pcn=pcn: e.tensor_tensor(out=POSM.t[:, :], in0=pcn.t[:, 0:64], in1=BASECAP.t[:, :], op=OP.add),
                      reads=[pcn, BASECAP], writes=[POSM])
                    yield
                    E("dve", lambda e, pcn=pcn: e.tensor_tensor(out=BASECAP.t[:, :], in0=pcn.t[:, 64:128], in1=BASECAP.t[:, :], op=OP.add),
                      reads=[pcn, BASECAP], writes=[BASECAP])
                    yield
                    for k, oh in enumerate((OH1, OH2)):
                        E("dve", lambda e, oh=oh: e.tensor_tensor(out=PRD.t[:, :], in0=oh.t[:, :], in1=POSM.t[:, :], op=OP.mult), reads=[oh, POSM], writes=[PRD])
                        yield
                        E("dve", lambda e, k=k: e.tensor_reduce(out=OFF_F.t[:, k:k + 1], in_=PRD.t[:, :], axis=AX.X, op=OP.add), reads=[PRD], writes=[OFF_F])
                        yield
                    E("dve", lambda e, t=t: e.tensor_copy(out=OFFS.t[:, 2 * t:2 * t + 2], in_=OFF_F.t[:, :]), reads=[OFF_F], writes=[OFFS])
                    yield
                    for k in range(2):
                        E("pool", lambda e, t=t, k=k: e.indirect_dma_start(out=xs_all[:, :], out_offset=bass.IndirectOffsetOnAxis(ap=OFFS.t[:, 2 * t + k:2 * t + k + 1], axis=0),
                                                                          in_=H2B.t[:, :], in_offset=None),
                          reads=[H2B, OFFS, XSB], dma=scsem)
                        yield
                    yield
                if b == 0:
                    E("sp", lambda e: e.dma_start(out=ROPE.t[:, :, :], in_=rope_d[:, :, 0:BLK]), writes=[ROPE], dma=rsem)
                run_rr([thrP1(), pendG[0]])
                if b > 0:
                    E("sp", lambda e, b=b: e.dma_start(out=ROPE.t[:, :, :], in_=rope_d[:, :, b * BLK:(b + 1) * BLK]), writes=[ROPE], dma=rsem)
                run_rr([thrL(0, SQ, RINV, "cv"), thrL(1, SQ_B, RINV_B, "tr"), thrScal()])
                dumps23()
                n0 = thrN(0, 0); n1 = thrN(1, 1); rp = thrRope(); s0 = thrS(0)
                nA = thrN(2, 0)
                nB = thrN(3, 1)
                run_rr([s0, n0, n1, rp, nA], after={s0: [n0, rp], nA: [n0]}, stop_on=(0,), must_finish=(1, 2, 3))
                run_rr([thrS(1), nA, nB, thrG(0)], stop_on=(0,), must_finish=(1, 3))
                run_rr([thrS(2), nB, thrG(1)])
                if b + 1 < NB:
                    ldx_blk(b + 1, 0)
                    ldx_blk(b + 1, 1)
                run_rr([thrS(3), thrG(2)])
                pendG[0] = thrG(TPB - 1, gb="g2")
            run_rr([pendG[0]])
            if debug:
                offs_d = dram("offs_d", [128, NT * 2], I32, "ExternalOutput")
                cw_d = dram("cw_d", [128, NT * 2], F32, "ExternalOutput")
                E("sp", lambda e: e.dma_start(out=offs_d, in_=OFFS.t[:, :]), reads=[OFFS], dma=fw.dsem())
                E("sp", lambda e: e.dma_start(out=cw_d, in_=CW.t[:, :]), reads=[CW], dma=fw.dsem())
            fw.barrier()

        with ExitStack() as p2:
            def sb2(name, shape, ty):
                return sb(name, shape, ty, p2)
            NWB = 6
            WGU = [sb2("WGU%d" % i, [128, 4096], BF16) for i in range(NWB)]
            WD = [sb2("WD%d" % i, [128, 2048], BF16) for i in range(NWB)]
            wsm = [fw.dsem() for _ in range(NWB)]
            wsm2 = [fw.dsem() for _ in range(NWB)]
            XS = [sb2("XS%d" % i, [128, 2, D], BF16) for i in range(4)]
            xsm = [fw.dsem() for _ in range(4)]
            XST = [sb2("XST%d" % i, [128, 8, CAP], BF16) for i in range(2)]
            GS = [sb2("GS%d" % i, [128, CAP], F32) for i in range(2)]
            ACTT = [sb2("ACTT%d" % i, [128, 2, CAP], BF16) for i in range(2)]
            YS = [sb2("YS%d" % i, [128, 2, D], BF16) for i in range(2)]
            ysm = [fw.dsem() for _ in range(2)]
            YAB = Buf("y_all")
            xs_e = xs_all.rearrange("(e r p) d -> e p r d", e=NE, p=128)
            y_e = y_all.rearrange("(e r p) d -> e p r d", e=NE, p=128)

            def load_w(ex):
                i = ex % NWB
                E("pool", lambda e: e.dma_start(out=WGU[i].t[:, :], in_=wgu_bf[ex]), writes=[WGU[i]], dma=wsm[i])
                E("pool", lambda e: e.dma_start(out=WD[i].t[:, :], in_=wd_bf[ex]), writes=[WD[i]], dma=wsm2[i])

            def ldxs(ex):
                j4 = ex % 4
                E("sp", lambda e: e.dma_start(out=XS[j4].t[:, :, :], in_=xs_e[ex]), reads=[XSB], writes=[XS[j4]], dma=xsm[j4])

            def stA(ex):
                j = ex % 2
                j4 = ex % 4
                for r in range(2):
                    pt = bank("tr2")
                    for kc in range(8):
                        E("pe", lambda e, kc=kc, pt=pt, r=r: e.transpose(out=bfv(pt)[:, kc * 128:(kc + 1) * 128], in_=XS[j4].t[:, r, kc * 128:(kc + 1) * 128], identity=IDB),
                          reads=[XS[j4], CBF], writes=[pt])
                    E("dve", lambda e, pt=pt, r=r: e.tensor_tensor(out=XST[j].t[:, :, r * 128:(r + 1) * 128], in0=v3(bfv(pt)[:, :], 8),
                                                                   in1=bc3(COLS.t[:, 56:64], 8, 128), op=OP.mult),
                      reads=[pt, COLS], writes=[XST[j]])

            def stB(ex):
                i = ex % NWB
                j = ex % 2
                for fc in range(2):
                    pg = bank("mm4")
                    for kc in range(8):
                        E("pe", lambda e, kc=kc, pg=pg, fc=fc: e.matmul(pg.t[:, 0:CAP], lhsT=WGU[i].t[:, kc * 256 + fc * 128:kc * 256 + fc * 128 + 128],
                                                                       rhs=XST[j].t[:, kc, :], start=(kc == 0), stop=(kc == 7)),
                          reads=[WGU[i], XST[j]], writes=[pg])
                    pu = bank("mm4")
                    for kc in range(8):
                        E("pe", lambda e, kc=kc, pu=pu, fc=fc: e.matmul(pu.t[:, 0:CAP], lhsT=WGU[i].t[:, 2048 + kc * 256 + fc * 128:2048 + kc * 256 + fc * 128 + 128],
                                                                       rhs=XST[j].t[:, kc, :], start=(kc == 0), stop=(kc == 7)),
                          reads=[WGU[i], XST[j]], writes=[pu])
                    gs = GS[fc]
                    E("act", lambda e, pg=pg, gs=gs: e.activation(out=gs.t[:, :], in_=pg.t[:, 0:CAP], func=AF.Silu), reads=[pg], writes=[gs])
                    E("dve", lambda e, pu=pu, fc=fc, gs=gs: e.tensor_tensor(out=ACTT[j].t[:, fc, :], in0=pu.t[:, 0:CAP], in1=gs.t[:, :], op=OP.mult),
                      reads=[pu, gs], writes=[ACTT[j]])

            def stC(ex):
                i = ex % NWB
                j = ex % 2
                for r in range(2):
                    for half in range(2):
                        py = bank("dw")
                        for fc in range(2):
                            E("pe", lambda e, fc=fc, py=py, r=r, half=half: e.matmul(py.t[:, :], lhsT=ACTT[j].t[:, fc, r * 128:(r + 1) * 128],
                                                                                    rhs=WD[i].t[:, fc * 1024 + half * 512:fc * 1024 + half * 512 + 512],
                                                                                    start=(fc == 0), stop=(fc == 1)),
                              reads=[ACTT[j], WD[i]], writes=[py])
                        if half == 0:
                            E("act", lambda e, py=py, r=r: e.activation(out=YS[j].t[:, r, 0:512], in_=py.t[:, :], func=AF.Copy), reads=[py], writes=[YS[j]])
                        else:
                            E("dve", lambda e, py=py, r=r: e.tensor_copy(out=YS[j].t[:, r, 512:1024], in_=py.t[:, :]), reads=[py], writes=[YS[j]])
                E("sp", lambda e: e.dma_start(out=y_e[ex], in_=YS[j].t[:, :, :]), reads=[YS[j]], dma=ysm[j])

            for ex in range(4):
                load_w(ex)
            for ex in range(3):
                ldxs(ex)
            for it in range(NE + 2):
                if it < NE:
                    stA(it)
                if it + 3 < NE:
                    ldxs(it + 3)
                if 0 <= it - 1 < NE:
                    stB(it - 1)
                if 0 <= it - 2 < NE:
                    stC(it - 2)
                if it + 4 < NE:
                    load_w(it + 4)
            fw.barrier()

        with ExitStack() as p3:
            def sb3(name, shape, ty):
                return sb(name, shape, ty, p3)
            FIN = sb3("FIN", [128, D], F32)
            fsem = fw.dsem()
            E("sp", lambda e: e.dma_start(out=FIN.t[:, :], in_=bass.AP(tensor=fin_d.tensor, offset=0, ap=[[0, 128], [1, D]])), writes=[FIN], dma=fsem)
            NB3 = 4
            X1L = [sb3("X1L%d" % i, [128, D], F32) for i in range(NB3)]
            Y1 = [sb3("Y1_%d" % i, [128, D], BF16) for i in range(NB3)]
            Y2 = [sb3("Y2_%d" % i, [128, D], BF16) for i in range(NB3)]
            l1 = [fw.dsem() for _ in range(NB3)]
            l2 = [fw.dsem() for _ in range(NB3)]
            l3 = [fw.dsem() for _ in range(NB3)]
            ACC = [sb3("ACC%d" % i, [128, D], F32) for i in range(2)]
            OUTT = [sb3("OUTT%d" % i, [128, D], F32) for i in range(2)]
            osm = [fw.dsem() for _ in range(2)]
            JK = sb3("JK", [128, D], BF16)
            S3 = sb3("S3", [128, 4 * NT], F32)
            E("pool", lambda e: e.memset(S3.t[:, :], 0.0), writes=[S3])

            def loads3(t):
                j = t % NB3
                E("sp", lambda e, t=t, j=j: e.dma_start(out=X1L[j].t[:, :], in_=x1_scr[t * 128:(t + 1) * 128, :]), writes=[X1L[j]], dma=l1[j])
                E("pool", lambda e, t=t, j=j: e.indirect_dma_start(out=Y1[j].t[:, :], out_offset=None, in_=y_all[:, :],
                                                                  in_offset=bass.IndirectOffsetOnAxis(ap=OFFS.t[:, 2 * t:2 * t + 1], axis=0)),
                  reads=[OFFS], writes=[Y1[j]], dma=l2[j])
                E("pool", lambda e, t=t, j=j: e.indirect_dma_start(out=Y2[j].t[:, :], out_offset=None, in_=y_all[:, :],
                                                                  in_offset=bass.IndirectOffsetOnAxis(ap=OFFS.t[:, 2 * t + 1:2 * t + 2], axis=0)),
                  reads=[OFFS], writes=[Y2[j]], dma=l3[j])

            for t in range(min(3, NT)):
                loads3(t)
            for t in range(NT):
                j = t % NB3
                k = t % 2
                E("dve", lambda e, t=t, j=j, k=k: e.scalar_tensor_tensor(out=ACC[k].t[:, :], in0=Y1[j].t[:, :], scalar=CW.t[:, 2 * t:2 * t + 1], in1=X1L[j].t[:, :],
                                                                        op0=OP.mult, op1=OP.add),
                  reads=[Y1[j], CW, X1L[j]], writes=[ACC[k]])
                E("dve", lambda e, t=t, j=j, k=k: e.scalar_tensor_tensor(out=ACC[k].t[:, :], in0=Y2[j].t[:, :], scalar=CW.t[:, 2 * t + 1:2 * t + 2], in1=ACC[k].t[:, :],
                                                                        op0=OP.mult, op1=OP.add),
                  reads=[Y2[j], CW, ACC[k]], writes=[ACC[k]])
                if t + 3 < NT:
                    loads3(t + 3)
                E("act", lambda e, t=t, k=k: e.activation(out=JK.t[:, :], in_=ACC[k].t[:, :], func=AF.Square, accum_out=S3.t[:, 4 * t:4 * t + 1]),
                  reads=[ACC[k], S3], writes=[JK, S3])
                rs = S3.t[:, 4 * t + 1:4 * t + 2]
                E("dve", lambda e, t=t, rs=rs: e.tensor_scalar(out=rs, in0=S3.t[:, 4 * t:4 * t + 1], scalar1=1.0 / D, scalar2=EPS, op0=OP.mult, op1=OP.add),
                  reads=[S3], writes=[S3])
                E("act", lambda e, rs=rs: e.activation(out=rs, in_=rs, func=AF.Ln), reads=[S3], writes=[S3])
                E("act", lambda e, rs=rs: e.activation(out=rs, in_=rs, func=AF.Exp, scale=-0.5), reads=[S3], writes=[S3])
                E("dve", lambda e, k=k, rs=rs: e.scalar_tensor_tensor(out=OUTT[k].t[:, :], in0=ACC[k].t[:, :], scalar=rs, in1=FIN.t[:, :], op0=OP.mult, op1=OP.mult),
                  reads=[ACC[k], S3, FIN], writes=[OUTT[k]])
                E("sp", lambda e, t=t, k=k: e.dma_start(out=out[t * 128:(t + 1) * 128, :], in_=OUTT[k].t[:, :]), reads=[OUTT[k]], dma=osm[k])
        fw.finish()
    nc._dump_names = list(dumps.keys())
    return nc


def _consts():
    f = np.float32
    i = np.arange(128)
    same = (i[:, None] // 64) == (i[None, :] // 64)
    cfm = np.zeros((128, NCF), f)

    def put(name, arr):
        a, b = _cfo[name]
        cfm[:, a:b] = arr
    put("ident", np.eye(128, dtype=f))
    put("ub", ((i[:, None] <= i[None, :]) & same).astype(f))
    put("slb", ((i[:, None] > i[None, :]) & same).astype(f))
    put("bb", same.astype(f))
    put("ones", np.ones((128, 128), f))
    inc = (i[None, :] >= i[:, None]) & same
    strict = (i[None, :] > i[:, None]) & same
    put("negm4", np.tile(np.where(inc, 0.0, -30000.0).astype(f), (1, 4)))
    put("strn4", np.where(strict, -1.0, 0.0).astype(f))
    e4 = np.zeros((128, 512), np.float64)
    kd = np.zeros((128, 4), np.float64)
    osc = np.zeros((128, 4), np.float64)
    for h in range(4):
        g = GAM[h]
        m = (i[None, :] >= i[:, None])
        e4[:, h * 128:(h + 1) * 128] = np.where(m, (128.0 ** -0.5) * g ** (-(i[:, None] + 1.0)), 0.0)
        kd[:, h] = (128.0 ** -0.5) * g ** (127.0 - i)
        osc[:, h] = g ** (i + 1.0)
    put("e4", e4.astype(f))
    put("su", (i[:, None] < i[None, :]).astype(f))
    pm = np.zeros((128, 128), f)
    pm[(i + 64) % 128, i] = 1.0
    put("pm", pm)
    put("cm", np.stack([(i < 64), (i >= 64)], 1).astype(f))
    put("kdsc", kd.astype(f))
    put("osc", osc.astype(f))
    put("iotacap", np.tile((np.arange(NE) * CAP).astype(f)[None, :], (128, 1)))
    pos = np.arange(S, dtype=f)
    inv = (f(10000.0) ** (-(np.arange(0, 128, 2, dtype=f)) / f(128.0))).astype(f)
    ang = (pos[:, None] * inv[None, :]).astype(f)
    cos = np.cos(ang).astype(f).T
    sin = np.sin(ang).astype(f).T
    rope = np.zeros((128, 2, S), f)
    rope[0:64, 0] = cos
    rope[64:128, 0] = cos
    rope[0:64, 1] = -sin
    rope[64:128, 1] = sin
    return cfm, rope


_CACHE = {}


def kernel(x, attn_norm, w_in, conv_a, a_log, dt_bias, norm_a, norm_b, w_out, ffn_norm,
           w_router_group, w_router_expert, w_gate, w_up, w_down, final_norm, _debug=False):
    f = np.float32
    x = np.asarray(x, f)
    w_in_l = np.ascontiguousarray(np.asarray(w_in, f)[0].reshape(8, 128, DIN).transpose(1, 0, 2))
    w_out_l = np.ascontiguousarray(np.asarray(w_out, f)[0].reshape(8, 128, D).transpose(1, 0, 2))
    wr = np.concatenate([np.asarray(w_router_group, f)[0], np.asarray(w_router_expert, f)[0]], axis=1)
    w_r_l = np.ascontiguousarray(wr.reshape(8, 128, 72).transpose(1, 0, 2).reshape(128, 8 * 72))
    wg = np.asarray(w_gate, f)[0].reshape(NE, 8, 128, 256).transpose(0, 2, 1, 3).reshape(NE, 128, 2048)
    wu = np.asarray(w_up, f)[0].reshape(NE, 8, 128, 256).transpose(0, 2, 1, 3).reshape(NE, 128, 2048)
    wgu_l = np.ascontiguousarray(np.concatenate([wg, wu], axis=2))
    wd_l = np.ascontiguousarray(np.asarray(w_down, f)[0].reshape(NE, 2, 128, D).transpose(0, 2, 1, 3).reshape(NE, 128, 2048))
    cols = np.zeros((128, NCOL), f)
    ca = np.asarray(conv_a, f)[0]
    cols[:, 0:48] = ca.reshape(4, 12, 128).transpose(2, 1, 0).reshape(128, 48)
    normfull = np.concatenate([np.tile(np.asarray(norm_a, f)[0], 4), np.asarray(norm_b, f)[0]])
    cols[:, 48:56] = normfull.reshape(8, 128).T
    cols[:, 56:64] = np.asarray(ffn_norm, f)[0].reshape(8, 128).T
    cols[:, 64:72] = np.asarray(attn_norm, f)[0].reshape(8, 128).T
    rows = np.concatenate([np.asarray(a_log, f)[0], np.asarray(dt_bias, f)[0]])[None, :].astype(f)
    fin = np.asarray(final_norm, f)[None, :]
    cfm, rope = _consts()
    key = bool(_debug)
    if key not in _CACHE:
        _CACHE[key] = build(debug=_debug)
    nc = _CACHE[key]
    in_maps = []
    ncores = 1 if _debug else NCORES
    for c in range(ncores):
        in_maps.append({"x": np.ascontiguousarray(x[c]), "w_in": w_in_l, "w_out": w_out_l, "w_r": w_r_l, "wgu": wgu_l, "wd": wd_l,
                        "cols": cols, "rows": rows, "fin": fin, "cf": cfm, "rope": rope})
    res = run_bass_kernel_spmd(nc, in_maps, core_ids=list(range(ncores)))
    outp = np.stack([np.asarray(r["out"], f) for r in res.results], axis=0)
    if _debug:
        kernel.dbg = [{k: np.asarray(r[k]) for k in ["x1_scr", "dbg_o", "offs_d", "cw_d"] + ["dd_" + n for n in nc._dump_names]} for r in res.results]
    return outp
```
